# Optimizing a Trainium2 kernel written in Bass

```python
import math
import jax, jax.numpy as jnp
from jax import lax
import numpy as np

D_MODEL = 1024
BATCH = 2
SEQ = 16384
DEPTH = 1
DEC_BATCH = 8
DEC_SEQ = 32
PAST_LEN = 1024

CHUNK = 64
Q_BLOCK = 128
HA = 4
DA = 64
DVA = 2 * DA
HR = 4
DKR = 128
DVR = 128
N_BUCKETS = 32
MAX_DIST = 128
PEER_HEADS = 8
N_KEYS = 128
N_EXPERTS = N_KEYS * N_KEYS
KEY_DIM = 128
PEER_TOPK = 16
PEER_BLOCK = 128
EPS = 1e-6

WA = HA * DVA
WR = HR * DVR
IN_SIZES = (HA * 2 * DA, HA * 2 * DA, HA * DVA, HR * DKR, HR * DKR, HR * DVR, HR * DVR, D_MODEL, D_MODEL)
D_IN = 2 * HA * 2 * DA + HA * DVA + 2 * HR * DKR + 2 * HR * DVR + 2 * D_MODEL

kernel_name = "diffattn_retnet_peer_adaln_stream"

F32 = jnp.float32


def rms(x):
    xf = x.astype(F32)
    return (xf * lax.rsqrt(jnp.mean(xf * xf, axis=-1, keepdims=True) + EPS)).astype(x.dtype)


def ada_mod(c, w_ada, b_ada):
    mod = jax.nn.silu(c) @ w_ada + b_ada
    return jnp.split(mod, 6, axis=-1)


def modulate(x, gain, shift, scale):
    return rms(x) * gain * (1 + scale[:, None, :]) + shift[:, None, :]


def rotate(x, pos):
    inv = 1.0 / (10000.0 ** jnp.linspace(0.0, 1.0, DKR // 2, dtype=F32))
    ang = pos[:, None].astype(F32) * inv[None, :]
    cos = jnp.cos(ang)[None, :, None, :]
    sin = jnp.sin(ang)[None, :, None, :]
    x1, x2 = x[..., : DKR // 2], x[..., DKR // 2:]
    return jnp.concatenate([x1 * cos - x2 * sin, x1 * sin + x2 * cos], axis=-1).astype(x.dtype)


def in_proj(h, w_in, pos):
    B, L = h.shape[:2]
    z = h @ w_in
    qa, ka, va, qr, kr, vr, gr, ga, gb = jnp.split(z, np.cumsum(IN_SIZES)[:-1].tolist(), axis=-1)
    qa = qa.reshape(B, L, HA, 2, DA)
    ka = ka.reshape(B, L, HA, 2, DA)
    va = va.reshape(B, L, HA, DVA)
    qr = rotate(qr.reshape(B, L, HR, DKR), pos)
    kr = rotate(kr.reshape(B, L, HR, DKR), pos) * (DKR ** -0.5)
    vr = vr.reshape(B, L, HR, DVR)
    return qa, ka, va, qr, kr, vr, gr, ga, gb


def t5_bucket(rel):
    nb = N_BUCKETS // 2
    ret = jnp.where(rel > 0, nb, 0)
    n = jnp.abs(rel)
    max_exact = nb // 2
    nf = jnp.maximum(n, max_exact).astype(F32)
    large = max_exact + (jnp.log(nf / max_exact) / math.log(MAX_DIST / max_exact) * (nb - max_exact)).astype(jnp.int32)
    large = jnp.minimum(large, nb - 1)
    return ret + jnp.where(n < max_exact, n, large)


def diff_attn(q, k, v, q_pos, k_pos, rel_bias, lam):
    s = jnp.einsum('bqhmd,bkhmd->bhmqk', q, k).astype(F32) * (DA ** -0.5)
    bias = rel_bias.astype(F32)[t5_bucket(k_pos[None, :] - q_pos[:, None])]
    bias = jnp.transpose(bias, (2, 0, 1))[None, :, None]
    visible = (k_pos[None, :] // CHUNK) <= (q_pos[:, None] // CHUNK)
    s = jnp.where(visible, s + bias, -1e30)
    p = jax.nn.softmax(s, axis=-1)
    attn = p[:, :, 0] - lam * p[:, :, 1]
    return jnp.einsum('bhqk,bkhv->bqhv', attn.astype(v.dtype), v)


def diff_attn_prompt(q, k, v, rel_bias, lam):
    B, S = q.shape[:2]
    nb = S // Q_BLOCK
    k_pos = jnp.arange(S, dtype=jnp.int32)
    qb = q.reshape(B, nb, Q_BLOCK, HA, 2, DA).transpose(1, 0, 2, 3, 4, 5)

    def blk(args):
        i, qi = args
        q_pos = i * Q_BLOCK + jnp.arange(Q_BLOCK, dtype=jnp.int32)
        return diff_attn(qi, k, v, q_pos, k_pos, rel_bias, lam)

    out = lax.map(blk, (jnp.arange(nb, dtype=jnp.int32), qb))
    return out.transpose(1, 0, 2, 3, 4).reshape(B, S, HA, DVA)


def log_gammas():
    return jnp.log(1.0 - 2.0 ** (-5.0 - jnp.arange(HR, dtype=F32)))


def retention_chunk(state, q, k, v):
    L = q.shape[1]
    lg = log_gammas()
    n = jnp.arange(L, dtype=F32)
    diff = n[:, None] - n[None, :]
    decay = jnp.where(diff >= 0, jnp.exp(jnp.maximum(diff, 0.0)[None] * lg[:, None, None]), 0.0)
    scores = jnp.einsum('blhk,bmhk->bhlm', q, k) * decay[None].astype(q.dtype)
    intra = jnp.einsum('bhlm,bmhv->blhv', scores, v)
    xi = jnp.exp((n + 1.0)[:, None] * lg[None, :]).astype(q.dtype)
    cross = jnp.einsum('blhk,bhkv->blhv', q, state) * xi[None, :, :, None]
    zeta = jnp.exp((L - 1.0 - n)[:, None] * lg[None, :]).astype(k.dtype)
    new_state = jnp.exp(L * lg).astype(state.dtype)[None, :, None, None] * state + \
        jnp.einsum('blhk,blhv->bhkv', k * zeta[None, :, :, None], v)
    return intra + cross, new_state


def retention_prompt(q, k, v):
    B, S = q.shape[:2]
    nc = S // CHUNK

    def to_chunks(t):
        return t.reshape(B, nc, CHUNK, HR, t.shape[-1]).swapaxes(0, 1)

    def step(state, xs):
        qc, kc, vc = xs
        o, state = retention_chunk(state, qc, kc, vc)
        return state, o

    state0 = jnp.zeros((B, HR, DKR, DVR), v.dtype)
    state, o = lax.scan(step, state0, (to_chunks(q), to_chunks(k), to_chunks(v)))
    return o.swapaxes(0, 1).reshape(B, S, HR, DVR), state


def out_mix(oa, orr, gr, ga, gb, lam_init, subln_a, subln_r, w_ba, w_br, w_o):
    B, L = oa.shape[:2]
    ya = (rms(oa) * subln_a * (1.0 - lam_init)).reshape(B, L, WA)
    yr = jax.nn.silu(gr) * (rms(orr) * subln_r).reshape(B, L, WR)
    y = jax.nn.sigmoid(ga) * (ya @ w_ba) + jax.nn.sigmoid(gb) * (yr @ w_br)
    return y @ w_o


def peer_ffn(h, w_pq, peer_keys, peer_u, peer_v):
    B, L, D = h.shape
    T = B * L
    pad = (-T) % PEER_BLOCK
    xt = jnp.pad(h.reshape(T, D), ((0, pad), (0, 0)))
    nb = xt.shape[0] // PEER_BLOCK

    def blk(xb):
        q = (xb @ w_pq).reshape(PEER_BLOCK, PEER_HEADS, 2, KEY_DIM)
        s = jnp.einsum('thpd,hpnd->thpn', q, peer_keys).astype(F32)
        sv, si = lax.top_k(s, PEER_TOPK)
        cand = (sv[:, :, 0, :, None] + sv[:, :, 1, None, :]).reshape(PEER_BLOCK, PEER_HEADS, PEER_TOPK * PEER_TOPK)
        cidx = (si[:, :, 0, :, None] * N_KEYS + si[:, :, 1, None, :]).reshape(PEER_BLOCK, PEER_HEADS, PEER_TOPK * PEER_TOPK)
        top_s, top_p = lax.top_k(cand, PEER_TOPK)
        idx = jnp.take_along_axis(cidx, top_p, axis=-1)
        g = jax.nn.softmax(top_s, axis=-1)
        a = jax.nn.gelu(jnp.einsum('td,thkd->thk', xb, peer_u[idx]).astype(F32), approximate=False)
        return jnp.einsum('thk,thkd->td', (g * a).astype(xb.dtype), peer_v[idx])

    out = lax.map(blk, xt.reshape(nb, PEER_BLOCK, D))
    return out.reshape(nb * PEER_BLOCK, D)[:T].reshape(B, L, D)


def trunk_layer(x, c, pos, attend, retain, lam_init, w_ada, b_ada, norm1, norm2, w_in,
                subln_a, subln_r, w_ba, w_br, w_o, w_pq, peer_keys, peer_u, peer_v):
    sh1, sc1, g1, sh2, sc2, g2 = ada_mod(c, w_ada, b_ada)
    h = modulate(x, norm1, sh1, sc1)
    qa, ka, va, qr, kr, vr, gr, ga, gb = in_proj(h, w_in, pos)
    oa = attend(qa, ka, va)
    orr, st = retain(qr, kr, vr)
    x = x + g1[:, None, :] * out_mix(oa, orr, gr, ga, gb, lam_init, subln_a, subln_r, w_ba, w_br, w_o)
    h = modulate(x, norm2, sh2, sc2)
    x = x + g2[:, None, :] * peer_ffn(h, w_pq, peer_keys, peer_u, peer_v)
    B, L = x.shape[:2]
    return x, ka.reshape(B, L, HA, 2 * DA), va, st


def setup_inputs(seed: int = 0) -> dict:
    key = jax.random.key(seed)
    ks = jax.random.split(key, 32)
    nrm = lambda k, shp, s: jax.random.normal(k, shp, F32) * s
    D = D_MODEL
    return {
        "x_prompt": nrm(ks[0], (BATCH, SEQ, D), 1.0),
        "x_sample": nrm(ks[1], (DEC_BATCH, DEC_SEQ, D), 1.0),
        "cache_k": nrm(ks[2], (DEPTH, DEC_BATCH, PAST_LEN, HA, 2 * DA), 1.0),
        "cache_v": nrm(ks[3], (DEPTH, DEC_BATCH, PAST_LEN, HA, DVA), 1.0),
        "state_ret": nrm(ks[4], (DEPTH, DEC_BATCH, HR, DKR, DVR), 0.1),
        "c_prompt": nrm(ks[5], (BATCH, D), 1.0),
        "c_sample": nrm(ks[6], (DEC_BATCH, D), 1.0),
        "w_ada": nrm(ks[7], (DEPTH, D, 6 * D), 0.02),
        "b_ada": nrm(ks[8], (DEPTH, 6 * D), 0.02),
        "norm1": 1.0 + nrm(ks[9], (DEPTH, D), 0.02),
        "norm2": 1.0 + nrm(ks[10], (DEPTH, D), 0.02),
        "w_in": nrm(ks[11], (DEPTH, D, D_IN), D ** -0.5),
        "lam_q1": nrm(ks[12], (DEPTH, DA), 0.1),
        "lam_k1": nrm(ks[13], (DEPTH, DA), 0.1),
        "lam_q2": nrm(ks[14], (DEPTH, DA), 0.1),
        "lam_k2": nrm(ks[15], (DEPTH, DA), 0.1),
        "subln_a": 1.0 + nrm(ks[16], (DEPTH, DVA), 0.02),
        "subln_r": 1.0 + nrm(ks[17], (DEPTH, DVR), 0.02),
        "w_ba": nrm(ks[18], (DEPTH, WA, D), WA ** -0.5),
        "w_br": nrm(ks[19], (DEPTH, WR, D), WR ** -0.5),
        "w_o": nrm(ks[20], (DEPTH, D, D), D ** -0.5),
        "rel_bias": nrm(ks[21], (N_BUCKETS, HA), 0.5),
        "w_pq": nrm(ks[22], (DEPTH, D, PEER_HEADS * 2 * KEY_DIM), D ** -0.5),
        "peer_keys": nrm(ks[23], (DEPTH, PEER_HEADS, 2, N_KEYS, KEY_DIM), KEY_DIM ** -0.5),
        "peer_u": nrm(ks[24], (DEPTH, N_EXPERTS, D), D ** -0.5),
        "peer_v": nrm(ks[25], (DEPTH, N_EXPERTS, D), PEER_HEADS ** -0.5),
        "final_norm": 1.0 + nrm(ks[26], (D,), 0.02),
    }


def reference(x_prompt, x_sample, cache_k, cache_v, state_ret, c_prompt, c_sample,
              w_ada, b_ada, norm1, norm2, w_in, lam_q1, lam_k1, lam_q2, lam_k2,
              subln_a, subln_r, w_ba, w_br, w_o, rel_bias, w_pq, peer_keys, peer_u, peer_v,
              final_norm):
    Bp, Lp = x_prompt.shape[:2]
    Bs, Ls = x_sample.shape[:2]
    pos_p = jnp.arange(Lp, dtype=jnp.int32)
    pos_s = PAST_LEN + jnp.arange(Ls, dtype=jnp.int32)
    k_pos_s = jnp.arange(PAST_LEN + Ls, dtype=jnp.int32)
    xp, xs = x_prompt, x_sample
    kp_l, vp_l, sp_l, ks_l, vs_l, ss_l = [], [], [], [], [], []
    for l in range(DEPTH):
        lam_init = 0.8 - 0.6 * math.exp(-0.3 * l)
        lam = (jnp.exp(jnp.sum(lam_q1[l].astype(F32) * lam_k1[l].astype(F32)))
               - jnp.exp(jnp.sum(lam_q2[l].astype(F32) * lam_k2[l].astype(F32))) + lam_init)
        params = (w_ada[l], b_ada[l], norm1[l], norm2[l], w_in[l], subln_a[l], subln_r[l],
                  w_ba[l], w_br[l], w_o[l], w_pq[l], peer_keys[l], peer_u[l], peer_v[l])

        attend_p = lambda q, k, v: diff_attn_prompt(q, k, v, rel_bias, lam)
        xp, kp, vp, sp = trunk_layer(xp, c_prompt, pos_p, attend_p, retention_prompt, lam_init, *params)

        def attend_s(q, k, v, l=l):
            kc = cache_k[l].reshape(Bs, PAST_LEN, HA, 2, DA).astype(k.dtype)
            k_all = jnp.concatenate([kc, k], axis=1)
            v_all = jnp.concatenate([cache_v[l].astype(v.dtype), v], axis=1)
            return diff_attn(q, k_all, v_all, pos_s, k_pos_s, rel_bias, lam)

        def retain_s(q, k, v, l=l):
            return retention_chunk(state_ret[l].astype(v.dtype), q, k, v)

        xs, ksn, vsn, ssn = trunk_layer(xs, c_sample, pos_s, attend_s, retain_s, lam_init, *params)
        kp_l.append(kp); vp_l.append(vp); sp_l.append(sp)
        ks_l.append(ksn); vs_l.append(vsn); ss_l.append(ssn)

    y_prompt = rms(xp) * final_norm
    y_sample = rms(xs) * final_norm
    return (y_prompt, y_sample, jnp.stack(kp_l), jnp.stack(vp_l), jnp.stack(sp_l),
            jnp.stack(ks_l), jnp.stack(vs_l), jnp.stack(ss_l))
```

```python
import math
from contextlib import ExitStack
from concourse.bass_utils import run_bass_kernel_spmd

import numpy as np
import concourse.bass as bass
import concourse.mybir as mybir

F32 = mybir.dt.float32
BF16 = mybir.dt.bfloat16
I32 = mybir.dt.int32
U32 = mybir.dt.uint32
U16 = mybir.dt.uint16
ALU = mybir.AluOpType
AF = mybir.ActivationFunctionType
AX = mybir.AxisListType

SEM_WINDOW = 12000
SAME_ENGINE_SYNC = True


class Obj:
    __slots__ = ("name", "w", "r", "excl")

    def __init__(self, name):
        self.name = name
        self.w = None
        self.r = {}
        self.excl = False


class Prog:
    ENGS = ("pe", "act", "dve", "pool", "sp")

    def __init__(self, nc, stack):
        self.nc = nc
        self.stack = stack
        self.cnt = {e: 0 for e in self.ENGS}
        self.ops = {e: [] for e in self.ENGS}
        self.waited = {e: {} for e in self.ENGS}
        self.esems = {}
        self.dsems = {}
        self.dcount = {}
        self.nsem = 0
        self.keymap = {}
        self.next_phys = 0

    def _esem(self, eng, k):
        key = (eng, k)
        if key not in self.esems:
            self.esems[key] = self.stack.enter_context(self.nc.semaphore(f"s_{eng}_{k}"))
            self.nsem += 1
        return self.esems[key]

    def _dkey(self, key):
        if key not in self.keymap:
            name = f"D{self.next_phys}"
            self.next_phys += 1
            if name not in self.dsems:
                self.dsems[name] = self.stack.enter_context(self.nc.semaphore(f"d_{name}"))
                self.dcount[name] = 0
                self.nsem += 1
            self.keymap[key] = name
        return self.keymap[key]

    def _deps(self, eng, reads, writes):
        deps = {}

        def add(src, val):
            if val > deps.get(src, 0):
                deps[src] = val

        for o in reads:
            if o.w is not None:
                add(*o.w)
            if o.excl:
                for s, v in o.r.items():
                    if s != eng:
                        add(s, v)
        for o in writes:
            if o.w is not None:
                add(*o.w)
            for s, v in o.r.items():
                add(s, v)
        waits = []
        for src, val in deps.items():
            if src == eng:
                if eng == "pe" or not SAME_ENGINE_SYNC:
                    continue
            if self.waited[eng].get(src, 0) >= val:
                continue
            self.waited[eng][src] = val
            if src in self.ENGS:
                k = (val - 1) // SEM_WINDOW
                waits.append((self._esem(src, k), (val - 1) % SEM_WINDOW + 1))
            else:
                waits.append((self.dsems[src], val))
        return waits

    def _mark(self, ev, reads, writes):
        src, val = ev
        for o in reads:
            if val > o.r.get(src, 0):
                o.r[src] = val
        for o in writes:
            o.w = ev
            o.r = {}

    def op(self, eng, fn, reads=(), writes=()):
        waits = self._deps(eng, reads, writes)
        n = self.cnt[eng] + 1
        self.cnt[eng] = n
        k = (n - 1) // SEM_WINDOW
        sem = self._esem(eng, k)
        self.ops[eng].append((waits, fn, sem, 1))
        self._mark((eng, n), reads, writes)

    def dma(self, q, fn, reads=(), writes=(), key=None):
        waits = self._deps(q, reads, writes)
        name = self._dkey(key)
        sem = self.dsems[name]
        self.dcount[name] += 16
        self.ops[q].append((waits, fn, sem, 16))
        self._mark((name, self.dcount[name]), reads, writes)

    def wait_all_dma(self, q="sp"):
        waits = []
        for key, sem in self.dsems.items():
            if self.dcount[key] > self.waited[q].get(key, 0):
                waits.append((sem, self.dcount[key]))
        self.ops[q].append((waits, None, None, 0))

    def emit(self):
        nc = self.nc
        handles = {"pe": "tensor", "act": "scalar", "dve": "vector", "pool": "gpsimd", "sp": "sync"}
        with nc.Block() as block:
            for eng in self.ENGS:
                lst = self.ops[eng]

                def body(e, lst=lst):
                    for waits, fn, sem, inc in lst:
                        for s, v in waits:
                            e.wait_ge(s, v)
                        if fn is not None:
                            ins = fn(e)
                            ins.then_inc(sem, inc)

                getattr(block, handles[eng])(body)


def _barrier(self):
    snap = [(s, v) for s, v in list(self.cnt.items()) + list(self.dcount.items()) if v > 0]
    for e in self.ENGS:
        waits = []
        for src, val in snap:
            if src == e and e in ("pe", "sp"):
                continue
            if self.waited[e].get(src, 0) >= val:
                continue
            self.waited[e][src] = val
            if src in self.ENGS:
                k = (val - 1) // SEM_WINDOW
                waits.append((self._esem(src, k), (val - 1) % SEM_WINDOW + 1))
            else:
                waits.append((self.dsems[src], val))
        self.ops[e].append((waits, None, None, 0))
    self.keymap = {}
    self.next_phys = 0


Prog.barrier = _barrier


D = 1024
NT = 128
OWN = [16 * i + 12 + j for i in range(8) for j in range(4)]
HA = 4
EPS = 1e-6
QA, KA, VA, QR, KR, VR, GR, GA, GB = 0, 512, 1024, 1536, 2048, 2560, 3072, 3584, 4608
LAM_INIT = 0.8 - 0.6 * math.exp(-0.3 * 0)
GAMMAS = [1.0 - 2.0 ** (-5.0 - h) for h in range(4)]
R0 = 511
NEG = -30000.0
WB = 49152
AR = 39936
import os
STOP = os.environ.get("MK_STOP", "")


class T:
    def __init__(self, ap, name):
        self.t = ap
        self.o = Obj(name)


def _prod(s):
    r = 1
    for v in s:
        r *= v
    return r


class Carver:
    def __init__(self, buf, size):
        self.buf = buf
        self.size = size
        self.off = 0

    def reset(self):
        self.off = 0

    def get(self, shape, dt, name):
        n = _prod(shape[1:])
        sz = n * (2 if dt in (F32, I32, U32) else 1)
        sz = (sz + 15) // 16 * 16
        assert self.off + sz <= self.size, (name, self.off, sz, self.size)
        v = self.buf[0:shape[0], self.off:self.off + sz]
        self.off += sz
        if dt != BF16:
            v = v.bitcast(dt)
        v = v[:, 0:n]
        if len(shape) == 3:
            v = v.rearrange("p (a b) -> p a b", a=shape[1])
        elif len(shape) == 4:
            v = v.rearrange("p (a b c) -> p a b c", a=shape[1], b=shape[2])
        return T(v, name)


def bcast(ap2, dims):
    base = ap2.ap
    new = [base[0]]
    for d in dims:
        if d[0] == 'b':
            new.append((0, d[1]))
        else:
            new.append(base[1])
    return bass.AP(ap2.tensor, ap2.offset, tuple(new))


def build_nc():
    nc = bass.Bass("TRN2", target_bir_lowering=False)
    din = lambda name, shape, dt=F32: nc.dram_tensor(name, list(shape), dt, kind="ExternalInput")
    dout = lambda name, shape, dt=F32: nc.dram_tensor(name, list(shape), dt, kind="ExternalOutput")
    dscr = lambda name, shape, dt: nc.dram_tensor(name, list(shape), dt, kind="Internal")

    x_all = din("x_all", [NT * 128, D]).ap()
    x_s = din("x_s", [32, D]).ap()
    cache_k = din("cache_k", [1024, 512]).ap()
    cache_v = din("cache_v", [1024, 512]).ap()
    state_in = din("state_in", [4, 128, 128]).ap()
    c_both = din("c_both", [2, D]).ap()
    w_ada = din("w_ada", [D, 6 * D]).ap()
    b_ada = din("b_ada", [6 * D])
    norm1 = din("norm1", [D]).ap()
    norm2 = din("norm2", [D]).ap()
    w_in = din("w_in", [D, 5632]).ap()
    lamv = din("lamv", [4, 64])
    subln = din("subln", [2, 128]).ap()
    w_ba = din("w_ba", [512, D]).ap()
    w_br = din("w_br", [512, D]).ap()
    w_o = din("w_o", [D, D]).ap()
    rel_bias = din("rel_bias", [32, 4])
    w_pq = din("w_pq", [D, 2048]).ap()
    peer_keys = din("peer_keys", [16, 128, 128]).ap()
    peer_u = din("peer_u", [16384, D])
    peer_v = din("peer_v", [16384, D])
    final_norm = din("final_norm", [D])
    rot = din("rot", [NT + 1, 128, 256]).ap()
    dtab_d = din("dtab", [128, 1296]).ap()
    mcol_d = din("mcol", [128, 1024]).ap()
    vsv_d = din("vsv", [128, 3]).ap()
    oht5 = din("oht5", [32, 1280]).ap()
    jrev_d = din("jrev", [128, 128]).ap()
    ident_d = din("ident", [128, 128]).ap()
    zwin_d = din("zwin", [128, 256]).ap()
    iota_d = din("iota16", [128, 16]).ap()

    y_own = dout("y_own", [4096, D]).ap()
    k_own = dout("k_own", [4096, 512]).ap()
    v_own = dout("v_own", [4096, 512]).ap()
    ret_p = dout("ret_p", [4, 128, 128]).ap()
    y_s = dout("y_s", [32, D]).ap()
    k_s = dout("k_s", [32, 512]).ap()
    v_s = dout("v_s", [32, 512]).ap()
    ret_s = dout("ret_s", [4, 128, 128]).ap()

    Kscr = dscr("Kscr", [4, 128, 16384], BF16).ap()
    Vscr = dscr("Vscr", [4, 128, 128, 128], BF16).ap()
    Qscr = dscr("Qscr", [4, 128, 4096], BF16).ap()
    YAscr = dscr("YAscr", [4, 128, 4096], BF16).ap()
    YRscr = dscr("YRscr", [4, 128, 4096], BF16).ap()
    KSs = dscr("KSs", [4, 128, 1152], BF16).ap()
    VSs = dscr("VSs", [4, 128, 9, 128], BF16).ap()
    UVbf = dscr("UVbf", [16384, 2, D], BF16)
    H2scr = dscr("H2scr", [33 * 128, D], BF16)
    modscr = dscr("modscr", [2, 6 * D], F32)
    Gscr = dscr("Gscr", [4, 1280], F32)
    OK_, OV_, OQ_, OYA, OYR, OKS, OVS, OMOD, OG = [Obj(n) for n in
                                                  ("Kscr", "Vscr", "Qscr", "YAscr", "YRscr", "KSs", "VSs", "modscr", "Gscr")]
    Oout = Obj("outputs")

    st = ExitStack()
    with st:
        P = Prog(nc, st)
        op, dma = P.op, P.dma
        sbt = lambda name, shape, dt: T(st.enter_context(nc.sbuf_tensor("sb_" + name, list(shape), dt))[:], name)
        wbuf_t = st.enter_context(nc.sbuf_tensor("wbuf", [128, WB], BF16))
        arena_t = st.enter_context(nc.sbuf_tensor("arena", [128, AR], BF16))
        Owb = Obj("wbuf_weights")
        s0 = T(st.enter_context(nc.psum_tensor("ps0", [128, 512], F32))[:], "ps0")
        s7 = T(st.enter_context(nc.psum_tensor("ps7", [128, 512], F32))[:], "ps7")
        pairs = []
        for nm in "ABC":
            pt = st.enter_context(nc.psum_tensor("pp" + nm, [128, 1024], F32))
            pairs.append((pt, T(pt[:, 0:512], nm + "0"), T(pt[:, 512:1024], nm + "1")))
        (pA, pA0, pA1), (pB, pB0, pB1), (pC, pC0, pC1) = pairs
        for _b in (s0, s7, pA0, pA1, pB0, pB1, pC0, pC1):
            _b.o.excl = True

        identb = sbt("identb", [128, 128], BF16)
        identf = sbt("identf", [128, 128], F32)
        jrev = sbt("jrev", [128, 128], F32)
        onesb = sbt("onesb", [128, 128], BF16)
        zwin = sbt("zwin", [128, 256], F32)
        iota16 = sbt("iota16", [128, 16], F32)
        epsc = sbt("epsc", [128, 1], F32)
        cols = sbt("cols", [128, 2, 6, 8], F32)
        ncols = sbt("ncols", [128, 2, 8], F32)
        AB = sbt("AB", [128, 2, 4, 8], F32)
        ABv = sbt("ABv", [128, 3, 2, 8], F32)
        vsv = sbt("vsv", [128, 3], F32)
        lamc = sbt("lamc", [128, 8], F32)
        sub = sbt("sub", [128, 2], F32)
        G1 = sbt("G1", [128, D], F32)
        G2 = sbt("G2", [128, D], F32)
        FN = sbt("FN", [128, D], F32)
        S = sbt("S", [128, 4, 128], F32)
        Sb = sbt("Sb", [128, 4, 128], BF16)
        Ss = sbt("Ss", [128, 4, 128], F32)
        Ssb = sbt("Ssb", [128, 4, 128], BF16)
        keysT = sbt("keysT", [128, 16, 128], BF16)
        dtab = sbt("dtab", [128, 1296], F32)
        qs_T = sbt("qs_T", [128, 4, 32], BF16)
        yas_T = sbt("yas_T", [128, 4, 32], BF16)
        yrs_T = sbt("yrs_T", [128, 4, 32], BF16)

        A = Carver(arena_t, AR)
        W = Carver(wbuf_t, WB)

        mult, add, sub_ = ALU.mult, ALU.add, ALU.subtract

        def ld(dst, src, key, reads=()):
            dma("sp", lambda e: e.dma_start(out=dst.t if isinstance(dst, T) else dst, in_=src),
                reads=list(reads), writes=[dst.o], key=key)

        def ld_slow(dst_ap, dst_obj, src, key, reads=()):
            dma("sp", lambda e: e.dma_start(out=dst_ap, in_=src, allow_slow_non_contiguous=True),
                reads=list(reads), writes=[dst_obj], key=key)

        ld(identf, ident_d, "c0")
        ld(jrev, jrev_d, "c1")
        ld(zwin, zwin_d, "c2")
        ld(iota16, iota_d, "c3")
        ld(dtab, dtab_d, "c4")
        ld(vsv, vsv_d, "c5")
        op("pool", lambda e: e.memset(onesb.t[:], 1.0), writes=[onesb.o])
        op("pool", lambda e: e.memset(epsc.t[:], EPS), writes=[epsc.o])
        op("dve", lambda e: e.tensor_copy(out=identb.t[:], in_=identf.t[:]), reads=[identf.o], writes=[identb.o])
        op("pool", lambda e: e.memset(S.t[:], 0.0), writes=[S.o])
        op("pool", lambda e: e.memset(Sb.t[:], 0.0), writes=[Sb.o])

        A.reset()
        cT = A.get([128, 8, 2], F32, "cT")
        for g in range(2):
            ld_slow(cT.t[:, :, g], cT.o, c_both[g].rearrange("(k p) -> p k", p=128), "cT")
        op("act", lambda e: e.activation(out=cT.t[:], in_=cT.t[:], func=AF.Silu), reads=[cT.o], writes=[cT.o])
        wst = [A.get([128, 8, 512], F32, f"wst{i}") for i in range(2)]
        modsb = W.get([2, 6 * D], F32, "modsb")
        badd = W.get([2, 6 * D], F32, "badd")
        ld(badd, bass.AP(b_ada, 0, ((0, 2), (1, 6 * D))), "badd")
        w_ada_v = w_ada.rearrange("(k p) n -> p k n", p=128)
        for cg in range(12):
            ws = wst[cg % 2]
            ld(ws, w_ada_v[:, :, cg * 512:(cg + 1) * 512], f"wst{cg % 2}")
            bank = pA0 if cg % 2 == 0 else pA1
            for k in range(8):
                op("pe", lambda e, k=k, ws=ws, bank=bank: e.matmul(bank.t[0:2, :], lhsT=cT.t[:, k, :], rhs=ws.t[:, k, :],
                                                                   start=(k == 0), stop=(k == 7)),
                   reads=[cT.o, ws.o], writes=[bank.o])
            op("dve", lambda e, cg=cg, bank=bank: e.tensor_tensor(out=modsb.t[:, cg * 512:(cg + 1) * 512], in0=bank.t[0:2, :],
                                                                  in1=badd.t[:, cg * 512:(cg + 1) * 512], op=add),
               reads=[bank.o, badd.o], writes=[modsb.o])
        dma("pool", lambda e: e.dma_start(out=modscr.ap(), in_=modsb.t[:]), reads=[modsb.o], writes=[OMOD], key="modscr")
        for g in range(2):
            for v in range(6):
                src = bass.AP(modscr, g * 6 * D + v * D, ((1, 128), (128, 8)))
                ld_slow(cols.t[:, g, v, :], cols.o, src, "cols", reads=[OMOD])
        ld_slow(ncols.t[:, 0, :], ncols.o, norm1.rearrange("(k p) -> p k", p=128), "ncols")
        ld_slow(ncols.t[:, 1, :], ncols.o, norm2.rearrange("(k p) -> p k", p=128), "ncols")
        for g in range(2):
            for j, (vsc, vsh) in enumerate(((1, 0), (4, 3))):
                op("dve", lambda e, g=g, j=j, vsc=vsc: e.scalar_tensor_tensor(
                    out=AB.t[:, g, 2 * j, :], in0=cols.t[:, g, vsc, :], scalar=1.0, in1=ncols.t[:, j, :], op0=add, op1=mult),
                   reads=[cols.o, ncols.o], writes=[AB.o])
                op("dve", lambda e, g=g, j=j, vsh=vsh: e.tensor_copy(out=AB.t[:, g, 2 * j + 1, :], in_=cols.t[:, g, vsh, :]),
                   reads=[cols.o], writes=[AB.o])
        for gi in range(3):
            for j in range(2):
                op("dve", lambda e, gi=gi, j=j: e.tensor_scalar(out=ABv.t[:, gi, j, :], in0=AB.t[:, 0, j, :],
                                                                scalar1=vsv.t[:, gi:gi + 1], scalar2=None, op0=mult),
                   reads=[AB.o, vsv.o], writes=[ABv.o])

        def load_rows(g):
            ld(G1, bass.AP(modscr, g * 6 * D + 2 * D, ((0, 128), (1, D))), "G1", reads=[OMOD])
            ld(G2, bass.AP(modscr, g * 6 * D + 5 * D, ((0, 128), (1, D))), "G2", reads=[OMOD])

        ld(FN, bass.AP(final_norm, 0, ((0, 128), (1, D))), "FN")
        lq = A.get([128, 4, 64], F32, "lq")
        ld(lq, bass.AP(lamv, 0, ((0, 128), (1, 256))), "lq")
        ljunk = A.get([128, 64], F32, "ljunk")
        for i in range(2):
            op("dve", lambda e, i=i: e.scalar_tensor_tensor(out=ljunk.t[:], in0=lq.t[:, 2 * i, :], scalar=1.0, in1=lq.t[:, 2 * i + 1, :],
                                                            op0=mult, op1=mult, accum_out=lamc.t[:, i:i + 1]),
               reads=[lq.o], writes=[ljunk.o, lamc.o])
        op("act", lambda e: e.activation(out=lamc.t[:, 2:4], in_=lamc.t[:, 0:2], func=AF.Exp), reads=[lamc.o], writes=[lamc.o])
        op("dve", lambda e: e.tensor_tensor(out=lamc.t[:, 4:5], in0=lamc.t[:, 3:4], in1=lamc.t[:, 2:3], op=sub_),
           reads=[lamc.o], writes=[lamc.o])
        op("dve", lambda e: e.tensor_scalar(out=lamc.t[:, 5:6], in0=lamc.t[:, 4:5], scalar1=-LAM_INIT, scalar2=None, op0=add),
           reads=[lamc.o], writes=[lamc.o])
        nlam = lamc.t[:, 5:6]
        ld_slow(sub.t[:, :], sub.o, subln.rearrange("a p -> p a"), "sub")
        op("dve", lambda e: e.tensor_scalar(out=sub.t[:, 0:1], in0=sub.t[:, 0:1], scalar1=1.0 - LAM_INIT, scalar2=None, op0=mult),
           reads=[sub.o], writes=[sub.o])
        rb = A.get([32, 4], F32, "rb")
        rb15 = A.get([32, 4], F32, "rb15")
        oh = A.get([32, 1280], F32, "oh")
        gsb = A.get([4, 1280], F32, "gsb")
        ld(rb, rel_bias.ap(), "rb")
        ld(rb15, bass.AP(rel_bias, 15 * 4, ((0, 32), (1, 4))), "rb15")
        ld(oh, oht5, "oh")
        op("dve", lambda e: e.tensor_tensor(out=rb.t[:], in0=rb.t[:], in1=rb15.t[:], op=sub_), reads=[rb.o, rb15.o], writes=[rb.o])
        for i, (a, b) in enumerate(((0, 512), (512, 1024), (1024, 1280))):
            op("pe", lambda e, a=a, b=b: e.matmul(pB0.t[0:4, 0:b - a], lhsT=rb.t[:, :], rhs=oh.t[:, a:b], start=True, stop=True),
               reads=[rb.o, oh.o], writes=[pB0.o])
            op("act", lambda e, a=a, b=b: e.copy(out=gsb.t[:, a:b], in_=pB0.t[0:4, 0:b - a]), reads=[pB0.o], writes=[gsb.o])
        dma("pool", lambda e: e.dma_start(out=Gscr.ap(), in_=gsb.t[:]), reads=[gsb.o], writes=[OG], key="gscr")
        kst = W.get([128, 16, 128], F32, "kst")
        ksb = W.get([128, 16, 128], BF16, "ksb")
        ld(kst, peer_keys.rearrange("c n d -> n c d"), "kst")
        op("pool", lambda e: e.tensor_copy(out=ksb.t[:], in_=kst.t[:]), reads=[kst.o], writes=[ksb.o])
        s0b = s0.t[:].bitcast(BF16)
        for c4 in range(4):
            for j in range(4):
                c = c4 * 4 + j
                op("pe", lambda e, c=c, j=j: e.transpose(out=s0b[:, j * 128:(j + 1) * 128], in_=ksb.t[:, c, :], identity=identb.t[:]),
                   reads=[ksb.o, identb.o], writes=[s0.o])
            op("dve", lambda e, c4=c4: e.tensor_copy(out=keysT.t[:, c4 * 4:(c4 + 1) * 4, :],
                                                     in_=s0b[:, 0:512].rearrange("p (a b) -> p a b", a=4)),
               reads=[s0.o], writes=[keysT.o])
        cvi = [W.get([128, 2, D], F32, f"cvi{i}") for i in range(3)]
        cvo = [W.get([128, 2, D], BF16, f"cvo{i}") for i in range(3)]
        ci_ = 0
        for ti_, tab in enumerate((peer_u, peer_v)):
            tv = tab.ap().rearrange("(p r) d -> p r d", p=128)
            dv = UVbf.ap().rearrange("(p r) t d -> p r t d", p=128)[:, :, ti_, :]
            for rr in range(0, 128, 2):
                a_, b_ = cvi[ci_ % 3], cvo[ci_ % 3]
                ld(a_, tv[:, rr:rr + 2, :], a_.o.name)
                eng = ("dve", "act", "pool")[ci_ % 3]
                if eng == "act":
                    op("act", lambda e, a_=a_, b_=b_: e.copy(out=b_.t[:], in_=a_.t[:]), reads=[a_.o], writes=[b_.o])
                else:
                    op(eng, lambda e, a_=a_, b_=b_: e.tensor_copy(out=b_.t[:], in_=a_.t[:]), reads=[a_.o], writes=[b_.o])
                dma("pool" if ci_ % 2 == 0 else "sp", lambda e, b_=b_, dv=dv, rr=rr: e.dma_start(out=dv[:, rr:rr + 2, :], in_=b_.t[:]),
                    reads=[b_.o], key=b_.o.name)
                ci_ += 1
        P.barrier()
        if STOP == "p0":
            P.wait_all_dma("sp")
            P.emit()
            return nc

        def load_weights(specs, stage):
            for i, (dst, src, K, n) in enumerate(specs):
                sg = stage[i % len(stage)]
                dma("sp", lambda e, sg=sg, src=src, K=K, n=n: e.dma_start(out=sg.t[:, 0:K, 0:n], in_=src),
                    writes=[sg.o], key=sg.o.name)
                eng = "pool" if i % 2 == 0 else "dve"
                op(eng, lambda e, sg=sg, dst=dst, K=K, n=n: e.tensor_copy(out=dst, in_=sg.t[:, 0:K, 0:n]),
                   reads=[sg.o], writes=[Owb])

        def wspecs(dst3, src2, K, ncol, src_cols=None, piece=256):
            out = []
            sv = src2.rearrange("(k p) n -> p k n", p=128)
            for a in range(0, ncol, piece):
                n = min(piece, ncol - a)
                out.append((dst3[:, :, a:a + n], sv[:, :, src_cols + a:src_cols + a + n], K, n))
            return out

        A.reset()
        W.reset()
        Wall = W.get([128, 8, 2560], BF16, "Wall").t
        Wown = W.get([128, 8, 2048], BF16, "Wown").t
        stage = [A.get([128, 8, 256], F32, f"stg{i}") for i in range(2)]
        specs = []
        specs += wspecs(Wall[:, :, 0:512], w_in, 8, 512, KA)
        specs += wspecs(Wall[:, :, 512:1024], w_in, 8, 512, KR)
        specs += wspecs(Wall[:, :, 1536:2048], w_in, 8, 512, VA)
        specs += wspecs(Wall[:, :, 2048:2560], w_in, 8, 512, VR)
        specs += wspecs(Wown[:, :, 0:512], w_in, 8, 512, QA)
        specs += wspecs(Wown[:, :, 512:1024], w_in, 8, 512, QR)
        specs += wspecs(Wown[:, :, 1536:2048], w_in, 8, 512, GR)
        for h in range(4):
            specs += wspecs(Wall[:, :, 1024 + h * 128:1024 + h * 128 + 64], w_in, 8, 64, KR + h * 128 + 64)
            specs += wspecs(Wall[:, :, 1024 + h * 128 + 64:1024 + h * 128 + 128], w_in, 8, 64, KR + h * 128)
            specs += wspecs(Wown[:, :, 1024 + h * 128:1024 + h * 128 + 64], w_in, 8, 64, QR + h * 128 + 64)
            specs += wspecs(Wown[:, :, 1024 + h * 128 + 64:1024 + h * 128 + 128], w_in, 8, 64, QR + h * 128)
        load_weights(specs, stage)

        xs = [A.get([128, D], F32, f"xs{i}") for i in range(2)]
        F1 = A.get([128, D], F32, "F1")
        xn = A.get([128, D], BF16, "xn")
        hTs = [A.get([128, 8, 128], BF16, f"hT{i}") for i in range(2)]
        ss = A.get([128, 4], F32, "ss")
        rots = [A.get([128, 256], F32, f"rot{i}") for i in range(2)]
        kaT = [A.get([128, 4, 128], BF16, f"kaT{i}") for i in range(2)]
        vab = [A.get([128, 512], BF16, f"vab{i}") for i in range(2)]
        kout = [A.get([128, 512], F32, f"kout{i}") for i in range(2)]
        vout = [A.get([128, 512], F32, f"vout{i}") for i in range(2)]
        t1 = A.get([128, 4, 128], F32, "t1")
        t2 = A.get([128, 4, 128], F32, "t2")
        krot = A.get([128, 4, 128], BF16, "krot")
        krz = A.get([128, 4, 128], BF16, "krz")
        vrb = A.get([128, 512], BF16, "vrb")
        qb = [A.get([128, 4, 128], BF16, f"qb{i}") for i in range(2)]
        qrf = A.get([128, 4, 128], F32, "qrf")
        qrb = A.get([128, 4, 128], BF16, "qrb")
        qxb = A.get([128, 4, 128], BF16, "qxb")
        PTr = A.get([128, 4, 128], BF16, "PTr")
        orr = A.get([128, 4, 128], F32, "orr")
        sq = A.get([128, 512], BF16, "sq")
        sd = A.get([128, 512], F32, "sd")
        gsl = A.get([128, 4, 128], F32, "gsl")
        yrb = [A.get([128, 4, 128], BF16, f"yrb{i}") for i in range(2)]
        ckf = A.get([128, 512], F32, "ckf")
        ckb = A.get([128, 512], BF16, "ckb")

        def norm_transpose(xt, n, Acol, Bcol, AcolO, hT):
            op("act", lambda e: e.activation(out=F1.t[0:n, :], in_=xt.t[0:n, :], func=AF.Square, accum_out=ss.t[0:n, 0:1]),
               reads=[xt.o], writes=[F1.o, ss.o])
            op("act", lambda e: e.activation(out=ss.t[0:n, 1:2], in_=ss.t[0:n, 0:1], func=AF.Sqrt, scale=1.0 / D, bias=epsc.t[0:n, :]),
               reads=[ss.o, epsc.o], writes=[ss.o])
            op("dve", lambda e: e.reciprocal(out=ss.t[0:n, 2:3], in_=ss.t[0:n, 1:2]), reads=[ss.o], writes=[ss.o])
            op("dve", lambda e: e.tensor_scalar(out=xn.t[0:n, :], in0=xt.t[0:n, :], scalar1=ss.t[0:n, 2:3], scalar2=None, op0=mult),
               reads=[xt.o, ss.o], writes=[xn.o])
            for c in range(8):
                op("pe", lambda e, c=c: e.transpose(out=s0b[:, c * 128:c * 128 + n], in_=xn.t[0:n, c * 128:(c + 1) * 128],
                                                    identity=identb.t[0:n, 0:n]),
                   reads=[xn.o, identb.o], writes=[s0.o])
            for c in range(8):
                op("dve" if c % 2 == 0 else "act",
                   (lambda e, c=c: e.tensor_scalar(out=hT.t[:, c, 0:n], in0=s0b[:, c * 128:c * 128 + n],
                                                   scalar1=Acol[:, c:c + 1], scalar2=Bcol[:, c:c + 1], op0=mult, op1=add))
                   if c % 2 == 0 else
                   (lambda e, c=c: e.activation(out=hT.t[:, c, 0:n], in_=s0b[:, c * 128:c * 128 + n], func=AF.Identity,
                                                scale=Acol[:, c:c + 1], bias=Bcol[:, c:c + 1])),
                   reads=[s0.o, AcolO], writes=[hT.o])

        def fm_proj(bank, Wt, col0, nchunks, hT, n, reads_extra=()):
            for j in range(nchunks):
                for k in range(8):
                    op("pe", lambda e, j=j, k=k: e.matmul(bank.t[:, j * 128:j * 128 + n],
                                                          lhsT=Wt[:, k, col0 + j * 128:col0 + (j + 1) * 128],
                                                          rhs=hT.t[:, k, 0:n], start=(k == 0), stop=(k == 7)),
                       reads=[hT.o, Owb], writes=[bank.o])

        def tm_proj(bank, Wt, col0, hT, n):
            for k in range(8):
                op("pe", lambda e, k=k: e.matmul(bank.t[0:n, :], lhsT=hT.t[:, k, 0:n], rhs=Wt[:, k, col0:col0 + 512],
                                                 start=(k == 0), stop=(k == 7)),
                   reads=[hT.o, Owb], writes=[bank.o])

        def v4(bank, n):
            return bank.t[:, :].rearrange("p (a b) -> p a b", a=4)[:, :, 0:n]

        def rotate(bx, bxs, rt, n, out_f32):
            Cb = bcast(rt.t[:, 0:n], [('b', 4), ('x',)])
            Sb_ = bcast(rt.t[:, 128:128 + n], [('b', 4), ('x',)])
            op("dve", lambda e: e.tensor_tensor(out=t1.t[:, :, 0:n], in0=v4(bx, n), in1=Cb, op=mult),
               reads=[bx.o, rt.o], writes=[t1.o])
            op("dve", lambda e: e.tensor_tensor(out=t2.t[:, :, 0:n], in0=v4(bxs, n), in1=Sb_, op=mult),
               reads=[bxs.o, rt.o], writes=[t2.o])
            op("pool", lambda e: e.tensor_tensor(out=out_f32.t[:, :, 0:n], in0=t1.t[:, :, 0:n], in1=t2.t[:, :, 0:n], op=add),
               reads=[t1.o, t2.o], writes=[out_f32.o])

        def p1_tile(s, n, own_idx, sample):
            slot = s % 2
            xt = xs[slot]
            hT = hTs[slot]
            rt = rots[slot]
            g = 1 if sample else 0
            if sample:
                dma("sp", lambda e: e.dma_start(out=xt.t[0:32, :], in_=x_s), writes=[xt.o], key=f"xs{slot}")
            else:
                ld(xt, x_all[s * 128:(s + 1) * 128, :], f"xs{slot}")
            ld(rt, rot[NT if sample else s], f"rot{slot}")
            if (not sample) and s < 12:
                Acol, Bcol, AO = ABv.t[:, s // 4, 0, :], ABv.t[:, s // 4, 1, :], ABv.o
            else:
                Acol, Bcol, AO = AB.t[:, g, 0, :], AB.t[:, g, 1, :], AB.o
            norm_transpose(xt, n, Acol, Bcol, AO, hT)
            St, Sbt = (Ss, Ssb) if sample else (S, Sb)
            dto = 1032 if sample else 0
            xio = 1160 if sample else 512
            zeo = 1288 if sample else 1024
            dtw = 32 if sample else 128
            fm_proj(pA0, Wall, 0, 4, hT, n)
            kt = kaT[slot]
            op("act", lambda e: e.copy(out=kt.t[:, :, 0:n], in_=v4(pA0, n)), reads=[pA0.o], writes=[kt.o])
            if sample:
                dma("pool", lambda e: e.dma_start(out=KSs[:, :, 1024:1056].rearrange("h p t -> p h t"), in_=kt.t[:, :, 0:32]),
                    reads=[kt.o], key=f"kaT{slot}")
            else:
                dma("pool", lambda e: e.dma_start(out=Kscr[:, :, s * 128:(s + 1) * 128].rearrange("h p t -> p h t"), in_=kt.t[:]),
                    reads=[kt.o], key=f"kaT{slot}")
            tm_proj(pB1, Wall, 1536, hT, n)
            vb_ = vab[slot]
            op("act", lambda e: e.copy(out=vb_.t[0:n, :], in_=pB1.t[0:n, :]), reads=[pB1.o], writes=[vb_.o])
            if sample:
                dma("pool", lambda e: e.dma_start(out=VSs[:, 0:32, 8, :].rearrange("h p d -> p h d"),
                                                  in_=vb_.t[0:32, :].rearrange("p (h d) -> p h d", h=4)),
                    reads=[vb_.o], key=f"vab{slot}")
            else:
                dma("pool", lambda e: e.dma_start(out=Vscr[:, :, s, :].rearrange("h p d -> p h d"),
                                                  in_=vb_.t[:, :].rearrange("p (h d) -> p h d", h=4)),
                    reads=[vb_.o], key=f"vab{slot}")
            if own_idx is not None:
                vo = vout[own_idx % 2]
                op("dve", lambda e: e.tensor_copy(out=vo.t[0:n, :], in_=pB1.t[0:n, :]), reads=[pB1.o], writes=[vo.o])
                dst = v_s if sample else v_own[own_idx * 128:(own_idx + 1) * 128, :]
                dma("pool", lambda e, dst=dst: e.dma_start(out=dst, in_=vo.t[0:n, :]), reads=[vo.o], key=f"vout{own_idx % 2}")
                tm_proj(pC1, Wall, 0, hT, n)
                ko = kout[own_idx % 2]
                op("act", lambda e: e.copy(out=ko.t[0:n, :], in_=pC1.t[0:n, :]), reads=[pC1.o], writes=[ko.o])
                dst = k_s if sample else k_own[own_idx * 128:(own_idx + 1) * 128, :]
                dma("pool", lambda e, dst=dst: e.dma_start(out=dst, in_=ko.t[0:n, :]), reads=[ko.o], key=f"kout{own_idx % 2}")
            fm_proj(pA1, Wall, 512, 4, hT, n)
            fm_proj(pB0, Wall, 1024, 4, hT, n)
            rotate(pA1, pB0, rt, n, qrf)
            op("act", lambda e: e.copy(out=krot.t[:, :, 0:n], in_=qrf.t[:, :, 0:n]), reads=[qrf.o], writes=[krot.o])
            for h in range(4):
                op("pe", lambda e, h=h: e.transpose(out=s0b[0:n, h * 128:(h + 1) * 128], in_=krot.t[:, h, 0:n], identity=identb.t[:]),
                   reads=[krot.o, identb.o], writes=[s0.o])
            for h in range(4):
                op("dve", lambda e, h=h: e.tensor_scalar(out=krz.t[0:n, h, :], in0=s0b[0:n, h * 128:(h + 1) * 128],
                                                         scalar1=dtab.t[0:n, zeo + h:zeo + h + 1], scalar2=None, op0=mult),
                   reads=[s0.o, dtab.o], writes=[krz.o])
            tm_proj(pC0, Wall, 2048, hT, n)
            op("act", lambda e: e.copy(out=vrb.t[0:n, :], in_=pC0.t[0:n, :]), reads=[pC0.o], writes=[vrb.o])
            if own_idx is not None and int(os.environ.get("MK_CUT", "99")) >= 1:
                fm_proj(pA0, Wown, 0, 4, hT, n)
                q_ = qb[own_idx % 2]
                if sample:
                    op("act", lambda e: e.activation(out=qs_T.t[:, :, :], in_=v4(pA0, 32), func=AF.Copy, scale=0.125),
                       reads=[pA0.o], writes=[qs_T.o])
                else:
                    op("act", lambda e: e.activation(out=q_.t[:], in_=v4(pA0, 128), func=AF.Copy, scale=0.125),
                       reads=[pA0.o], writes=[q_.o])
                    dma("pool", lambda e: e.dma_start(out=Qscr[:, :, own_idx * 128:(own_idx + 1) * 128].rearrange("h p t -> p h t"),
                                                      in_=q_.t[:]), reads=[q_.o], key=f"qb{own_idx % 2}")
                if int(os.environ.get("MK_CUT", "99")) >= 2:
                    fm_proj(pA1, Wown, 512, 4, hT, n)
                    fm_proj(pB0, Wown, 1024, 4, hT, n)
                    rotate(pA1, pB0, rt, n, qrf)
                    op("act", lambda e: e.copy(out=qrb.t[:, :, 0:n], in_=qrf.t[:, :, 0:n]), reads=[qrf.o], writes=[qrb.o])
                    XIv = dtab.t[:, xio:xio + 4 * dtw].rearrange("p (a b) -> p a b", a=4)
                    op("pool", lambda e: e.tensor_tensor(out=qxb.t[:, :, 0:n], in0=qrf.t[:, :, 0:n], in1=XIv[:, :, 0:n], op=mult),
                       reads=[qrf.o, dtab.o], writes=[qxb.o])
                    for h in range(4):
                        op("pe", lambda e, h=h: e.matmul(pC1.t[0:n, h * 128:h * 128 + n], lhsT=krot.t[:, h, 0:n], rhs=qrb.t[:, h, 0:n],
                                                         start=True, stop=True), reads=[krot.o, qrb.o], writes=[pC1.o])
                    DTv = dtab.t[:, dto:dto + 4 * dtw].rearrange("p (a b) -> p a b", a=4)
                    op("dve", lambda e: e.tensor_tensor(out=PTr.t[0:n, :, 0:n], in0=v4(pC1, n)[0:n], in1=DTv[0:n, :, 0:n], op=mult),
                       reads=[pC1.o, dtab.o], writes=[PTr.o])
                    for h in range(4):
                        op("pe", lambda e, h=h: e.matmul(s7.t[:, h * 128:h * 128 + n], lhsT=vrb.t[0:n, h * 128:(h + 1) * 128],
                                                         rhs=PTr.t[0:n, h, 0:n], start=True, stop=False),
                           reads=[vrb.o, PTr.o], writes=[s7.o])
                        op("pe", lambda e, h=h: e.matmul(s7.t[:, h * 128:h * 128 + n], lhsT=Sbt.t[:, h, :],
                                                         rhs=qxb.t[:, h, 0:n], start=False, stop=True),
                           reads=[Sbt.o, qxb.o], writes=[s7.o])
                    op("act", lambda e: e.copy(out=orr.t[:, :, 0:n], in_=v4(s7, n)), reads=[s7.o], writes=[orr.o])
                    op("act", lambda e: e.activation(out=sq.t[:, 0:512].rearrange("p (a b) -> p a b", a=4)[:, :, 0:n],
                                                     in_=orr.t[:, :, 0:n], func=AF.Square), reads=[orr.o], writes=[sq.o])
                    for h in range(4):
                        op("pe", lambda e, h=h: e.matmul(pC1.t[:, h * 128:h * 128 + n], lhsT=onesb.t[:, :],
                                                         rhs=sq.t[:, h * 128:h * 128 + n], start=True, stop=True),
                           reads=[onesb.o, sq.o], writes=[pC1.o])
                    sdv = sd.t[:, :].rearrange("p (a b) -> p a b", a=4)[:, :, 0:n]
                    op("act", lambda e: e.activation(out=sdv, in_=v4(pC1, n), func=AF.Sqrt, scale=1.0 / 128, bias=epsc.t[:, :]),
                       reads=[pC1.o, epsc.o], writes=[sd.o])
                    op("dve", lambda e: e.reciprocal(out=sdv, in_=sdv), reads=[sd.o], writes=[sd.o])
                    op("dve", lambda e: e.scalar_tensor_tensor(out=orr.t[:, :, 0:n], in0=orr.t[:, :, 0:n], scalar=sub.t[:, 1:2],
                                                               in1=sdv, op0=mult, op1=mult),
                       reads=[orr.o, sub.o, sd.o], writes=[orr.o])
                    fm_proj(pA0, Wown, 1536, 4, hT, n)
                    op("act", lambda e: e.activation(out=gsl.t[:, :, 0:n], in_=v4(pA0, n), func=AF.Silu), reads=[pA0.o], writes=[gsl.o])
                    if sample:
                        op("pool", lambda e: e.tensor_tensor(out=yrs_T.t[:, :, :], in0=orr.t[:, :, 0:32], in1=gsl.t[:, :, 0:32], op=mult),
                           reads=[orr.o, gsl.o], writes=[yrs_T.o])
                    else:
                        y_ = yrb[own_idx % 2]
                        op("pool", lambda e: e.tensor_tensor(out=y_.t[:], in0=orr.t[:], in1=gsl.t[:], op=mult),
                           reads=[orr.o, gsl.o], writes=[y_.o])
                        dma("pool", lambda e: e.dma_start(out=YRscr[:, :, own_idx * 128:(own_idx + 1) * 128].rearrange("h p t -> p h t"),
                                                          in_=y_.t[:]), reads=[y_.o], key=f"yrb{own_idx % 2}")
            L = 32 if sample else 128
            for h in range(4):
                op("pe", lambda e, h=h: e.matmul(s7.t[:, h * 128:(h + 1) * 128], lhsT=krz.t[0:n, h, :],
                                                 rhs=vrb.t[0:n, h * 128:(h + 1) * 128], start=True, stop=True),
                   reads=[krz.o, vrb.o], writes=[s7.o])
            for h in range(4):
                op("dve", lambda e, h=h: e.scalar_tensor_tensor(out=St.t[:, h, :], in0=St.t[:, h, :], scalar=float(GAMMAS[h] ** L),
                                                                in1=s7.t[:, h * 128:(h + 1) * 128], op0=mult, op1=add),
                   reads=[St.o, s7.o], writes=[St.o])
            op("pool", lambda e: e.tensor_copy(out=Sbt.t[:], in_=St.t[:]), reads=[St.o], writes=[Sbt.o])

        _tiles = [int(v) for v in os.environ["MK_TILES"].split(",")] if os.environ.get("MK_TILES") else list(range(NT))
        for s in _tiles:
            p1_tile(s, 128, OWN.index(s) if s in OWN else None, False)
        dma("pool", lambda e: e.dma_start(out=ret_p.rearrange("h k v -> k h v"), in_=S.t[:]), reads=[S.o], key="retp")

        if os.environ.get("MK_NOSAMPLE"):
            P.barrier()
            P.wait_all_dma("sp")
            P.emit()
            return nc
        ld(Ss, state_in.rearrange("h k v -> k h v"), "Ss")
        op("pool", lambda e: e.tensor_copy(out=Ssb.t[:], in_=Ss.t[:]), reads=[Ss.o], writes=[Ssb.o])
        for blk in range(8):
            ld(ckf, cache_k[blk * 128:(blk + 1) * 128, :], "ckf")
            op("pool", lambda e: e.tensor_copy(out=ckb.t[:], in_=ckf.t[:]), reads=[ckf.o], writes=[ckb.o])
            for h in range(4):
                op("pe", lambda e, h=h: e.transpose(out=s0b[:, h * 128:(h + 1) * 128], in_=ckb.t[:, h * 128:(h + 1) * 128],
                                                    identity=identb.t[:]), reads=[ckb.o, identb.o], writes=[s0.o])
            kt = kaT[blk % 2]
            op("act", lambda e, kt=kt: e.copy(out=kt.t[:], in_=s0b[:, 0:512].rearrange("p (a b) -> p a b", a=4)),
               reads=[s0.o], writes=[kt.o])
            dma("pool", lambda e, kt=kt, blk=blk: e.dma_start(out=KSs[:, :, blk * 128:(blk + 1) * 128].rearrange("h p t -> p h t"),
                                                             in_=kt.t[:]), reads=[kt.o], key=f"kaT{blk % 2}")
            ld(ckf, cache_v[blk * 128:(blk + 1) * 128, :], "ckf")
            vb_ = vab[blk % 2]
            op("pool", lambda e, vb_=vb_: e.tensor_copy(out=vb_.t[:], in_=ckf.t[:]), reads=[ckf.o], writes=[vb_.o])
            dma("pool", lambda e, vb_=vb_, blk=blk: e.dma_start(out=VSs[:, :, blk, :].rearrange("h p d -> p h d"),
                                                               in_=vb_.t[:, :].rearrange("p (h d) -> p h d", h=4)),
                reads=[vb_.o], key=f"vab{blk % 2}")
        p1_tile(NT, 32, 32, True)
        dma("pool", lambda e: e.dma_start(out=ret_s.rearrange("h k v -> k h v"), in_=Ss.t[:]), reads=[Ss.o], key="rets")
        P.barrier()
        if STOP == "p1":
            P.wait_all_dma("sp")
            P.emit()
            return nc

        W.reset()
        Kt = [W.get([128, 1024], BF16, f"Kt{i}") for i in range(2)]
        Vc = [W.get([128, 8, 128], BF16, f"Vc{i}") for i in range(2)]
        Qt = [W.get([128, 512], BF16, f"Qt{i}") for i in range(2)]
        PT = [W.get([128, 512], BF16, f"PT{i}") for i in range(6)]
        sacc = [W.get([128, 512], F32, f"sacc{i}") for i in range(2)]
        onesf = W.get([128, 128], F32, "onesf")
        shi = W.get([128, 512], BF16, "shi")
        slo = W.get([128, 512], BF16, "slo")
        tmpf = [W.get([128, 512], F32, f"tmpf{i}") for i in range(2)]
        BT = [W.get([128, 512], F32, f"BT{i}") for i in range(5)]
        Hk = W.get([128, 512], F32, "Hk")
        mcol = W.get([128, 1024], F32, "mcol")
        fR = W.get([128, 512], F32, "fR")
        fo0 = W.get([128, 512], F32, "fo0")
        fo1 = W.get([128, 512], F32, "fo1")
        foa = W.get([128, 512], F32, "foa")
        fsd = W.get([128, 512], F32, "fsd")
        fsq = W.get([128, 512], BF16, "fsq")
        yab = [W.get([128, 512], BF16, f"yab{i}") for i in range(2)]
        zc = W.get([128, 1], F32, "zc")
        ld(mcol, mcol_d, "mcol")
        op("pool", lambda e: e.memset(zc.t[:], 0.0), writes=[zc.o])
        op("pool", lambda e: e.memset(onesf.t[:], 1.0), writes=[onesf.o])
        STb = [s0, s7, pC0, pC1]
        cnt = {"st": 0, "pt": 0, "ch": 0, "q": 0, "tf": 0, "ya": 0}

        def attend(h, Qap, Qo, nq, blocks, out_cb):
            accO = [pA0, pB0]
            accL = [pA1, pB1]
            nb = len(blocks)
            pend = {}

            def stage1(bi):
                b = blocks[bi]
                nk = b["nk"]
                if b.get("pre") is not None:
                    b["pre"]()
                Kap, Ko = b["K"]
                stbs, pts = [], []
                for m in range(2):
                    stb = STb[cnt["st"] % 4]; cnt["st"] += 1
                    stbs.append(stb)
                    op("pe", lambda e, m=m, stb=stb: e.matmul(stb.t[0:nk, 0:nq], lhsT=Kap[m * 64:(m + 1) * 64, :],
                                                              rhs=Qap[m * 64:(m + 1) * 64, :], start=True, stop=True),
                       reads=[Ko, Qo], writes=[stb.o])
                for m in range(2):
                    stb = stbs[m]
                    pt = PT[cnt["pt"] % 6]; cnt["pt"] += 1
                    pts.append(pt)
                    if b["bias"] is not None:
                        bap, bo = b["bias"]
                        tf = tmpf[cnt["tf"] % 2]; cnt["tf"] += 1
                        op("dve", lambda e, tf=tf, stb=stb: e.tensor_tensor(out=tf.t[0:nk, 0:nq], in0=stb.t[0:nk, 0:nq], in1=bap, op=add),
                           reads=[stb.o, bo], writes=[tf.o])
                        src, so = tf.t[0:nk, 0:nq], tf.o
                    else:
                        src, so = stb.t[0:nk, 0:nq], stb.o
                    op("act", lambda e, pt=pt, src=src: e.activation(out=pt.t[0:nk, 0:nq], in_=src, func=AF.Exp, bias=b["mc"][0:nk, :]),
                       reads=[so, mcol.o, zc.o], writes=[pt.o])
                    for (r0, r1, c0, c1) in b["zero"]:
                        op("pool", lambda e, pt=pt, r0=r0, r1=r1, c0=c0, c1=c1: e.memset(pt.t[r0:r1, c0:c1], 0.0), writes=[pt.o])
                pend[bi] = pts

            def stage2(bi):
                b = blocks[bi]
                nk = b["nk"]
                pts = pend.pop(bi)
                Vap, Vo = b["V"]
                for m in range(2):
                    pt = pts[m]
                    O_ = accO[m]
                    op("pe", lambda e, pt=pt, O_=O_: e.matmul(O_.t[:, 0:nq], lhsT=Vap, rhs=pt.t[0:nk, 0:nq], start=(bi == 0), stop=(bi == nb - 1)),
                       reads=[Vo, pt.o], writes=[O_.o])
                for m in range(2):
                    pt = pts[m]
                    sa = sacc[m]
                    if bi == 0:
                        op("dve", lambda e, pt=pt, sa=sa: e.tensor_copy(out=sa.t[0:nk, 0:nq], in_=pt.t[0:nk, 0:nq]), reads=[pt.o], writes=[sa.o])
                    else:
                        op("dve", lambda e, pt=pt, sa=sa: e.tensor_tensor(out=sa.t[0:nk, 0:nq], in0=sa.t[0:nk, 0:nq], in1=pt.t[0:nk, 0:nq], op=add),
                           reads=[pt.o, sa.o], writes=[sa.o])

            for bi in range(nb + 1):
                if bi < nb:
                    stage1(bi)
                if bi >= 1:
                    stage2(bi - 1)
            for m, fo in ((0, fo0), (1, fo1)):
                O_, L_ = accO[m], accL[m]
                sa = sacc[m]
                op("dve", lambda e, sa=sa: e.tensor_copy(out=shi.t[:, 0:nq], in_=sa.t[:, 0:nq]), reads=[sa.o], writes=[shi.o])
                op("dve", lambda e, sa=sa: e.tensor_tensor(out=slo.t[:, 0:nq], in0=sa.t[:, 0:nq], in1=shi.t[:, 0:nq], op=sub_),
                   reads=[sa.o, shi.o], writes=[slo.o])
                op("pe", lambda e, L_=L_: e.matmul(L_.t[:, 0:nq], lhsT=onesb.t[:, :], rhs=shi.t[:, 0:nq], start=True, stop=False),
                   reads=[onesb.o, shi.o], writes=[L_.o])
                op("pe", lambda e, L_=L_: e.matmul(L_.t[:, 0:nq], lhsT=onesb.t[:, :], rhs=slo.t[:, 0:nq], start=False, stop=True),
                   reads=[onesb.o, slo.o], writes=[L_.o])
                op("dve", lambda e, L_=L_: e.reciprocal(out=fR.t[:, 0:nq], in_=L_.t[:, 0:nq]), reads=[L_.o], writes=[fR.o])
                op("dve", lambda e, O_=O_, fo=fo: e.tensor_tensor(out=fo.t[:, 0:nq], in0=O_.t[:, 0:nq], in1=fR.t[:, 0:nq], op=mult),
                   reads=[O_.o, fR.o], writes=[fo.o])
            op("dve", lambda e: e.scalar_tensor_tensor(out=foa.t[:, 0:nq], in0=fo1.t[:, 0:nq], scalar=nlam, in1=fo0.t[:, 0:nq],
                                                       op0=mult, op1=add), reads=[fo0.o, fo1.o, lamc.o], writes=[foa.o])
            op("act", lambda e: e.activation(out=fsq.t[:, 0:nq], in_=foa.t[:, 0:nq], func=AF.Square), reads=[foa.o], writes=[fsq.o])
            op("pe", lambda e: e.matmul(pC1.t[:, 0:nq], lhsT=onesb.t[:, :], rhs=fsq.t[:, 0:nq], start=True, stop=True),
               reads=[onesb.o, fsq.o], writes=[pC1.o])
            op("act", lambda e: e.activation(out=fsd.t[:, 0:nq], in_=pC1.t[:, 0:nq], func=AF.Sqrt, scale=1.0 / 128, bias=epsc.t[:, :]),
               reads=[pC1.o, epsc.o], writes=[fsd.o])
            op("dve", lambda e: e.reciprocal(out=fsd.t[:, 0:nq], in_=fsd.t[:, 0:nq]), reads=[fsd.o], writes=[fsd.o])
            out_cb()

        for h in range(4):
            for j in range(-1, 4):
                src = bass.AP(Gscr, h * 1280 + (R0 - 127 - 128 * j), ((1, 128), (1, 512)))
                ld(Hk, src, "Hk", reads=[OG])
                op("pe", lambda e: e.matmul(pC1.t[:, :], lhsT=jrev.t[:, :], rhs=Hk.t[:, :], start=True, stop=True),
                   reads=[jrev.o, Hk.o], writes=[pC1.o])
                bt = BT[j + 1]
                op("act", lambda e, bt=bt: e.copy(out=bt.t[:], in_=pC1.t[:, :]), reads=[pC1.o], writes=[bt.o])
            for i in range(8):
                sg = 4 * i + 3
                nblk = 4 * sg + 4
                q_ = Qt[cnt["q"] % 2]; cnt["q"] += 1
                ld(q_, Qscr[h, :, i * 512:(i + 1) * 512], q_.o.name, reads=[OQ_])
                blocks = []
                base = cnt["ch"]
                nch = nblk // 8
                cnt["ch"] += nch

                def issue(ci, h=h, base=base):
                    kc_ = Kt[(base + ci) % 2]; vc_ = Vc[(base + ci) % 2]
                    ld(kc_, Kscr[h, :, ci * 1024:(ci + 1) * 1024], kc_.o.name, reads=[OK_])
                    ld(vc_, Vscr[h, :, ci * 8:(ci + 1) * 8, :], vc_.o.name, reads=[OV_])

                for kb in range(nblk):
                    ci = kb // 8
                    kc = Kt[(base + ci) % 2]; vc = Vc[(base + ci) % 2]
                    kl = kb % 8
                    j = kb - 4 * sg
                    b = dict(K=(kc.t[:, kl * 128:(kl + 1) * 128], kc.o), V=(vc.t[:, kl, :], vc.o), nk=128,
                             bias=None, mc=mcol.t[:, i * 128 + kb:i * 128 + kb + 1], zero=[], pre=None)
                    if kb == 0:
                        b["pre"] = (lambda issue=issue, nch=nch: [issue(c_) for c_ in range(min(2, nch))])
                    elif kl == 0 and ci + 1 < nch:
                        b["pre"] = (lambda issue=issue, ci=ci: issue(ci + 1))
                    if j >= -1:
                        b["bias"] = (BT[j + 1].t[:, :], BT[j + 1].o)
                    if j >= 0:
                        if j > 0:
                            b["zero"].append((0, 128, 0, 128 * j))
                        b["zero"].append((64, 128, 128 * j, 128 * j + 64))
                    blocks.append(b)

                def out_cb(i=i, h=h):
                    ya = yab[cnt["ya"] % 2]; cnt["ya"] += 1
                    op("dve", lambda e: e.scalar_tensor_tensor(out=ya.t[:, :], in0=foa.t[:, :], scalar=sub.t[:, 0:1], in1=fsd.t[:, :],
                                                               op0=mult, op1=mult), reads=[foa.o, sub.o, fsd.o], writes=[ya.o])
                    dma("pool", lambda e: e.dma_start(out=YAscr[h, :, i * 512:(i + 1) * 512], in_=ya.t[:, :]),
                        reads=[ya.o], key=ya.o.name)

                attend(h, q_.t[:, :], q_.o, 512, blocks, out_cb)
            kc = Kt[cnt["ch"] % 2]; vc = Vc[cnt["ch"] % 2]; cnt["ch"] += 1
            ld(kc, KSs[h, :, 0:1024], kc.o.name, reads=[OKS])
            ld(vc, VSs[h, :, 0:8, :], vc.o.name, reads=[OVS])
            kc2 = Kt[cnt["ch"] % 2]; vc2 = Vc[cnt["ch"] % 2]; cnt["ch"] += 1
            dma("sp", lambda e, kc2=kc2, h=h: e.dma_start(out=kc2.t[:, 0:32], in_=KSs[h, :, 1024:1056]), reads=[OKS], writes=[kc2.o],
                key=kc2.o.name)
            dma("sp", lambda e, vc2=vc2, h=h: e.dma_start(out=vc2.t[0:32, 0, :], in_=VSs[h, 0:32, 8, :]), reads=[OVS], writes=[vc2.o],
                key=vc2.o.name)
            blocks = []
            for kb in range(8):
                b = dict(K=(kc.t[:, kb * 128:(kb + 1) * 128], kc.o), V=(vc.t[:, kb, :], vc.o), nk=128, bias=None, mc=zc.t[:, 0:1], zero=[])
                if kb == 7:
                    b["bias"] = (BT[0].t[:, 0:32], BT[0].o)
                blocks.append(b)
            blocks.append(dict(K=(kc2.t[:, 0:32], kc2.o), V=(vc2.t[0:32, 0, :], vc2.o), nk=32,
                               bias=(BT[1].t[0:32, 0:32], BT[1].o), mc=zc.t[:, 0:1], zero=[]))

            def out_cb_s(h=h):
                op("dve", lambda e: e.scalar_tensor_tensor(out=yas_T.t[:, h, :], in0=foa.t[:, 0:32], scalar=sub.t[:, 0:1],
                                                           in1=fsd.t[:, 0:32], op0=mult, op1=mult),
                   reads=[foa.o, sub.o, fsd.o], writes=[yas_T.o])

            attend(h, qs_T.t[:, h, :], qs_T.o, 32, blocks, out_cb_s)
        P.barrier()
        if STOP == "p2":
            P.wait_all_dma("sp")
            P.emit()
            return nc

        A.reset()
        W.reset()
        Wg = W.get([128, 8, 2048], BF16, "Wg").t
        Wo = W.get([128, 8, 1024], BF16, "Wo").t
        Wpq = W.get([128, 8, 2048], BF16, "Wpq").t
        Wba = W.get([128, 4, 1024], BF16, "Wba").t
        Wbr = W.get([128, 4, 1024], BF16, "Wbr").t
        craw = A.get([128, 2, 8, 128], F32, "cand")
        stage3 = []
        for i in range(2):
            sg_ = T(craw.t[:, i], "cand")
            sg_.o = craw.o
            stage3.append(sg_)
        NG = 4
        uvb = [A.get([128, 2 * D], BF16, f"uv{i}") for i in range(NG)]
        acol = [A.get([128, 1], F32, f"acol{i}") for i in range(NG)]
        gcol = [A.get([128, 1], F32, f"gcol{i}") for i in range(NG)]
        specs = []
        specs += wspecs(Wg, w_in, 8, 2048, GA, piece=128)
        specs += wspecs(Wo, w_o, 8, 1024, 0, piece=128)
        specs += wspecs(Wpq, w_pq, 8, 2048, 0, piece=128)
        specs += wspecs(Wba, w_ba, 4, 1024, 0, piece=128)
        specs += wspecs(Wbr, w_br, 4, 1024, 0, piece=128)
        load_weights(specs, stage3)
        xs3 = A.get([128, D], F32, "xs3")
        xnew = A.get([128, D], F32, "xnew")
        F3 = A.get([128, D], F32, "F1b")
        xn3 = A.get([128, D], BF16, "xnb")
        hT3 = A.get([128, 8, 128], BF16, "hT3")
        h2T = A.get([128, 8, 128], BF16, "h2T")
        ss3 = A.get([128, 4], F32, "ss3")
        sga = A.get([128, 8, 128], BF16, "sga")
        sgb = A.get([128, 8, 128], BF16, "sgb")
        yain = A.get([128, 4, 128], BF16, "yain")
        yrin = A.get([128, 4, 128], BF16, "yrin")
        u1 = A.get([128, 4, 128], F32, "u1")
        u2 = A.get([128, 4, 128], F32, "u2")
        yT = A.get([128, 8, 128], BF16, "yT")
        h2tm = A.get([128, D], BF16, "h2tm")
        qT = A.get([128, 16, 128], BF16, "qT")
        wk = A.get([128, 256], F32, "wk")
        mx = A.get([128, 16, 16], F32, "mx")
        ix = A.get([128, 16, 16], U32, "ix")
        ixf = A.get([128, 16, 16], F32, "ixf")
        cand = T(craw.t.rearrange("p a b c -> p (a b c)").rearrange("p (a b c) -> p a b c", a=8, b=16), "cand")
        cand.o = craw.o
        ts_ = A.get([128, 8, 16], F32, "ts")
        pos = A.get([128, 8, 16], U32, "pos")
        piu = A.get([128, 8, 16], U32, "piu")
        pju = A.get([128, 8, 16], U32, "pju")
        pj = A.get([128, 8, 16], F32, "pj")
        pi_ = A.get([128, 8, 16], F32, "pi")
        seli = A.get([128, 8, 16], F32, "seli")
        selj = A.get([128, 8, 16], F32, "selj")
        eidx = A.get([128, 128], F32, "eidx")
        ee = A.get([128, 8, 16], F32, "ee")
        esum = A.get([128, 8], F32, "esum")
        gg = A.get([128, 128], F32, "gg")
        eidxT = A.get([128, 128], I32, "eidxT")
        gT = A.get([128, 128], F32, "gT")
        cm = [A.get([128, 128], BF16, f"cm{i}") for i in range(NG)]
        s0b = s0.t[:].bitcast(BF16)
        load_rows(0)
        NH = 3
        dj = xn3
        hb = [A.get([128, D], BF16, f"hb{i}") for i in range(NH)]
        print("P3 arena used", A.off, "of", AR)
        OH2 = Obj("H2scr")

        def norm_transpose3(xt, n, Acol, Bcol, hT):
            op("act", lambda e: e.activation(out=F3.t[0:n, :], in_=xt.t[0:n, :], func=AF.Square, accum_out=ss3.t[0:n, 0:1]),
               reads=[xt.o], writes=[F3.o, ss3.o])
            op("act", lambda e: e.activation(out=ss3.t[0:n, 1:2], in_=ss3.t[0:n, 0:1], func=AF.Sqrt, scale=1.0 / D, bias=epsc.t[0:n, :]),
               reads=[ss3.o, epsc.o], writes=[ss3.o])
            op("dve", lambda e: e.reciprocal(out=ss3.t[0:n, 2:3], in_=ss3.t[0:n, 1:2]), reads=[ss3.o], writes=[ss3.o])
            op("dve", lambda e: e.tensor_scalar(out=xn3.t[0:n, :], in0=xt.t[0:n, :], scalar1=ss3.t[0:n, 2:3], scalar2=None, op0=mult),
               reads=[xt.o, ss3.o], writes=[xn3.o])
            for c in range(8):
                op("pe", lambda e, c=c: e.transpose(out=s0b[:, c * 128:c * 128 + n], in_=xn3.t[0:n, c * 128:(c + 1) * 128],
                                                    identity=identb.t[0:n, 0:n]), reads=[xn3.o, identb.o], writes=[s0.o])
            for c in range(8):
                op("dve", lambda e, c=c: e.tensor_scalar(out=hT.t[:, c, 0:n], in0=s0b[:, c * 128:c * 128 + n],
                                                         scalar1=Acol[:, c:c + 1], scalar2=Bcol[:, c:c + 1], op0=mult, op1=add),
                   reads=[s0.o, AB.o], writes=[hT.o])

        def top16(src_ap, src_o, n, width, out_v, out_i, vo, io):
            op("dve", lambda e: e.max(out=out_v[:, 0:8], in_=src_ap), reads=[src_o], writes=[vo])
            op("dve", lambda e: e.match_replace(out=wk.t[0:n, 0:width], in_to_replace=out_v[:, 0:8], in_values=src_ap, imm_value=-1e30),
               reads=[src_o, vo], writes=[wk.o])
            op("dve", lambda e: e.max(out=out_v[:, 8:16], in_=wk.t[0:n, 0:width]), reads=[wk.o], writes=[vo])
            op("dve", lambda e: e.max_index(out=out_i[:, 0:8], in_max=out_v[:, 0:8], in_values=src_ap), reads=[src_o, vo], writes=[io])
            op("dve", lambda e: e.max_index(out=out_i[:, 8:16], in_max=out_v[:, 8:16], in_values=wk.t[0:n, 0:width]),
               reads=[wk.o, vo], writes=[io])

        def p3_tile(oi, n, sample):
            g = 1 if sample else 0
            A1c, B1c, A2c, B2c = (AB.t[:, g, k, :] for k in range(4))
            if sample:
                dma("sp", lambda e: e.dma_start(out=xs3.t[0:32, :], in_=x_s), writes=[xs3.o], key="xs3")
                ya_ap, yr_ap, ya_o, yr_o = yas_T.t, yrs_T.t, yas_T.o, yrs_T.o
            else:
                s = OWN[oi]
                ld(xs3, x_all[s * 128:(s + 1) * 128, :], "xs3")
                ld(yain, YAscr[:, :, oi * 128:(oi + 1) * 128].rearrange("h p t -> p h t"), "yain", reads=[OYA])
                ld(yrin, YRscr[:, :, oi * 128:(oi + 1) * 128].rearrange("h p t -> p h t"), "yrin", reads=[OYR])
                ya_ap, yr_ap, ya_o, yr_o = yain.t, yrin.t, yain.o, yrin.o
            norm_transpose3(xs3, n, A1c, B1c, hT3)
            for gi, sgt in ((0, sga), (1, sgb)):
                for half, bank in ((0, pA0), (1, pA1)):
                    fm_proj(bank, Wg, gi * 1024 + half * 512, 4, hT3, n)
                    op("act", lambda e, sgt=sgt, half=half, bank=bank: e.activation(out=sgt.t[:, half * 4:(half + 1) * 4, 0:n], in_=v4(bank, n),
                                                                                    func=AF.Sigmoid), reads=[bank.o], writes=[sgt.o])
            for half, (bA, bR) in ((0, (pB0, pC0)), (1, (pB1, pC1))):
                for j in range(4):
                    jj = half * 4 + j
                    for h in range(4):
                        op("pe", lambda e, j=j, jj=jj, h=h, bA=bA: e.matmul(bA.t[:, j * 128:j * 128 + n], lhsT=Wba[:, h, jj * 128:(jj + 1) * 128],
                                                                             rhs=ya_ap[:, h, 0:n], start=(h == 0), stop=(h == 3)),
                           reads=[Owb, ya_o], writes=[bA.o])
                    for h in range(4):
                        op("pe", lambda e, j=j, jj=jj, h=h, bR=bR: e.matmul(bR.t[:, j * 128:j * 128 + n], lhsT=Wbr[:, h, jj * 128:(jj + 1) * 128],
                                                                             rhs=yr_ap[:, h, 0:n], start=(h == 0), stop=(h == 3)),
                           reads=[Owb, yr_o], writes=[bR.o])
                op("dve", lambda e, half=half, bA=bA: e.tensor_tensor(out=u1.t[:, :, 0:n], in0=v4(bA, n), in1=sga.t[:, half * 4:(half + 1) * 4, 0:n],
                                                                      op=mult), reads=[bA.o, sga.o], writes=[u1.o])
                op("dve", lambda e, half=half, bR=bR: e.tensor_tensor(out=u2.t[:, :, 0:n], in0=v4(bR, n), in1=sgb.t[:, half * 4:(half + 1) * 4, 0:n],
                                                                      op=mult), reads=[bR.o, sgb.o], writes=[u2.o])
                op("pool", lambda e, half=half: e.tensor_tensor(out=yT.t[:, half * 4:(half + 1) * 4, 0:n], in0=u1.t[:, :, 0:n], in1=u2.t[:, :, 0:n],
                                                                op=add), reads=[u1.o, u2.o], writes=[yT.o])
            for half, bank in ((0, pA0), (1, pA1)):
                for k in range(8):
                    op("pe", lambda e, k=k, half=half, bank=bank: e.matmul(bank.t[0:n, :], lhsT=yT.t[:, k, 0:n], rhs=Wo[:, k, half * 512:(half + 1) * 512],
                                                                           start=(k == 0), stop=(k == 7)), reads=[yT.o, Owb], writes=[bank.o])
            op("dve", lambda e: e.tensor_tensor(out=F3.t[0:n, :], in0=pA[0:n, :], in1=G1.t[0:n, :], op=mult),
               reads=[pA0.o, pA1.o, G1.o], writes=[F3.o])
            op("pool", lambda e: e.tensor_tensor(out=xnew.t[0:n, :], in0=F3.t[0:n, :], in1=xs3.t[0:n, :], op=add),
               reads=[F3.o, xs3.o], writes=[xnew.o])
            norm_transpose3(xnew, n, A2c, B2c, h2T)
            for c in range(8):
                op("pe", lambda e, c=c: e.transpose(out=s0b[0:n, c * 128:(c + 1) * 128], in_=h2T.t[:, c, 0:n], identity=identb.t[:, :]),
                   reads=[h2T.o, identb.o], writes=[s0.o])
            op("act", lambda e: e.copy(out=h2tm.t[0:n, :], in_=s0b[0:n, :]), reads=[s0.o], writes=[h2tm.o])
            dma("sp", lambda e: e.dma_start(out=H2scr.ap()[oi * 128:oi * 128 + n, :], in_=h2tm.t[0:n, :]), reads=[h2tm.o], writes=[OH2], key="h2st")
            for c4, bank in enumerate((pA0, pA1, pB0, pB1)):
                fm_proj(bank, Wpq, c4 * 512, 4, h2T, n)
                op("act", lambda e, c4=c4, bank=bank: e.copy(out=qT.t[:, c4 * 4:(c4 + 1) * 4, 0:n], in_=v4(bank, n)), reads=[bank.o], writes=[qT.o])
            banks4 = (pA0, pA1, pB0, pB1)
            for c in range(16):
                bank = banks4[c // 4]
                op("pe", lambda e, c=c, bank=bank: e.matmul(bank.t[0:n, (c % 4) * 128:(c % 4 + 1) * 128], lhsT=qT.t[:, c, 0:n], rhs=keysT.t[:, c, :],
                                                            start=True, stop=True), reads=[qT.o, keysT.o], writes=[bank.o])
            for c in range(16):
                bank = banks4[c // 4]
                top16(bank.t[0:n, (c % 4) * 128:(c % 4 + 1) * 128], bank.o, n, 128, mx.t[0:n, c, :], ix.t[0:n, c, :], mx.o, ix.o)
            op("dve", lambda e: e.tensor_copy(out=ixf.t[0:n], in_=ix.t[0:n]), reads=[ix.o], writes=[ixf.o])

            def mxv(off, pat):
                base = mx.t[0:n, 0, 0:1]
                return bass.AP(base.tensor, base.offset + off, (base.ap[0],) + pat)

            def ixv(off, pat):
                base = ixf.t[0:n, 0, 0:1]
                return bass.AP(base.tensor, base.offset + off, (base.ap[0],) + pat)

            op("dve", lambda e: e.tensor_tensor(out=cand.t[0:n], in0=mxv(0, ((32, 8), (1, 16), (0, 16))), in1=mxv(16, ((32, 8), (0, 16), (1, 16))),
                                                op=add), reads=[mx.o], writes=[cand.o])
            for h in range(8):
                top16(cand.t[0:n, h].rearrange("p a b -> p (a b)"), cand.o, n, 256, ts_.t[0:n, h, :], pos.t[0:n, h, :], ts_.o, pos.o)
            t0 = bass.AP(ts_.t[0:n, 0, 0:1].tensor, ts_.t[0:n, 0, 0:1].offset, (ts_.t[0:n, 0, 0:1].ap[0], (16, 8), (0, 16)))
            op("dve", lambda e: e.tensor_tensor(out=ee.t[0:n], in0=ts_.t[0:n], in1=t0, op=sub_), reads=[ts_.o], writes=[ee.o])
            op("act", lambda e: e.activation(out=ee.t[0:n], in_=ee.t[0:n], func=AF.Exp), reads=[ee.o], writes=[ee.o])
            op("dve", lambda e: e.reduce_sum(out=esum.t[0:n, :], in_=ee.t[0:n], axis=AX.X), reads=[ee.o], writes=[esum.o])
            op("dve", lambda e: e.reciprocal(out=esum.t[0:n, :], in_=esum.t[0:n, :]), reads=[esum.o], writes=[esum.o])
            e0 = bass.AP(esum.t[0:n, 0:1].tensor, esum.t[0:n, 0:1].offset, (esum.t[0:n, 0:1].ap[0], (1, 8), (0, 16)))
            op("dve", lambda e: e.tensor_tensor(out=gg.t[0:n, :].rearrange("p (a b) -> p a b", a=8), in0=ee.t[0:n], in1=e0, op=mult),
               reads=[ee.o, esum.o], writes=[gg.o])
            op("dve", lambda e: e.tensor_scalar(out=piu.t[0:n], in0=pos.t[0:n], scalar1=4, scalar2=None, op0=ALU.logical_shift_right),
               reads=[pos.o], writes=[piu.o])
            op("dve", lambda e: e.tensor_scalar(out=pju.t[0:n], in0=pos.t[0:n], scalar1=15, scalar2=None, op0=ALU.bitwise_and),
               reads=[pos.o], writes=[pju.o])
            op("dve", lambda e: e.tensor_copy(out=pi_.t[0:n], in_=piu.t[0:n]), reads=[piu.o], writes=[pi_.o])
            op("dve", lambda e: e.tensor_copy(out=pj.t[0:n], in_=pju.t[0:n]), reads=[pju.o], writes=[pj.o])
            io_ = iota16.t[0:n, 0:1]
            iotav = bass.AP(io_.tensor, io_.offset, (io_.ap[0], (0, 8), (0, 16), (1, 16)))
            for (pp, off, sel) in ((pi_, 0, seli), (pj, 16, selj)):
                b_ = pp.t[0:n, 0, 0:1]
                pv = bass.AP(b_.tensor, b_.offset, (b_.ap[0], (16, 8), (1, 16), (0, 16)))
                op("dve", lambda e, pv=pv: e.tensor_tensor(out=cand.t[0:n], in0=pv, in1=iotav, op=ALU.is_equal),
                   reads=[pp.o, iota16.o], writes=[cand.o])
                op("dve", lambda e, off=off: e.tensor_tensor(out=cand.t[0:n], in0=cand.t[0:n], in1=ixv(off, ((32, 8), (0, 16), (1, 16))), op=mult),
                   reads=[cand.o, ixf.o], writes=[cand.o])
                op("dve", lambda e, sel=sel: e.reduce_sum(out=sel.t[0:n], in_=cand.t[0:n], axis=AX.X), reads=[cand.o], writes=[sel.o])
            op("dve", lambda e: e.scalar_tensor_tensor(out=eidx.t[0:n, :].rearrange("p (a b) -> p a b", a=8), in0=seli.t[0:n], scalar=128.0,
                                                       in1=selj.t[0:n], op0=mult, op1=add), reads=[seli.o, selj.o], writes=[eidx.o])
            op("pe", lambda e: e.transpose(out=s7.t[:, 0:n], in_=eidx.t[0:n, :], identity=identf.t[0:n, 0:n]), reads=[eidx.o, identf.o], writes=[s7.o])
            op("dve", lambda e: e.tensor_copy(out=eidxT.t[:, 0:n], in_=s7.t[:, 0:n]), reads=[s7.o], writes=[eidxT.o])
            op("pe", lambda e: e.transpose(out=s7.t[:, 128:128 + n], in_=gg.t[0:n, :], identity=identf.t[0:n, 0:n]), reads=[gg.o, identf.o], writes=[s7.o])
            op("act", lambda e: e.copy(out=gT.t[:, 0:n], in_=s7.t[:, 128:128 + n]), reads=[s7.o], writes=[gT.o])
            UVsrc = UVbf.ap().rearrange("e t d -> e (t d)")

            def st_gather(t):
                uv = uvb[t % NG]
                dma("pool", lambda e: e.indirect_dma_start(out=uv.t[:, :], out_offset=None, in_=UVsrc,
                                                           in_offset=bass.IndirectOffsetOnAxis(ap=eidxT.t[:, t:t + 1], axis=0)),
                    reads=[eidxT.o], writes=[uv.o], key=f"{uv.o.name}_{oi % 4}")

            def st_bcast(t):
                hb_ = hb[t % NH]
                src = bass.AP(H2scr, (oi * 128 + t) * D, ((0, 128), (1, D)))
                dma("sp", lambda e: e.dma_start(out=hb_.t[:, :], in_=src), reads=[OH2], writes=[hb_.o], key=f"{hb_.o.name}_{oi % 2}")

            def st_dot(t):
                uv, ac, gc, hb_ = uvb[t % NG], acol[t % NG], gcol[t % NG], hb[t % NH]
                op("dve", lambda e: e.scalar_tensor_tensor(out=dj.t[:, :], in0=uv.t[:, 0:D], scalar=1.0, in1=hb_.t[:, :],
                                                           op0=mult, op1=mult, accum_out=ac.t[:, 0:1]),
                   reads=[uv.o, hb_.o], writes=[dj.o, ac.o])
                op("act", lambda e: e.activation(out=gc.t[:, 0:1], in_=ac.t[:, 0:1], func=AF.Gelu), reads=[ac.o], writes=[gc.o])

            def st_cm(t):
                gc, c_ = gcol[t % NG], cm[t % NG]
                op("pool", lambda e: e.tensor_scalar(out=c_.t[:, 0:n], in0=zwin.t[:, 127 - t:127 - t + n], scalar1=gc.t[:, 0:1],
                                                    scalar2=gT.t[:, t:t + 1], op0=mult, op1=mult),
                   reads=[zwin.o, gc.o, gT.o], writes=[c_.o])

            def st_acc(t):
                uv, c_ = uvb[t % NG], cm[t % NG]
                for half, bank in ((0, pB0), (1, pB1)):
                    op("pe", lambda e, half=half, bank=bank: e.matmul(bank.t[0:n, :], lhsT=c_.t[:, 0:n],
                                                                      rhs=uv.t[:, D + half * 512:D + (half + 1) * 512],
                                                                      start=(t == 0), stop=(t == n - 1)),
                       reads=[c_.o, uv.o], writes=[bank.o])

            for t in range(n + 2):
                if t < n:
                    st_gather(t)
                    st_bcast(t)
                    st_dot(t)
                if 1 <= t <= n:
                    st_cm(t - 1)
                if t >= 2:
                    st_acc(t - 2)
            op("dve", lambda e: e.tensor_tensor(out=F3.t[0:n, :], in0=pB[0:n, :], in1=G2.t[0:n, :], op=mult), reads=[pB0.o, pB1.o, G2.o], writes=[F3.o])
            op("pool", lambda e: e.tensor_tensor(out=xnew.t[0:n, :], in0=F3.t[0:n, :], in1=xnew.t[0:n, :], op=add), reads=[F3.o, xnew.o], writes=[xnew.o])
            op("act", lambda e: e.activation(out=F3.t[0:n, :], in_=xnew.t[0:n, :], func=AF.Square, accum_out=ss3.t[0:n, 0:1]), reads=[xnew.o], writes=[F3.o, ss3.o])
            op("act", lambda e: e.activation(out=ss3.t[0:n, 1:2], in_=ss3.t[0:n, 0:1], func=AF.Sqrt, scale=1.0 / D, bias=epsc.t[0:n, :]),
               reads=[ss3.o, epsc.o], writes=[ss3.o])
            op("dve", lambda e: e.reciprocal(out=ss3.t[0:n, 2:3], in_=ss3.t[0:n, 1:2]), reads=[ss3.o], writes=[ss3.o])
            op("dve", lambda e: e.scalar_tensor_tensor(out=F3.t[0:n, :], in0=xnew.t[0:n, :], scalar=ss3.t[0:n, 2:3], in1=FN.t[0:n, :], op0=mult, op1=mult),
               reads=[xnew.o, ss3.o, FN.o], writes=[F3.o])
            dst = y_s if sample else y_own[oi * 128:(oi + 1) * 128, :]
            dma("sp", lambda e: e.dma_start(out=dst, in_=F3.t[0:n, :]), reads=[F3.o], key="yout")

        for oi in range(32):
            p3_tile(oi, 128, False)
        load_rows(1)
        p3_tile(32, 32, True)
        P.wait_all_dma("sp")
        P.emit()
    return nc


def _t5_bucket(rel):
    nb = 16
    ret = 16 if rel > 0 else 0
    n = abs(rel)
    if n < 8:
        return ret + n
    nf = np.float32(max(n, 8))
    large = 8 + int(np.float32(np.log(nf / np.float32(8)) / np.float32(math.log(16.0)) * np.float32(8)))
    return ret + min(large, 15)


def _static_tables(r):
    shift = 3 - r
    inv = (1.0 / (10000.0 ** np.linspace(0.0, 1.0, 64, dtype=np.float32))).astype(np.float32)
    rot = np.zeros((NT + 1, 128, 256), np.float32)
    pos = (np.arange(NT * 128, dtype=np.int64) - shift * 512).clip(0).astype(np.float32)
    ang = (pos[:, None] * inv[None, :]).astype(np.float32).astype(np.float64)
    cos, sin = np.cos(ang).astype(np.float32), np.sin(ang).astype(np.float32)
    C = np.concatenate([cos, cos], 1).T.reshape(128, NT, 128)
    Sg = np.concatenate([-sin, sin], 1).T.reshape(128, NT, 128)
    rot[:NT, :, 0:128] = C.transpose(1, 0, 2)
    rot[:NT, :, 128:256] = Sg.transpose(1, 0, 2)
    ps = (1024 + np.arange(32)).astype(np.float32)
    angs = (ps[:, None] * inv[None, :]).astype(np.float32).astype(np.float64)
    cs, sn = np.cos(angs).astype(np.float32), np.sin(angs).astype(np.float32)
    rot[NT, :, 0:32] = np.concatenate([cs, cs], 1).T
    rot[NT, :, 128:160] = np.concatenate([-sn, sn], 1).T
    dtab = np.zeros((128, 1296), np.float32)
    sc = 128.0 ** -0.5
    for h in range(4):
        lg = math.log(GAMMAS[h])
        for (L, dto, xio, zeo) in ((128, 0, 512, 1024), (32, 1032, 1160, 1288)):
            n = np.arange(L, dtype=np.float64)
            diff = n[None, :] - n[:, None]
            DT = np.where(diff >= 0, np.exp(np.maximum(diff, 0) * lg), 0.0) * sc
            dtab[0:L, dto + h * L:dto + (h + 1) * L] = DT
            dtab[:, xio + h * L:xio + (h + 1) * L] = np.exp((n + 1.0) * lg)[None, :]
            dtab[0:L, zeo + h] = np.exp((L - 1.0 - n) * lg) * sc
    mcol = np.zeros((128, 1024), np.float32)
    for i in range(8):
        mcol[:, i * 128:i * 128 + 4 * shift] = NEG
    vsv = np.ones((128, 3), np.float32)
    vsv[:, 0:shift] = 0.0
    return rot, dtab, mcol, vsv


def kernel(x_prompt, x_sample, cache_k, cache_v, state_ret, c_prompt, c_sample, w_ada, b_ada, norm1, norm2, w_in,
           lam_q1, lam_k1, lam_q2, lam_k2, subln_a, subln_r, w_ba, w_br, w_o, rel_bias, w_pq, peer_keys, peer_u, peer_v,
           final_norm):
    f = lambda a: np.ascontiguousarray(np.asarray(a, dtype=np.float32))
    x_prompt, x_sample = f(x_prompt), f(x_sample)
    oht5 = np.zeros((32, 1280), np.float32)
    for i in range(1151):
        oht5[_t5_bucket(R0 - i), i] = 1.0
    zwin = np.zeros((128, 256), np.float32)
    zwin[:, 127] = 1.0
    shared = dict(
        w_ada=f(w_ada)[0], b_ada=f(b_ada)[0], norm1=f(norm1)[0], norm2=f(norm2)[0], w_in=f(w_in)[0],
        lamv=np.stack([f(lam_q1)[0], f(lam_k1)[0], f(lam_q2)[0], f(lam_k2)[0]]),
        subln=np.stack([f(subln_a)[0], f(subln_r)[0]]), w_ba=f(w_ba)[0], w_br=f(w_br)[0], w_o=f(w_o)[0],
        rel_bias=f(rel_bias), w_pq=f(w_pq)[0], peer_keys=f(peer_keys)[0].reshape(16, 128, 128),
        peer_u=f(peer_u)[0], peer_v=f(peer_v)[0], final_norm=f(final_norm), oht5=oht5,
        jrev=np.ascontiguousarray(np.eye(128, dtype=np.float32)[::-1]), ident=np.eye(128, dtype=np.float32), zwin=zwin,
        iota16=np.tile(np.arange(16, dtype=np.float32)[None, :], (128, 1)),
    )
    tabs = [_static_tables(r) for r in range(4)]
    in_maps = []
    for c in range(8):
        b, r = c // 4, c % 4
        shift = 3 - r
        xa = np.zeros((NT * 128, D), np.float32)
        xa[shift * 512:] = x_prompt[b, :NT * 128 - shift * 512]
        rot, dtab, mcol, vsv = tabs[r]
        m = dict(shared)
        m.update(x_all=xa, x_s=x_sample[c], cache_k=f(cache_k)[0, c].reshape(1024, 512), cache_v=f(cache_v)[0, c].reshape(1024, 512),
                 state_in=f(state_ret)[0, c], c_both=np.stack([f(c_prompt)[b], f(c_sample)[c]]),
                 rot=rot, dtab=dtab, mcol=mcol, vsv=vsv)
        in_maps.append(m)
    nc = build_nc()
    if STOP:
        return nc, in_maps
    res = run_bass_kernel_spmd(nc, in_maps, core_ids=list(range(8)))
    R = res.results
    y_prompt = np.zeros((2, 16384, D), np.float32)
    k_prompt = np.zeros((1, 2, 16384, 4, 128), np.float32)
    v_prompt = np.zeros((1, 2, 16384, 4, 128), np.float32)
    ret_prompt = np.zeros((1, 2, 4, 128, 128), np.float32)
    y_sample = np.zeros((8, 32, D), np.float32)
    k_sample = np.zeros((1, 8, 32, 4, 128), np.float32)
    v_sample = np.zeros((1, 8, 32, 4, 128), np.float32)
    ret_sample = np.zeros((1, 8, 4, 128, 128), np.float32)
    for c in range(8):
        b, r = c // 4, c % 4
        shift = 3 - r
        for oi, s in enumerate(OWN):
            t0 = s * 128 - shift * 512
            y_prompt[b, t0:t0 + 128] = R[c]["y_own"][oi * 128:(oi + 1) * 128]
            k_prompt[0, b, t0:t0 + 128] = R[c]["k_own"][oi * 128:(oi + 1) * 128].reshape(128, 4, 128)
            v_prompt[0, b, t0:t0 + 128] = R[c]["v_own"][oi * 128:(oi + 1) * 128].reshape(128, 4, 128)
        if r == 3:
            ret_prompt[0, b] = R[c]["ret_p"]
        y_sample[c] = R[c]["y_s"]
        k_sample[0, c] = R[c]["k_s"].reshape(32, 4, 128)
        v_sample[0, c] = R[c]["v_s"].reshape(32, 4, 128)
        ret_sample[0, c] = R[c]["ret_s"]
    return (y_prompt, y_sample, k_prompt, v_prompt, ret_prompt, k_sample, v_sample, ret_sample)
```

```python
import math
from contextlib import ExitStack
from concourse.bass_utils import run_bass_kernel_spmd

import numpy as np
import concourse.bass as bass
import concourse.mybir as mybir

F32 = mybir.dt.float32
BF16 = mybir.dt.bfloat16
I32 = mybir.dt.int32
U32 = mybir.dt.uint32
U16 = mybir.dt.uint16
ALU = mybir.AluOpType
AF = mybir.ActivationFunctionType
AX = mybir.AxisListType

SEM_WINDOW = 12000
SAME_ENGINE_SYNC = True


class Obj:
    __slots__ = ("name", "w", "r", "excl")

    def __init__(self, name):
        self.name = name
        self.w = None
        self.r = {}
        self.excl = False


class Prog:
    ENGS = ("pe", "act", "dve", "pool", "sp")

    def __init__(self, nc, stack):
        self.nc = nc
        self.stack = stack
        self.cnt = {e: 0 for e in self.ENGS}
        self.ops = {e: [] for e in self.ENGS}
        self.waited = {e: {} for e in self.ENGS}
        self.esems = {}
        self.dsems = {}
        self.dcount = {}
        self.nsem = 0
        self.keymap = {}
        self.next_phys = 0

    def _esem(self, eng, k):
        key = (eng, k)
        if key not in self.esems:
            self.esems[key] = self.stack.enter_context(self.nc.semaphore(f"s_{eng}_{k}"))
            self.nsem += 1
        return self.esems[key]

    def _dkey(self, key):
        if key not in self.keymap:
            name = f"D{self.next_phys}"
            self.next_phys += 1
            if name not in self.dsems:
                self.dsems[name] = self.stack.enter_context(self.nc.semaphore(f"d_{name}"))
                self.dcount[name] = 0
                self.nsem += 1
            self.keymap[key] = name
        return self.keymap[key]

    def _deps(self, eng, reads, writes):
        deps = {}

        def add(src, val):
            if val > deps.get(src, 0):
                deps[src] = val

        for o in reads:
            if o.w is not None:
                add(*o.w)
            if o.excl:
                for s, v in o.r.items():
                    if s != eng:
                        add(s, v)
        for o in writes:
            if o.w is not None:
                add(*o.w)
            for s, v in o.r.items():
                add(s, v)
        waits = []
        for src, val in deps.items():
            if src == eng:
                if eng == "pe" or not SAME_ENGINE_SYNC:
                    continue
            if self.waited[eng].get(src, 0) >= val:
                continue
            self.waited[eng][src] = val
            if src in self.ENGS:
                k = (val - 1) // SEM_WINDOW
                waits.append((self._esem(src, k), (val - 1) % SEM_WINDOW + 1))
            else:
                waits.append((self.dsems[src], val))
        return waits

    def _mark(self, ev, reads, writes):
        src, val = ev
        for o in reads:
            if val > o.r.get(src, 0):
                o.r[src] = val
        for o in writes:
            o.w = ev
            o.r = {}

    def op(self, eng, fn, reads=(), writes=()):
        waits = self._deps(eng, reads, writes)
        n = self.cnt[eng] + 1
        self.cnt[eng] = n
        k = (n - 1) // SEM_WINDOW
        sem = self._esem(eng, k)
        self.ops[eng].append((waits, fn, sem, 1))
        self._mark((eng, n), reads, writes)

    def dma(self, q, fn, reads=(), writes=(), key=None):
        waits = self._deps(q, reads, writes)
        name = self._dkey(key)
        sem = self.dsems[name]
        self.dcount[name] += 16
        self.ops[q].append((waits, fn, sem, 16))
        self._mark((name, self.dcount[name]), reads, writes)

    def wait_all_dma(self, q="sp"):
        waits = []
        for key, sem in self.dsems.items():
            if self.dcount[key] > self.waited[q].get(key, 0):
                waits.append((sem, self.dcount[key]))
        self.ops[q].append((waits, None, None, 0))

    def emit(self):
        nc = self.nc
        handles = {"pe": "tensor", "act": "scalar", "dve": "vector", "pool": "gpsimd", "sp": "sync"}
        with nc.Block() as block:
            for eng in self.ENGS:
                lst = self.ops[eng]

                def body(e, lst=lst):
                    for waits, fn, sem, inc in lst:
                        for s, v in waits:
                            e.wait_ge(s, v)
                        if fn is not None:
                            ins = fn(e)
                            ins.then_inc(sem, inc)

                getattr(block, handles[eng])(body)


def _barrier(self):
    snap = [(s, v) for s, v in list(self.cnt.items()) + list(self.dcount.items()) if v > 0]
    for e in self.ENGS:
        waits = []
        for src, val in snap:
            if src == e and e in ("pe", "sp"):
                continue
            if self.waited[e].get(src, 0) >= val:
                continue
            self.waited[e][src] = val
            if src in self.ENGS:
                k = (val - 1) // SEM_WINDOW
                waits.append((self._esem(src, k), (val - 1) % SEM_WINDOW + 1))
            else:
                waits.append((self.dsems[src], val))
        self.ops[e].append((waits, None, None, 0))
    self.keymap = {}
    self.next_phys = 0


Prog.barrier = _barrier


D = 1024
NT = 128
OWN = [16 * i + 12 + j for i in range(8) for j in range(4)]
HA = 4
EPS = 1e-6
QA, KA, VA, QR, KR, VR, GR, GA, GB = 0, 512, 1024, 1536, 2048, 2560, 3072, 3584, 4608
LAM_INIT = 0.8 - 0.6 * math.exp(-0.3 * 0)
GAMMAS = [1.0 - 2.0 ** (-5.0 - h) for h in range(4)]
R0 = 511
NEG = -30000.0
WB = 49152
AR = 39936
import os
STOP = os.environ.get("MK_STOP", "")


class T:
    def __init__(self, ap, name):
        self.t = ap
        self.o = Obj(name)


def _prod(s):
    r = 1
    for v in s:
        r *= v
    return r


class Carver:
    def __init__(self, buf, size):
        self.buf = buf
        self.size = size
        self.off = 0

    def reset(self):
        self.off = 0

    def get(self, shape, dt, name):
        n = _prod(shape[1:])
        sz = n * (2 if dt in (F32, I32, U32) else 1)
        sz = (sz + 15) // 16 * 16
        assert self.off + sz <= self.size, (name, self.off, sz, self.size)
        v = self.buf[0:shape[0], self.off:self.off + sz]
        self.off += sz
        if dt != BF16:
            v = v.bitcast(dt)
        v = v[:, 0:n]
        if len(shape) == 3:
            v = v.rearrange("p (a b) -> p a b", a=shape[1])
        elif len(shape) == 4:
            v = v.rearrange("p (a b c) -> p a b c", a=shape[1], b=shape[2])
        return T(v, name)


def bcast(ap2, dims):
    base = ap2.ap
    new = [base[0]]
    for d in dims:
        if d[0] == 'b':
            new.append((0, d[1]))
        else:
            new.append(base[1])
    return bass.AP(ap2.tensor, ap2.offset, tuple(new))


def build_nc():
    nc = bass.Bass("TRN2", target_bir_lowering=False)
    din = lambda name, shape, dt=F32: nc.dram_tensor(name, list(shape), dt, kind="ExternalInput")
    dout = lambda name, shape, dt=F32: nc.dram_tensor(name, list(shape), dt, kind="ExternalOutput")
    dscr = lambda name, shape, dt: nc.dram_tensor(name, list(shape), dt, kind="Internal")

    x_all = din("x_all", [NT * 128, D]).ap()
    x_s = din("x_s", [32, D]).ap()
    cache_k = din("cache_k", [1024, 512]).ap()
    cache_v = din("cache_v", [1024, 512]).ap()
    state_in = din("state_in", [4, 128, 128]).ap()
    c_both = din("c_both", [2, D]).ap()
    w_ada = din("w_ada", [D, 6 * D]).ap()
    b_ada = din("b_ada", [6 * D])
    norm1 = din("norm1", [D]).ap()
    norm2 = din("norm2", [D]).ap()
    w_in = din("w_in", [D, 5632]).ap()
    lamv = din("lamv", [4, 64])
    subln = din("subln", [2, 128]).ap()
    w_ba = din("w_ba", [512, D]).ap()
    w_br = din("w_br", [512, D]).ap()
    w_o = din("w_o", [D, D]).ap()
    rel_bias = din("rel_bias", [32, 4])
    w_pq = din("w_pq", [D, 2048]).ap()
    peer_keys = din("peer_keys", [16, 128, 128]).ap()
    peer_u = din("peer_u", [16384, D])
    peer_v = din("peer_v", [16384, D])
    final_norm = din("final_norm", [D])
    rot = din("rot", [NT + 1, 128, 256]).ap()
    dtab_d = din("dtab", [128, 1296]).ap()
    mcol_d = din("mcol", [128, 1024]).ap()
    vsv_d = din("vsv", [128, 3]).ap()
    oht5 = din("oht5", [32, 1280]).ap()
    jrev_d = din("jrev", [128, 128]).ap()
    ident_d = din("ident", [128, 128]).ap()
    zwin_d = din("zwin", [128, 256]).ap()
    iota_d = din("iota16", [128, 16]).ap()

    y_own = dout("y_own", [4096, D]).ap()
    k_own = dout("k_own", [4096, 512]).ap()
    v_own = dout("v_own", [4096, 512]).ap()
    ret_p = dout("ret_p", [4, 128, 128]).ap()
    y_s = dout("y_s", [32, D]).ap()
    k_s = dout("k_s", [32, 512]).ap()
    v_s = dout("v_s", [32, 512]).ap()
    ret_s = dout("ret_s", [4, 128, 128]).ap()

    Kscr = dscr("Kscr", [4, 128, 16384], BF16).ap()
    Vscr = dscr("Vscr", [4, 128, 128, 128], BF16).ap()
    Qscr = dscr("Qscr", [4, 128, 4096], BF16).ap()
    YAscr = dscr("YAscr", [4, 128, 4096], BF16).ap()
    YRscr = dscr("YRscr", [4, 128, 4096], BF16).ap()
    KSs = dscr("KSs", [4, 128, 1152], BF16).ap()
    VSs = dscr("VSs", [4, 128, 9, 128], BF16).ap()
    UVbf = dscr("UVbf", [16384, 2, D], BF16)
    H2scr = dscr("H2scr", [33 * 128, D], BF16)
    modscr = dscr("modscr", [2, 6 * D], F32)
    Gscr = dscr("Gscr", [4, 1280], F32)
    OK_, OV_, OQ_, OYA, OYR, OKS, OVS, OMOD, OG = [Obj(n) for n in
                                                  ("Kscr", "Vscr", "Qscr", "YAscr", "YRscr", "KSs", "VSs", "modscr", "Gscr")]
    Oout = Obj("outputs")

    st = ExitStack()
    with st:
        P = Prog(nc, st)
        op, dma = P.op, P.dma
        sbt = lambda name, shape, dt: T(st.enter_context(nc.sbuf_tensor("sb_" + name, list(shape), dt))[:], name)
        wbuf_t = st.enter_context(nc.sbuf_tensor("wbuf", [128, WB], BF16))
        arena_t = st.enter_context(nc.sbuf_tensor("arena", [128, AR], BF16))
        Owb = Obj("wbuf_weights")
        s0 = T(st.enter_context(nc.psum_tensor("ps0", [128, 512], F32))[:], "ps0")
        s7 = T(st.enter_context(nc.psum_tensor("ps7", [128, 512], F32))[:], "ps7")
        pairs = []
        for nm in "ABC":
            pt = st.enter_context(nc.psum_tensor("pp" + nm, [128, 1024], F32))
            pairs.append((pt, T(pt[:, 0:512], nm + "0"), T(pt[:, 512:1024], nm + "1")))
        (pA, pA0, pA1), (pB, pB0, pB1), (pC, pC0, pC1) = pairs
        for _b in (s0, s7, pA0, pA1, pB0, pB1, pC0, pC1):
            _b.o.excl = True

        identb = sbt("identb", [128, 128], BF16)
        identf = sbt("identf", [128, 128], F32)
        jrev = sbt("jrev", [128, 128], F32)
        onesb = sbt("onesb", [128, 128], BF16)
        zwin = sbt("zwin", [128, 256], F32)
        iota16 = sbt("iota16", [128, 16], F32)
        epsc = sbt("epsc", [128, 1], F32)
        cols = sbt("cols", [128, 2, 6, 8], F32)
        ncols = sbt("ncols", [128, 2, 8], F32)
        AB = sbt("AB", [128, 2, 4, 8], F32)
        ABv = sbt("ABv", [128, 3, 2, 8], F32)
        vsv = sbt("vsv", [128, 3], F32)
        lamc = sbt("lamc", [128, 8], F32)
        sub = sbt("sub", [128, 2], F32)
        G1 = sbt("G1", [128, D], F32)
        G2 = sbt("G2", [128, D], F32)
        FN = sbt("FN", [128, D], F32)
        S = sbt("S", [128, 4, 128], F32)
        Sb = sbt("Sb", [128, 4, 128], BF16)
        Ss = sbt("Ss", [128, 4, 128], F32)
        Ssb = sbt("Ssb", [128, 4, 128], BF16)
        keysT = sbt("keysT", [128, 16, 128], BF16)
        dtab = sbt("dtab", [128, 1296], F32)
        qs_T = sbt("qs_T", [128, 4, 32], BF16)
        yas_T = sbt("yas_T", [128, 4, 32], BF16)
        yrs_T = sbt("yrs_T", [128, 4, 32], BF16)

        A = Carver(arena_t, AR)
        W = Carver(wbuf_t, WB)

        mult, add, sub_ = ALU.mult, ALU.add, ALU.subtract

        def ld(dst, src, key, reads=()):
            dma("sp", lambda e: e.dma_start(out=dst.t if isinstance(dst, T) else dst, in_=src),
                reads=list(reads), writes=[dst.o], key=key)

        def ld_slow(dst_ap, dst_obj, src, key, reads=()):
            dma("sp", lambda e: e.dma_start(out=dst_ap, in_=src, allow_slow_non_contiguous=True),
                reads=list(reads), writes=[dst_obj], key=key)

        ld(identf, ident_d, "c0")
        ld(jrev, jrev_d, "c1")
        ld(zwin, zwin_d, "c2")
        ld(iota16, iota_d, "c3")
        ld(dtab, dtab_d, "c4")
        ld(vsv, vsv_d, "c5")
        op("pool", lambda e: e.memset(onesb.t[:], 1.0), writes=[onesb.o])
        op("pool", lambda e: e.memset(epsc.t[:], EPS), writes=[epsc.o])
        op("dve", lambda e: e.tensor_copy(out=identb.t[:], in_=identf.t[:]), reads=[identf.o], writes=[identb.o])
        op("pool", lambda e: e.memset(S.t[:], 0.0), writes=[S.o])
        op("pool", lambda e: e.memset(Sb.t[:], 0.0), writes=[Sb.o])

        A.reset()
        cT = A.get([128, 8, 2], F32, "cT")
        for g in range(2):
            ld_slow(cT.t[:, :, g], cT.o, c_both[g].rearrange("(k p) -> p k", p=128), "cT")
        op("act", lambda e: e.activation(out=cT.t[:], in_=cT.t[:], func=AF.Silu), reads=[cT.o], writes=[cT.o])
        wst = [A.get([128, 8, 512], F32, f"wst{i}") for i in range(2)]
        modsb = W.get([2, 6 * D], F32, "modsb")
        badd = W.get([2, 6 * D], F32, "badd")
        ld(badd, bass.AP(b_ada, 0, ((0, 2), (1, 6 * D))), "badd")
        w_ada_v = w_ada.rearrange("(k p) n -> p k n", p=128)
        for cg in range(12):
            ws = wst[cg % 2]
            ld(ws, w_ada_v[:, :, cg * 512:(cg + 1) * 512], f"wst{cg % 2}")
            bank = pA0 if cg % 2 == 0 else pA1
            for k in range(8):
                op("pe", lambda e, k=k, ws=ws, bank=bank: e.matmul(bank.t[0:2, :], lhsT=cT.t[:, k, :], rhs=ws.t[:, k, :],
                                                                   start=(k == 0), stop=(k == 7)),
                   reads=[cT.o, ws.o], writes=[bank.o])
            op("dve", lambda e, cg=cg, bank=bank: e.tensor_tensor(out=modsb.t[:, cg * 512:(cg + 1) * 512], in0=bank.t[0:2, :],
                                                                  in1=badd.t[:, cg * 512:(cg + 1) * 512], op=add),
               reads=[bank.o, badd.o], writes=[modsb.o])
        dma("pool", lambda e: e.dma_start(out=modscr.ap(), in_=modsb.t[:]), reads=[modsb.o], writes=[OMOD], key="modscr")
        for g in range(2):
            for v in range(6):
                src = bass.AP(modscr, g * 6 * D + v * D, ((1, 128), (128, 8)))
                ld_slow(cols.t[:, g, v, :], cols.o, src, "cols", reads=[OMOD])
        ld_slow(ncols.t[:, 0, :], ncols.o, norm1.rearrange("(k p) -> p k", p=128), "ncols")
        ld_slow(ncols.t[:, 1, :], ncols.o, norm2.rearrange("(k p) -> p k", p=128), "ncols")
        for g in range(2):
            for j, (vsc, vsh) in enumerate(((1, 0), (4, 3))):
                op("dve", lambda e, g=g, j=j, vsc=vsc: e.scalar_tensor_tensor(
                    out=AB.t[:, g, 2 * j, :], in0=cols.t[:, g, vsc, :], scalar=1.0, in1=ncols.t[:, j, :], op0=add, op1=mult),
                   reads=[cols.o, ncols.o], writes=[AB.o])
                op("dve", lambda e, g=g, j=j, vsh=vsh: e.tensor_copy(out=AB.t[:, g, 2 * j + 1, :], in_=cols.t[:, g, vsh, :]),
                   reads=[cols.o], writes=[AB.o])
        for gi in range(3):
            for j in range(2):
                op("dve", lambda e, gi=gi, j=j: e.tensor_scalar(out=ABv.t[:, gi, j, :], in0=AB.t[:, 0, j, :],
                                                                scalar1=vsv.t[:, gi:gi + 1], scalar2=None, op0=mult),
                   reads=[AB.o, vsv.o], writes=[ABv.o])

        def load_rows(g):
            ld(G1, bass.AP(modscr, g * 6 * D + 2 * D, ((0, 128), (1, D))), "G1", reads=[OMOD])
            ld(G2, bass.AP(modscr, g * 6 * D + 5 * D, ((0, 128), (1, D))), "G2", reads=[OMOD])

        ld(FN, bass.AP(final_norm, 0, ((0, 128), (1, D))), "FN")
        lq = A.get([128, 4, 64], F32, "lq")
        ld(lq, bass.AP(lamv, 0, ((0, 128), (1, 256))), "lq")
        ljunk = A.get([128, 64], F32, "ljunk")
        for i in range(2):
            op("dve", lambda e, i=i: e.scalar_tensor_tensor(out=ljunk.t[:], in0=lq.t[:, 2 * i, :], scalar=1.0, in1=lq.t[:, 2 * i + 1, :],
                                                            op0=mult, op1=mult, accum_out=lamc.t[:, i:i + 1]),
               reads=[lq.o], writes=[ljunk.o, lamc.o])
        op("act", lambda e: e.activation(out=lamc.t[:, 2:4], in_=lamc.t[:, 0:2], func=AF.Exp), reads=[lamc.o], writes=[lamc.o])
        op("dve", lambda e: e.tensor_tensor(out=lamc.t[:, 4:5], in0=lamc.t[:, 3:4], in1=lamc.t[:, 2:3], op=sub_),
           reads=[lamc.o], writes=[lamc.o])
        op("dve", lambda e: e.tensor_scalar(out=lamc.t[:, 5:6], in0=lamc.t[:, 4:5], scalar1=-LAM_INIT, scalar2=None, op0=add),
           reads=[lamc.o], writes=[lamc.o])
        nlam = lamc.t[:, 5:6]
        ld_slow(sub.t[:, :], sub.o, subln.rearrange("a p -> p a"), "sub")
        op("dve", lambda e: e.tensor_scalar(out=sub.t[:, 0:1], in0=sub.t[:, 0:1], scalar1=1.0 - LAM_INIT, scalar2=None, op0=mult),
           reads=[sub.o], writes=[sub.o])
        rb = A.get([32, 4], F32, "rb")
        rb15 = A.get([32, 4], F32, "rb15")
        oh = A.get([32, 1280], F32, "oh")
        gsb = A.get([4, 1280], F32, "gsb")
        ld(rb, rel_bias.ap(), "rb")
        ld(rb15, bass.AP(rel_bias, 15 * 4, ((0, 32), (1, 4))), "rb15")
        ld(oh, oht5, "oh")
        op("dve", lambda e: e.tensor_tensor(out=rb.t[:], in0=rb.t[:], in1=rb15.t[:], op=sub_), reads=[rb.o, rb15.o], writes=[rb.o])
        for i, (a, b) in enumerate(((0, 512), (512, 1024), (1024, 1280))):
            op("pe", lambda e, a=a, b=b: e.matmul(pB0.t[0:4, 0:b - a], lhsT=rb.t[:, :], rhs=oh.t[:, a:b], start=True, stop=True),
               reads=[rb.o, oh.o], writes=[pB0.o])
            op("act", lambda e, a=a, b=b: e.copy(out=gsb.t[:, a:b], in_=pB0.t[0:4, 0:b - a]), reads=[pB0.o], writes=[gsb.o])
        dma("pool", lambda e: e.dma_start(out=Gscr.ap(), in_=gsb.t[:]), reads=[gsb.o], writes=[OG], key="gscr")
        kst = W.get([128, 16, 128], F32, "kst")
        ksb = W.get([128, 16, 128], BF16, "ksb")
        ld(kst, peer_keys.rearrange("c n d -> n c d"), "kst")
        op("pool", lambda e: e.tensor_copy(out=ksb.t[:], in_=kst.t[:]), reads=[kst.o], writes=[ksb.o])
        s0b = s0.t[:].bitcast(BF16)
        for c4 in range(4):
            for j in range(4):
                c = c4 * 4 + j
                op("pe", lambda e, c=c, j=j: e.transpose(out=s0b[:, j * 128:(j + 1) * 128], in_=ksb.t[:, c, :], identity=identb.t[:]),
                   reads=[ksb.o, identb.o], writes=[s0.o])
            op("dve", lambda e, c4=c4: e.tensor_copy(out=keysT.t[:, c4 * 4:(c4 + 1) * 4, :],
                                                     in_=s0b[:, 0:512].rearrange("p (a b) -> p a b", a=4)),
               reads=[s0.o], writes=[keysT.o])
        cvi = [W.get([128, 2, D], F32, f"cvi{i}") for i in range(3)]
        cvo = [W.get([128, 2, D], BF16, f"cvo{i}") for i in range(3)]
        ci_ = 0
        for ti_, tab in enumerate((peer_u, peer_v)):
            tv = tab.ap().rearrange("(p r) d -> p r d", p=128)
            dv = UVbf.ap().rearrange("(p r) t d -> p r t d", p=128)[:, :, ti_, :]
            for rr in range(0, 128, 2):
                a_, b_ = cvi[ci_ % 3], cvo[ci_ % 3]
                ld(a_, tv[:, rr:rr + 2, :], a_.o.name)
                eng = ("dve", "act", "pool")[ci_ % 3]
                if eng == "act":
                    op("act", lambda e, a_=a_, b_=b_: e.copy(out=b_.t[:], in_=a_.t[:]), reads=[a_.o], writes=[b_.o])
                else:
                    op(eng, lambda e, a_=a_, b_=b_: e.tensor_copy(out=b_.t[:], in_=a_.t[:]), reads=[a_.o], writes=[b_.o])
                dma("pool" if ci_ % 2 == 0 else "sp", lambda e, b_=b_, dv=dv, rr=rr: e.dma_start(out=dv[:, rr:rr + 2, :], in_=b_.t[:]),
                    reads=[b_.o], key=b_.o.name)
                ci_ += 1
        P.barrier()
        if STOP == "p0":
            P.wait_all_dma("sp")
            P.emit()
            return nc

        def load_weights(specs, stage):
            for i, (dst, src, K, n) in enumerate(specs):
                sg = stage[i % len(stage)]
                dma("sp", lambda e, sg=sg, src=src, K=K, n=n: e.dma_start(out=sg.t[:, 0:K, 0:n], in_=src),
                    writes=[sg.o], key=sg.o.name)
                eng = "pool" if i % 2 == 0 else "dve"
                op(eng, lambda e, sg=sg, dst=dst, K=K, n=n: e.tensor_copy(out=dst, in_=sg.t[:, 0:K, 0:n]),
                   reads=[sg.o], writes=[Owb])

        def wspecs(dst3, src2, K, ncol, src_cols=None, piece=256):
            out = []
            sv = src2.rearrange("(k p) n -> p k n", p=128)
            for a in range(0, ncol, piece):
                n = min(piece, ncol - a)
                out.append((dst3[:, :, a:a + n], sv[:, :, src_cols + a:src_cols + a + n], K, n))
            return out

        A.reset()
        W.reset()
        Wall = W.get([128, 8, 2560], BF16, "Wall").t
        Wown = W.get([128, 8, 2048], BF16, "Wown").t
        stage = [A.get([128, 8, 256], F32, f"stg{i}") for i in range(2)]
        specs = []
        specs += wspecs(Wall[:, :, 0:512], w_in, 8, 512, KA)
        specs += wspecs(Wall[:, :, 512:1024], w_in, 8, 512, KR)
        specs += wspecs(Wall[:, :, 1536:2048], w_in, 8, 512, VA)
        specs += wspecs(Wall[:, :, 2048:2560], w_in, 8, 512, VR)
        specs += wspecs(Wown[:, :, 0:512], w_in, 8, 512, QA)
        specs += wspecs(Wown[:, :, 512:1024], w_in, 8, 512, QR)
        specs += wspecs(Wown[:, :, 1536:2048], w_in, 8, 512, GR)
        for h in range(4):
            specs += wspecs(Wall[:, :, 1024 + h * 128:1024 + h * 128 + 64], w_in, 8, 64, KR + h * 128 + 64)
            specs += wspecs(Wall[:, :, 1024 + h * 128 + 64:1024 + h * 128 + 128], w_in, 8, 64, KR + h * 128)
            specs += wspecs(Wown[:, :, 1024 + h * 128:1024 + h * 128 + 64], w_in, 8, 64, QR + h * 128 + 64)
            specs += wspecs(Wown[:, :, 1024 + h * 128 + 64:1024 + h * 128 + 128], w_in, 8, 64, QR + h * 128)
        load_weights(specs, stage)

        xs = [A.get([128, D], F32, f"xs{i}") for i in range(2)]
        F1 = A.get([128, D], F32, "F1")
        xn = A.get([128, D], BF16, "xn")
        hTs = [A.get([128, 8, 128], BF16, f"hT{i}") for i in range(2)]
        ss = A.get([128, 4], F32, "ss")
        rots = [A.get([128, 256], F32, f"rot{i}") for i in range(2)]
        kaT = [A.get([128, 4, 128], BF16, f"kaT{i}") for i in range(2)]
        vab = [A.get([128, 512], BF16, f"vab{i}") for i in range(2)]
        kout = [A.get([128, 512], F32, f"kout{i}") for i in range(2)]
        vout = [A.get([128, 512], F32, f"vout{i}") for i in range(2)]
        t1 = A.get([128, 4, 128], F32, "t1")
        t2 = A.get([128, 4, 128], F32, "t2")
        krot = A.get([128, 4, 128], BF16, "krot")
        krz = A.get([128, 4, 128], BF16, "krz")
        vrb = A.get([128, 512], BF16, "vrb")
        qb = [A.get([128, 4, 128], BF16, f"qb{i}") for i in range(2)]
        qrf = A.get([128, 4, 128], F32, "qrf")
        qrb = A.get([128, 4, 128], BF16, "qrb")
        qxb = A.get([128, 4, 128], BF16, "qxb")
        PTr = A.get([128, 4, 128], BF16, "PTr")
        orr = A.get([128, 4, 128], F32, "orr")
        sq = A.get([128, 512], BF16, "sq")
        sd = A.get([128, 512], F32, "sd")
        gsl = A.get([128, 4, 128], F32, "gsl")
        yrb = [A.get([128, 4, 128], BF16, f"yrb{i}") for i in range(2)]
        ckf = A.get([128, 512], F32, "ckf")
        ckb = A.get([128, 512], BF16, "ckb")

        def norm_transpose(xt, n, Acol, Bcol, AcolO, hT):
            op("act", lambda e: e.activation(out=F1.t[0:n, :], in_=xt.t[0:n, :], func=AF.Square, accum_out=ss.t[0:n, 0:1]),
               reads=[xt.o], writes=[F1.o, ss.o])
            op("act", lambda e: e.activation(out=ss.t[0:n, 1:2], in_=ss.t[0:n, 0:1], func=AF.Sqrt, scale=1.0 / D, bias=epsc.t[0:n, :]),
               reads=[ss.o, epsc.o], writes=[ss.o])
            op("dve", lambda e: e.reciprocal(out=ss.t[0:n, 2:3], in_=ss.t[0:n, 1:2]), reads=[ss.o], writes=[ss.o])
            op("dve", lambda e: e.tensor_scalar(out=xn.t[0:n, :], in0=xt.t[0:n, :], scalar1=ss.t[0:n, 2:3], scalar2=None, op0=mult),
               reads=[xt.o, ss.o], writes=[xn.o])
            for c in range(8):
                op("pe", lambda e, c=c: e.transpose(out=s0b[:, c * 128:c * 128 + n], in_=xn.t[0:n, c * 128:(c + 1) * 128],
                                                    identity=identb.t[0:n, 0:n]),
                   reads=[xn.o, identb.o], writes=[s0.o])
            for c in range(8):
                op("dve" if c % 2 == 0 else "act",
                   (lambda e, c=c: e.tensor_scalar(out=hT.t[:, c, 0:n], in0=s0b[:, c * 128:c * 128 + n],
                                                   scalar1=Acol[:, c:c + 1], scalar2=Bcol[:, c:c + 1], op0=mult, op1=add))
                   if c % 2 == 0 else
                   (lambda e, c=c: e.activation(out=hT.t[:, c, 0:n], in_=s0b[:, c * 128:c * 128 + n], func=AF.Identity,
                                                scale=Acol[:, c:c + 1], bias=Bcol[:, c:c + 1])),
                   reads=[s0.o, AcolO], writes=[hT.o])

        def fm_proj(bank, Wt, col0, nchunks, hT, n, reads_extra=()):
            for j in range(nchunks):
                for k in range(8):
                    op("pe", lambda e, j=j, k=k: e.matmul(bank.t[:, j * 128:j * 128 + n],
                                                          lhsT=Wt[:, k, col0 + j * 128:col0 + (j + 1) * 128],
                                                          rhs=hT.t[:, k, 0:n], start=(k == 0), stop=(k == 7)),
                       reads=[hT.o, Owb], writes=[bank.o])

        def tm_proj(bank, Wt, col0, hT, n):
            for k in range(8):
                op("pe", lambda e, k=k: e.matmul(bank.t[0:n, :], lhsT=hT.t[:, k, 0:n], rhs=Wt[:, k, col0:col0 + 512],
                                                 start=(k == 0), stop=(k == 7)),
                   reads=[hT.o, Owb], writes=[bank.o])

        def v4(bank, n):
            return bank.t[:, :].rearrange("p (a b) -> p a b", a=4)[:, :, 0:n]

        def rotate(bx, bxs, rt, n, out_f32):
            Cb = bcast(rt.t[:, 0:n], [('b', 4), ('x',)])
            Sb_ = bcast(rt.t[:, 128:128 + n], [('b', 4), ('x',)])
            op("dve", lambda e: e.tensor_tensor(out=t1.t[:, :, 0:n], in0=v4(bx, n), in1=Cb, op=mult),
               reads=[bx.o, rt.o], writes=[t1.o])
            op("dve", lambda e: e.tensor_tensor(out=t2.t[:, :, 0:n], in0=v4(bxs, n), in1=Sb_, op=mult),
               reads=[bxs.o, rt.o], writes=[t2.o])
            op("pool", lambda e: e.tensor_tensor(out=out_f32.t[:, :, 0:n], in0=t1.t[:, :, 0:n], in1=t2.t[:, :, 0:n], op=add),
               reads=[t1.o, t2.o], writes=[out_f32.o])

        def p1_tile(s, n, own_idx, sample):
            slot = s % 2
            xt = xs[slot]
            hT = hTs[slot]
            rt = rots[slot]
            g = 1 if sample else 0
            if sample:
                dma("sp", lambda e: e.dma_start(out=xt.t[0:32, :], in_=x_s), writes=[xt.o], key=f"xs{slot}")
            else:
                ld(xt, x_all[s * 128:(s + 1) * 128, :], f"xs{slot}")
            ld(rt, rot[NT if sample else s], f"rot{slot}")
            if (not sample) and s < 12:
                Acol, Bcol, AO = ABv.t[:, s // 4, 0, :], ABv.t[:, s // 4, 1, :], ABv.o
            else:
                Acol, Bcol, AO = AB.t[:, g, 0, :], AB.t[:, g, 1, :], AB.o
            norm_transpose(xt, n, Acol, Bcol, AO, hT)
            St, Sbt = (Ss, Ssb) if sample else (S, Sb)
            dto = 1032 if sample else 0
            xio = 1160 if sample else 512
            zeo = 1288 if sample else 1024
            dtw = 32 if sample else 128
            fm_proj(pA0, Wall, 0, 4, hT, n)
            kt = kaT[slot]
            op("act", lambda e: e.copy(out=kt.t[:, :, 0:n], in_=v4(pA0, n)), reads=[pA0.o], writes=[kt.o])
            if sample:
                dma("pool", lambda e: e.dma_start(out=KSs[:, :, 1024:1056].rearrange("h p t -> p h t"), in_=kt.t[:, :, 0:32]),
                    reads=[kt.o], key=f"kaT{slot}")
            else:
                dma("pool", lambda e: e.dma_start(out=Kscr[:, :, s * 128:(s + 1) * 128].rearrange("h p t -> p h t"), in_=kt.t[:]),
                    reads=[kt.o], key=f"kaT{slot}")
            tm_proj(pB1, Wall, 1536, hT, n)
            vb_ = vab[slot]
            op("act", lambda e: e.copy(out=vb_.t[0:n, :], in_=pB1.t[0:n, :]), reads=[pB1.o], writes=[vb_.o])
            if sample:
                dma("pool", lambda e: e.dma_start(out=VSs[:, 0:32, 8, :].rearrange("h p d -> p h d"),
                                                  in_=vb_.t[0:32, :].rearrange("p (h d) -> p h d", h=4)),
                    reads=[vb_.o], key=f"vab{slot}")
            else:
                dma("pool", lambda e: e.dma_start(out=Vscr[:, :, s, :].rearrange("h p d -> p h d"),
                                                  in_=vb_.t[:, :].rearrange("p (h d) -> p h d", h=4)),
                    reads=[vb_.o], key=f"vab{slot}")
            if own_idx is not None:
                vo = vout[own_idx % 2]
                op("dve", lambda e: e.tensor_copy(out=vo.t[0:n, :], in_=pB1.t[0:n, :]), reads=[pB1.o], writes=[vo.o])
                dst = v_s if sample else v_own[own_idx * 128:(own_idx + 1) * 128, :]
                dma("pool", lambda e, dst=dst: e.dma_start(out=dst, in_=vo.t[0:n, :]), reads=[vo.o], key=f"vout{own_idx % 2}")
                tm_proj(pC1, Wall, 0, hT, n)
                ko = kout[own_idx % 2]
                op("act", lambda e: e.copy(out=ko.t[0:n, :], in_=pC1.t[0:n, :]), reads=[pC1.o], writes=[ko.o])
                dst = k_s if sample else k_own[own_idx * 128:(own_idx + 1) * 128, :]
                dma("pool", lambda e, dst=dst: e.dma_start(out=dst, in_=ko.t[0:n, :]), reads=[ko.o], key=f"kout{own_idx % 2}")
            fm_proj(pA1, Wall, 512, 4, hT, n)
            fm_proj(pB0, Wall, 1024, 4, hT, n)
            rotate(pA1, pB0, rt, n, qrf)
            op("act", lambda e: e.copy(out=krot.t[:, :, 0:n], in_=qrf.t[:, :, 0:n]), reads=[qrf.o], writes=[krot.o])
            for h in range(4):
                op("pe", lambda e, h=h: e.transpose(out=s0b[0:n, h * 128:(h + 1) * 128], in_=krot.t[:, h, 0:n], identity=identb.t[:]),
                   reads=[krot.o, identb.o], writes=[s0.o])
            for h in range(4):
                op("dve", lambda e, h=h: e.tensor_scalar(out=krz.t[0:n, h, :], in0=s0b[0:n, h * 128:(h + 1) * 128],
                                                         scalar1=dtab.t[0:n, zeo + h:zeo + h + 1], scalar2=None, op0=mult),
                   reads=[s0.o, dtab.o], writes=[krz.o])
            tm_proj(pC0, Wall, 2048, hT, n)
            op("act", lambda e: e.copy(out=vrb.t[0:n, :], in_=pC0.t[0:n, :]), reads=[pC0.o], writes=[vrb.o])
            if own_idx is not None and int(os.environ.get("MK_CUT", "99")) >= 1:
                fm_proj(pA0, Wown, 0, 4, hT, n)
                q_ = qb[own_idx % 2]
                if sample:
                    op("act", lambda e: e.activation(out=qs_T.t[:, :, :], in_=v4(pA0, 32), func=AF.Copy, scale=0.125),
                       reads=[pA0.o], writes=[qs_T.o])
                else:
                    op("act", lambda e: e.activation(out=q_.t[:], in_=v4(pA0, 128), func=AF.Copy, scale=0.125),
                       reads=[pA0.o], writes=[q_.o])
                    dma("pool", lambda e: e.dma_start(out=Qscr[:, :, own_idx * 128:(own_idx + 1) * 128].rearrange("h p t -> p h t"),
                                                      in_=q_.t[:]), reads=[q_.o], key=f"qb{own_idx % 2}")
                if int(os.environ.get("MK_CUT", "99")) >= 2:
                    fm_proj(pA1, Wown, 512, 4, hT, n)
                    fm_proj(pB0, Wown, 1024, 4, hT, n)
                    rotate(pA1, pB0, rt, n, qrf)
                    op("act", lambda e: e.copy(out=qrb.t[:, :, 0:n], in_=qrf.t[:, :, 0:n]), reads=[qrf.o], writes=[qrb.o])
                    XIv = dtab.t[:, xio:xio + 4 * dtw].rearrange("p (a b) -> p a b", a=4)
                    op("pool", lambda e: e.tensor_tensor(out=qxb.t[:, :, 0:n], in0=qrf.t[:, :, 0:n], in1=XIv[:, :, 0:n], op=mult),
                       reads=[qrf.o, dtab.o], writes=[qxb.o])
                    for h in range(4):
                        op("pe", lambda e, h=h: e.matmul(pC1.t[0:n, h * 128:h * 128 + n], lhsT=krot.t[:, h, 0:n], rhs=qrb.t[:, h, 0:n],
                                                         start=True, stop=True), reads=[krot.o, qrb.o], writes=[pC1.o])
                    DTv = dtab.t[:, dto:dto + 4 * dtw].rearrange("p (a b) -> p a b", a=4)
                    op("dve", lambda e: e.tensor_tensor(out=PTr.t[0:n, :, 0:n], in0=v4(pC1, n)[0:n], in1=DTv[0:n, :, 0:n], op=mult),
                       reads=[pC1.o, dtab.o], writes=[PTr.o])
                    for h in range(4):
                        op("pe", lambda e, h=h: e.matmul(s7.t[:, h * 128:h * 128 + n], lhsT=vrb.t[0:n, h * 128:(h + 1) * 128],
                                                         rhs=PTr.t[0:n, h, 0:n], start=True, stop=False),
                           reads=[vrb.o, PTr.o], writes=[s7.o])
                        op("pe", lambda e, h=h: e.matmul(s7.t[:, h * 128:h * 128 + n], lhsT=Sbt.t[:, h, :],
                                                         rhs=qxb.t[:, h, 0:n], start=False, stop=True),
                           reads=[Sbt.o, qxb.o], writes=[s7.o])
                    op("act", lambda e: e.copy(out=orr.t[:, :, 0:n], in_=v4(s7, n)), reads=[s7.o], writes=[orr.o])
                    op("act", lambda e: e.activation(out=sq.t[:, 0:512].rearrange("p (a b) -> p a b", a=4)[:, :, 0:n],
                                                     in_=orr.t[:, :, 0:n], func=AF.Square), reads=[orr.o], writes=[sq.o])
                    for h in range(4):
                        op("pe", lambda e, h=h: e.matmul(pC1.t[:, h * 128:h * 128 + n], lhsT=onesb.t[:, :],
                                                         rhs=sq.t[:, h * 128:h * 128 + n], start=True, stop=True),
                           reads=[onesb.o, sq.o], writes=[pC1.o])
                    sdv = sd.t[:, :].rearrange("p (a b) -> p a b", a=4)[:, :, 0:n]
                    op("act", lambda e: e.activation(out=sdv, in_=v4(pC1, n), func=AF.Sqrt, scale=1.0 / 128, bias=epsc.t[:, :]),
                       reads=[pC1.o, epsc.o], writes=[sd.o])
                    op("dve", lambda e: e.reciprocal(out=sdv, in_=sdv), reads=[sd.o], writes=[sd.o])
                    op("dve", lambda e: e.scalar_tensor_tensor(out=orr.t[:, :, 0:n], in0=orr.t[:, :, 0:n], scalar=sub.t[:, 1:2],
                                                               in1=sdv, op0=mult, op1=mult),
                       reads=[orr.o, sub.o, sd.o], writes=[orr.o])
                    fm_proj(pA0, Wown, 1536, 4, hT, n)
                    op("act", lambda e: e.activation(out=gsl.t[:, :, 0:n], in_=v4(pA0, n), func=AF.Silu), reads=[pA0.o], writes=[gsl.o])
                    if sample:
                        op("pool", lambda e: e.tensor_tensor(out=yrs_T.t[:, :, :], in0=orr.t[:, :, 0:32], in1=gsl.t[:, :, 0:32], op=mult),
                           reads=[orr.o, gsl.o], writes=[yrs_T.o])
                    else:
                        y_ = yrb[own_idx % 2]
                        op("pool", lambda e: e.tensor_tensor(out=y_.t[:], in0=orr.t[:], in1=gsl.t[:], op=mult),
                           reads=[orr.o, gsl.o], writes=[y_.o])
                        dma("pool", lambda e: e.dma_start(out=YRscr[:, :, own_idx * 128:(own_idx + 1) * 128].rearrange("h p t -> p h t"),
                                                          in_=y_.t[:]), reads=[y_.o], key=f"yrb{own_idx % 2}")
            L = 32 if sample else 128
            for h in range(4):
                op("pe", lambda e, h=h: e.matmul(s7.t[:, h * 128:(h + 1) * 128], lhsT=krz.t[0:n, h, :],
                                                 rhs=vrb.t[0:n, h * 128:(h + 1) * 128], start=True, stop=True),
                   reads=[krz.o, vrb.o], writes=[s7.o])
            for h in range(4):
                op("dve", lambda e, h=h: e.scalar_tensor_tensor(out=St.t[:, h, :], in0=St.t[:, h, :], scalar=float(GAMMAS[h] ** L),
                                                                in1=s7.t[:, h * 128:(h + 1) * 128], op0=mult, op1=add),
                   reads=[St.o, s7.o], writes=[St.o])
            op("pool", lambda e: e.tensor_copy(out=Sbt.t[:], in_=St.t[:]), reads=[St.o], writes=[Sbt.o])

        _tiles = [int(v) for v in os.environ["MK_TILES"].split(",")] if os.environ.get("MK_TILES") else list(range(NT))
        for s in _tiles:
            p1_tile(s, 128, OWN.index(s) if s in OWN else None, False)
        dma("pool", lambda e: e.dma_start(out=ret_p.rearrange("h k v -> k h v"), in_=S.t[:]), reads=[S.o], key="retp")

        if os.environ.get("MK_NOSAMPLE"):
            P.barrier()
            P.wait_all_dma("sp")
            P.emit()
            return nc
        ld(Ss, state_in.rearrange("h k v -> k h v"), "Ss")
        op("pool", lambda e: e.tensor_copy(out=Ssb.t[:], in_=Ss.t[:]), reads=[Ss.o], writes=[Ssb.o])
        for blk in range(8):
            ld(ckf, cache_k[blk * 128:(blk + 1) * 128, :], "ckf")
            op("pool", lambda e: e.tensor_copy(out=ckb.t[:], in_=ckf.t[:]), reads=[ckf.o], writes=[ckb.o])
            for h in range(4):
                op("pe", lambda e, h=h: e.transpose(out=s0b[:, h * 128:(h + 1) * 128], in_=ckb.t[:, h * 128:(h + 1) * 128],
                                                    identity=identb.t[:]), reads=[ckb.o, identb.o], writes=[s0.o])
            kt = kaT[blk % 2]
            op("act", lambda e, kt=kt: e.copy(out=kt.t[:], in_=s0b[:, 0:512].rearrange("p (a b) -> p a b", a=4)),
               reads=[s0.o], writes=[kt.o])
            dma("pool", lambda e, kt=kt, blk=blk: e.dma_start(out=KSs[:, :, blk * 128:(blk + 1) * 128].rearrange("h p t -> p h t"),
                                                             in_=kt.t[:]), reads=[kt.o], key=f"kaT{blk % 2}")
            ld(ckf, cache_v[blk * 128:(blk + 1) * 128, :], "ckf")
            vb_ = vab[blk % 2]
            op("pool", lambda e, vb_=vb_: e.tensor_copy(out=vb_.t[:], in_=ckf.t[:]), reads=[ckf.o], writes=[vb_.o])
            dma("pool", lambda e, vb_=vb_, blk=blk: e.dma_start(out=VSs[:, :, blk, :].rearrange("h p d -> p h d"),
                                                               in_=vb_.t[:, :].rearrange("p (h d) -> p h d", h=4)),
                reads=[vb_.o], key=f"vab{blk % 2}")
        p1_tile(NT, 32, 32, True)
        dma("pool", lambda e: e.dma_start(out=ret_s.rearrange("h k v -> k h v"), in_=Ss.t[:]), reads=[Ss.o], key="rets")
        P.barrier()
        if STOP == "p1":
            P.wait_all_dma("sp")
            P.emit()
            return nc

        W.reset()
        Kt = [W.get([128, 1024], BF16, f"Kt{i}") for i in range(2)]
        Vc = [W.get([128, 8, 128], BF16, f"Vc{i}") for i in range(2)]
        Qt = [W.get([128, 512], BF16, f"Qt{i}") for i in range(2)]
        PT = [W.get([128, 512], BF16, f"PT{i}") for i in range(6)]
        sacc = [W.get([128, 512], F32, f"sacc{i}") for i in range(2)]
        onesf = W.get([128, 128], F32, "onesf")
        shi = W.get([128, 512], BF16, "shi")
        slo = W.get([128, 512], BF16, "slo")
        tmpf = [W.get([128, 512], F32, f"tmpf{i}") for i in range(2)]
        BT = [W.get([128, 512], F32, f"BT{i}") for i in range(5)]
        Hk = W.get([128, 512], F32, "Hk")
        mcol = W.get([128, 1024], F32, "mcol")
        fR = W.get([128, 512], F32, "fR")
        fo0 = W.get([128, 512], F32, "fo0")
        fo1 = W.get([128, 512], F32, "fo1")
        foa = W.get([128, 512], F32, "foa")
        fsd = W.get([128, 512], F32, "fsd")
        fsq = W.get([128, 512], BF16, "fsq")
        yab = [W.get([128, 512], BF16, f"yab{i}") for i in range(2)]
        zc = W.get([128, 1], F32, "zc")
        ld(mcol, mcol_d, "mcol")
        op("pool", lambda e: e.memset(zc.t[:], 0.0), writes=[zc.o])
        op("pool", lambda e: e.memset(onesf.t[:], 1.0), writes=[onesf.o])
        STb = [s0, s7, pC0, pC1]
        cnt = {"st": 0, "pt": 0, "ch": 0, "q": 0, "tf": 0, "ya": 0}

        def attend(h, Qap, Qo, nq, blocks, out_cb):
            accO = [pA0, pB0]
            accL = [pA1, pB1]
            nb = len(blocks)
            pend = {}

            def stage1(bi):
                b = blocks[bi]
                nk = b["nk"]
                if b.get("pre") is not None:
                    b["pre"]()
                Kap, Ko = b["K"]
                stbs, pts = [], []
                for m in range(2):
                    stb = STb[cnt["st"] % 4]; cnt["st"] += 1
                    stbs.append(stb)
                    op("pe", lambda e, m=m, stb=stb: e.matmul(stb.t[0:nk, 0:nq], lhsT=Kap[m * 64:(m + 1) * 64, :],
                                                              rhs=Qap[m * 64:(m + 1) * 64, :], start=True, stop=True),
                       reads=[Ko, Qo], writes=[stb.o])
                for m in range(2):
                    stb = stbs[m]
                    pt = PT[cnt["pt"] % 6]; cnt["pt"] += 1
                    pts.append(pt)
                    if b["bias"] is not None:
                        bap, bo = b["bias"]
                        tf = tmpf[cnt["tf"] % 2]; cnt["tf"] += 1
                        op("dve", lambda e, tf=tf, stb=stb: e.tensor_tensor(out=tf.t[0:nk, 0:nq], in0=stb.t[0:nk, 0:nq], in1=bap, op=add),
                           reads=[stb.o, bo], writes=[tf.o])
                        src, so = tf.t[0:nk, 0:nq], tf.o
                    else:
                        src, so = stb.t[0:nk, 0:nq], stb.o
                    op("act", lambda e, pt=pt, src=src: e.activation(out=pt.t[0:nk, 0:nq], in_=src, func=AF.Exp, bias=b["mc"][0:nk, :]),
                       reads=[so, mcol.o, zc.o], writes=[pt.o])
                    for (r0, r1, c0, c1) in b["zero"]:
                        op("pool", lambda e, pt=pt, r0=r0, r1=r1, c0=c0, c1=c1: e.memset(pt.t[r0:r1, c0:c1], 0.0), writes=[pt.o])
                pend[bi] = pts

            def stage2(bi):
                b = blocks[bi]
                nk = b["nk"]
                pts = pend.pop(bi)
                Vap, Vo = b["V"]
                for m in range(2):
                    pt = pts[m]
                    O_ = accO[m]
                    op("pe", lambda e, pt=pt, O_=O_: e.matmul(O_.t[:, 0:nq], lhsT=Vap, rhs=pt.t[0:nk, 0:nq], start=(bi == 0), stop=(bi == nb - 1)),
                       reads=[Vo, pt.o], writes=[O_.o])
                for m in range(2):
                    pt = pts[m]
                    sa = sacc[m]
                    if bi == 0:
                        op("dve", lambda e, pt=pt, sa=sa: e.tensor_copy(out=sa.t[0:nk, 0:nq], in_=pt.t[0:nk, 0:nq]), reads=[pt.o], writes=[sa.o])
                    else:
                        op("dve", lambda e, pt=pt, sa=sa: e.tensor_tensor(out=sa.t[0:nk, 0:nq], in0=sa.t[0:nk, 0:nq], in1=pt.t[0:nk, 0:nq], op=add),
                           reads=[pt.o, sa.o], writes=[sa.o])

            for bi in range(nb + 1):
                if bi < nb:
                    stage1(bi)
                if bi >= 1:
                    stage2(bi - 1)
            for m, fo in ((0, fo0), (1, fo1)):
                O_, L_ = accO[m], accL[m]
                sa = sacc[m]
                op("dve", lambda e, sa=sa: e.tensor_copy(out=shi.t[:, 0:nq], in_=sa.t[:, 0:nq]), reads=[sa.o], writes=[shi.o])
                op("dve", lambda e, sa=sa: e.tensor_tensor(out=slo.t[:, 0:nq], in0=sa.t[:, 0:nq], in1=shi.t[:, 0:nq], op=sub_),
                   reads=[sa.o, shi.o], writes=[slo.o])
                op("pe", lambda e, L_=L_: e.matmul(L_.t[:, 0:nq], lhsT=onesb.t[:, :], rhs=shi.t[:, 0:nq], start=True, stop=False),
                   reads=[onesb.o, shi.o], writes=[L_.o])
                op("pe", lambda e, L_=L_: e.matmul(L_.t[:, 0:nq], lhsT=onesb.t[:, :], rhs=slo.t[:, 0:nq], start=False, stop=True),
                   reads=[onesb.o, slo.o], writes=[L_.o])
                op("dve", lambda e, L_=L_: e.reciprocal(out=fR.t[:, 0:nq], in_=L_.t[:, 0:nq]), reads=[L_.o], writes=[fR.o])
                op("dve", lambda e, O_=O_, fo=fo: e.tensor_tensor(out=fo.t[:, 0:nq], in0=O_.t[:, 0:nq], in1=fR.t[:, 0:nq], op=mult),
                   reads=[O_.o, fR.o], writes=[fo.o])
            op("dve", lambda e: e.scalar_tensor_tensor(out=foa.t[:, 0:nq], in0=fo1.t[:, 0:nq], scalar=nlam, in1=fo0.t[:, 0:nq],
                                                       op0=mult, op1=add), reads=[fo0.o, fo1.o, lamc.o], writes=[foa.o])
            op("act", lambda e: e.activation(out=fsq.t[:, 0:nq], in_=foa.t[:, 0:nq], func=AF.Square), reads=[foa.o], writes=[fsq.o])
            op("pe", lambda e: e.matmul(pC1.t[:, 0:nq], lhsT=onesb.t[:, :], rhs=fsq.t[:, 0:nq], start=True, stop=True),
               reads=[onesb.o, fsq.o], writes=[pC1.o])
            op("act", lambda e: e.activation(out=fsd.t[:, 0:nq], in_=pC1.t[:, 0:nq], func=AF.Sqrt, scale=1.0 / 128, bias=epsc.t[:, :]),
               reads=[pC1.o, epsc.o], writes=[fsd.o])
            op("dve", lambda e: e.reciprocal(out=fsd.t[:, 0:nq], in_=fsd.t[:, 0:nq]), reads=[fsd.o], writes=[fsd.o])
            out_cb()

        for h in range(4):
            for j in range(-1, 4):
                src = bass.AP(Gscr, h * 1280 + (R0 - 127 - 128 * j), ((1, 128), (1, 512)))
                ld(Hk, src, "Hk", reads=[OG])
                op("pe", lambda e: e.matmul(pC1.t[:, :], lhsT=jrev.t[:, :], rhs=Hk.t[:, :], start=True, stop=True),
                   reads=[jrev.o, Hk.o], writes=[pC1.o])
                bt = BT[j + 1]
                op("act", lambda e, bt=bt: e.copy(out=bt.t[:], in_=pC1.t[:, :]), reads=[pC1.o], writes=[bt.o])
            for i in range(8):
                sg = 4 * i + 3
                nblk = 4 * sg + 4
                q_ = Qt[cnt["q"] % 2]; cnt["q"] += 1
                ld(q_, Qscr[h, :, i * 512:(i + 1) * 512], q_.o.name, reads=[OQ_])
                blocks = []
                base = cnt["ch"]
                nch = nblk // 8
                cnt["ch"] += nch

                def issue(ci, h=h, base=base):
                    kc_ = Kt[(base + ci) % 2]; vc_ = Vc[(base + ci) % 2]
                    ld(kc_, Kscr[h, :, ci * 1024:(ci + 1) * 1024], kc_.o.name, reads=[OK_])
                    ld(vc_, Vscr[h, :, ci * 8:(ci + 1) * 8, :], vc_.o.name, reads=[OV_])

                for kb in range(nblk):
                    ci = kb // 8
                    kc = Kt[(base + ci) % 2]; vc = Vc[(base + ci) % 2]
                    kl = kb % 8
                    j = kb - 4 * sg
                    b = dict(K=(kc.t[:, kl * 128:(kl + 1) * 128], kc.o), V=(vc.t[:, kl, :], vc.o), nk=128,
                             bias=None, mc=mcol.t[:, i * 128 + kb:i * 128 + kb + 1], zero=[], pre=None)
                    if kb == 0:
                        b["pre"] = (lambda issue=issue, nch=nch: [issue(c_) for c_ in range(min(2, nch))])
                    elif kl == 0 and ci + 1 < nch:
                        b["pre"] = (lambda issue=issue, ci=ci: issue(ci + 1))
                    if j >= -1:
                        b["bias"] = (BT[j + 1].t[:, :], BT[j + 1].o)
                    if j >= 0:
                        if j > 0:
                            b["zero"].append((0, 128, 0, 128 * j))
                        b["zero"].append((64, 128, 128 * j, 128 * j + 64))
                    blocks.append(b)

                def out_cb(i=i, h=h):
                    ya = yab[cnt["ya"] % 2]; cnt["ya"] += 1
                    op("dve", lambda e: e.scalar_tensor_tensor(out=ya.t[:, :], in0=foa.t[:, :], scalar=sub.t[:, 0:1], in1=fsd.t[:, :],
                                                               op0=mult, op1=mult), reads=[foa.o, sub.o, fsd.o], writes=[ya.o])
                    dma("pool", lambda e: e.dma_start(out=YAscr[h, :, i * 512:(i + 1) * 512], in_=ya.t[:, :]),
                        reads=[ya.o], key=ya.o.name)

                attend(h, q_.t[:, :], q_.o, 512, blocks, out_cb)
            kc = Kt[cnt["ch"] % 2]; vc = Vc[cnt["ch"] % 2]; cnt["ch"] += 1
            ld(kc, KSs[h, :, 0:1024], kc.o.name, reads=[OKS])
            ld(vc, VSs[h, :, 0:8, :], vc.o.name, reads=[OVS])
            kc2 = Kt[cnt["ch"] % 2]; vc2 = Vc[cnt["ch"] % 2]; cnt["ch"] += 1
            dma("sp", lambda e, kc2=kc2, h=h: e.dma_start(out=kc2.t[:, 0:32], in_=KSs[h, :, 1024:1056]), reads=[OKS], writes=[kc2.o],
                key=kc2.o.name)
            dma("sp", lambda e, vc2=vc2, h=h: e.dma_start(out=vc2.t[0:32, 0, :], in_=VSs[h, 0:32, 8, :]), reads=[OVS], writes=[vc2.o],
                key=vc2.o.name)
            blocks = []
            for kb in range(8):
                b = dict(K=(kc.t[:, kb * 128:(kb + 1) * 128], kc.o), V=(vc.t[:, kb, :], vc.o), nk=128, bias=None, mc=zc.t[:, 0:1], zero=[])
                if kb == 7:
                    b["bias"] = (BT[0].t[:, 0:32], BT[0].o)
                blocks.append(b)
            blocks.append(dict(K=(kc2.t[:, 0:32], kc2.o), V=(vc2.t[0:32, 0, :], vc2.o), nk=32,
                               bias=(BT[1].t[0:32, 0:32], BT[1].o), mc=zc.t[:, 0:1], zero=[]))

            def out_cb_s(h=h):
                op("dve", lambda e: e.scalar_tensor_tensor(out=yas_T.t[:, h, :], in0=foa.t[:, 0:32], scalar=sub.t[:, 0:1],
                                                           in1=fsd.t[:, 0:32], op0=mult, op1=mult),
                   reads=[foa.o, sub.o, fsd.o], writes=[yas_T.o])

            attend(h, qs_T.t[:, h, :], qs_T.o, 32, blocks, out_cb_s)
        P.barrier()
        if STOP == "p2":
            P.wait_all_dma("sp")
            P.emit()
            return nc

        A.reset()
        W.reset()
        Wg = W.get([128, 8, 2048], BF16, "Wg").t
        Wo = W.get([128, 8, 1024], BF16, "Wo").t
        Wpq = W.get([128, 8, 2048], BF16, "Wpq").t
        Wba = W.get([128, 4, 1024], BF16, "Wba").t
        Wbr = W.get([128, 4, 1024], BF16, "Wbr").t
        craw = A.get([128, 2, 8, 128], F32, "cand")
        stage3 = []
        for i in range(2):
            sg_ = T(craw.t[:, i], "cand")
            sg_.o = craw.o
            stage3.append(sg_)
        NG = 4
        uvb = [A.get([128, 2 * D], BF16, f"uv{i}") for i in range(NG)]
        acol = [A.get([128, 1], F32, f"acol{i}") for i in range(NG)]
        gcol = [A.get([128, 1], F32, f"gcol{i}") for i in range(NG)]
        specs = []
        specs += wspecs(Wg, w_in, 8, 2048, GA, piece=128)
        specs += wspecs(Wo, w_o, 8, 1024, 0, piece=128)
        specs += wspecs(Wpq, w_pq, 8, 2048, 0, piece=128)
        specs += wspecs(Wba, w_ba, 4, 1024, 0, piece=128)
        specs += wspecs(Wbr, w_br, 4, 1024, 0, piece=128)
        load_weights(specs, stage3)
        xs3 = A.get([128, D], F32, "xs3")
        xnew = A.get([128, D], F32, "xnew")
        F3 = A.get([128, D], F32, "F1b")
        xn3 = A.get([128, D], BF16, "xnb")
        hT3 = A.get([128, 8, 128], BF16, "hT3")
        h2T = A.get([128, 8, 128], BF16, "h2T")
        ss3 = A.get([128, 4], F32, "ss3")
        sga = A.get([128, 8, 128], BF16, "sga")
        sgb = A.get([128, 8, 128], BF16, "sgb")
        yain = A.get([128, 4, 128], BF16, "yain")
        yrin = A.get([128, 4, 128], BF16, "yrin")
        u1 = A.get([128, 4, 128], F32, "u1")
        u2 = A.get([128, 4, 128], F32, "u2")
        yT = A.get([128, 8, 128], BF16, "yT")
        h2tm = A.get([128, D], BF16, "h2tm")
        qT = A.get([128, 16, 128], BF16, "qT")
        wk = A.get([128, 256], F32, "wk")
        mx = A.get([128, 16, 16], F32, "mx")
        ix = A.get([128, 16, 16], U32, "ix")
        ixf = A.get([128, 16, 16], F32, "ixf")
        cand = T(craw.t.rearrange("p a b c -> p (a b c)").rearrange("p (a b c) -> p a b c", a=8, b=16), "cand")
        cand.o = craw.o
        ts_ = A.get([128, 8, 16], F32, "ts")
        pos = A.get([128, 8, 16], U32, "pos")
        piu = A.get([128, 8, 16], U32, "piu")
        pju = A.get([128, 8, 16], U32, "pju")
        pj = A.get([128, 8, 16], F32, "pj")
        pi_ = A.get([128, 8, 16], F32, "pi")
        seli = A.get([128, 8, 16], F32, "seli")
        selj = A.get([128, 8, 16], F32, "selj")
        eidx = A.get([128, 128], F32, "eidx")
        ee = A.get([128, 8, 16], F32, "ee")
        esum = A.get([128, 8], F32, "esum")
        gg = A.get([128, 128], F32, "gg")
        eidxT = A.get([128, 128], I32, "eidxT")
        gT = A.get([128, 128], F32, "gT")
        cm = [A.get([128, 128], BF16, f"cm{i}") for i in range(NG)]
        s0b = s0.t[:].bitcast(BF16)
        load_rows(0)
        NH = 3
        dj = xn3
        hb = [A.get([128, D], BF16, f"hb{i}") for i in range(NH)]
        print("P3 arena used", A.off, "of", AR)
        OH2 = Obj("H2scr")

        def norm_transpose3(xt, n, Acol, Bcol, hT):
            op("act", lambda e: e.activation(out=F3.t[0:n, :], in_=xt.t[0:n, :], func=AF.Square, accum_out=ss3.t[0:n, 0:1]),
               reads=[xt.o], writes=[F3.o, ss3.o])
            op("act", lambda e: e.activation(out=ss3.t[0:n, 1:2], in_=ss3.t[0:n, 0:1], func=AF.Sqrt, scale=1.0 / D, bias=epsc.t[0:n, :]),
               reads=[ss3.o, epsc.o], writes=[ss3.o])
            op("dve", lambda e: e.reciprocal(out=ss3.t[0:n, 2:3], in_=ss3.t[0:n, 1:2]), reads=[ss3.o], writes=[ss3.o])
            op("dve", lambda e: e.tensor_scalar(out=xn3.t[0:n, :], in0=xt.t[0:n, :], scalar1=ss3.t[0:n, 2:3], scalar2=None, op0=mult),
               reads=[xt.o, ss3.o], writes=[xn3.o])
            for c in range(8):
                op("pe", lambda e, c=c: e.transpose(out=s0b[:, c * 128:c * 128 + n], in_=xn3.t[0:n, c * 128:(c + 1) * 128],
                                                    identity=identb.t[0:n, 0:n]), reads=[xn3.o, identb.o], writes=[s0.o])
            for c in range(8):
                op("dve", lambda e, c=c: e.tensor_scalar(out=hT.t[:, c, 0:n], in0=s0b[:, c * 128:c * 128 + n],
                                                         scalar1=Acol[:, c:c + 1], scalar2=Bcol[:, c:c + 1], op0=mult, op1=add),
                   reads=[s0.o, AB.o], writes=[hT.o])

        def top16(src_ap, src_o, n, width, out_v, out_i, vo, io):
            op("dve", lambda e: e.max(out=out_v[:, 0:8], in_=src_ap), reads=[src_o], writes=[vo])
            op("dve", lambda e: e.match_replace(out=wk.t[0:n, 0:width], in_to_replace=out_v[:, 0:8], in_values=src_ap, imm_value=-1e30),
               reads=[src_o, vo], writes=[wk.o])
            op("dve", lambda e: e.max(out=out_v[:, 8:16], in_=wk.t[0:n, 0:width]), reads=[wk.o], writes=[vo])
            op("dve", lambda e: e.max_index(out=out_i[:, 0:8], in_max=out_v[:, 0:8], in_values=src_ap), reads=[src_o, vo], writes=[io])
            op("dve", lambda e: e.max_index(out=out_i[:, 8:16], in_max=out_v[:, 8:16], in_values=wk.t[0:n, 0:width]),
               reads=[wk.o, vo], writes=[io])

        def p3_tile(oi, n, sample):
            g = 1 if sample else 0
            A1c, B1c, A2c, B2c = (AB.t[:, g, k, :] for k in range(4))
            if sample:
                dma("sp", lambda e: e.dma_start(out=xs3.t[0:32, :], in_=x_s), writes=[xs3.o], key="xs3")
                ya_ap, yr_ap, ya_o, yr_o = yas_T.t, yrs_T.t, yas_T.o, yrs_T.o
            else:
                s = OWN[oi]
                ld(xs3, x_all[s * 128:(s + 1) * 128, :], "xs3")
                ld(yain, YAscr[:, :, oi * 128:(oi + 1) * 128].rearrange("h p t -> p h t"), "yain", reads=[OYA])
                ld(yrin, YRscr[:, :, oi * 128:(oi + 1) * 128].rearrange("h p t -> p h t"), "yrin", reads=[OYR])
                ya_ap, yr_ap, ya_o, yr_o = yain.t, yrin.t, yain.o, yrin.o
            norm_transpose3(xs3, n, A1c, B1c, hT3)
            for gi, sgt in ((0, sga), (1, sgb)):
                for half, bank in ((0, pA0), (1, pA1)):
                    fm_proj(bank, Wg, gi * 1024 + half * 512, 4, hT3, n)
                    op("act", lambda e, sgt=sgt, half=half, bank=bank: e.activation(out=sgt.t[:, half * 4:(half + 1) * 4, 0:n], in_=v4(bank, n),
                                                                                    func=AF.Sigmoid), reads=[bank.o], writes=[sgt.o])
            for half, (bA, bR) in ((0, (pB0, pC0)), (1, (pB1, pC1))):
                for j in range(4):
                    jj = half * 4 + j
                    for h in range(4):
                        op("pe", lambda e, j=j, jj=jj, h=h, bA=bA: e.matmul(bA.t[:, j * 128:j * 128 + n], lhsT=Wba[:, h, jj * 128:(jj + 1) * 128],
                                                                             rhs=ya_ap[:, h, 0:n], start=(h == 0), stop=(h == 3)),
                           reads=[Owb, ya_o], writes=[bA.o])
                    for h in range(4):
                        op("pe", lambda e, j=j, jj=jj, h=h, bR=bR: e.matmul(bR.t[:, j * 128:j * 128 + n], lhsT=Wbr[:, h, jj * 128:(jj + 1) * 128],
                                                                             rhs=yr_ap[:, h, 0:n], start=(h == 0), stop=(h == 3)),
                           reads=[Owb, yr_o], writes=[bR.o])
                op("dve", lambda e, half=half, bA=bA: e.tensor_tensor(out=u1.t[:, :, 0:n], in0=v4(bA, n), in1=sga.t[:, half * 4:(half + 1) * 4, 0:n],
                                                                      op=mult), reads=[bA.o, sga.o], writes=[u1.o])
                op("dve", lambda e, half=half, bR=bR: e.tensor_tensor(out=u2.t[:, :, 0:n], in0=v4(bR, n), in1=sgb.t[:, half * 4:(half + 1) * 4, 0:n],
                                                                      op=mult), reads=[bR.o, sgb.o], writes=[u2.o])
                op("pool", lambda e, half=half: e.tensor_tensor(out=yT.t[:, half * 4:(half + 1) * 4, 0:n], in0=u1.t[:, :, 0:n], in1=u2.t[:, :, 0:n],
                                                                op=add), reads=[u1.o, u2.o], writes=[yT.o])
            for half, bank in ((0, pA0), (1, pA1)):
                for k in range(8):
                    op("pe", lambda e, k=k, half=half, bank=bank: e.matmul(bank.t[0:n, :], lhsT=yT.t[:, k, 0:n], rhs=Wo[:, k, half * 512:(half + 1) * 512],
                                                                           start=(k == 0), stop=(k == 7)), reads=[yT.o, Owb], writes=[bank.o])
            op("dve", lambda e: e.tensor_tensor(out=F3.t[0:n, :], in0=pA[0:n, :], in1=G1.t[0:n, :], op=mult),
               reads=[pA0.o, pA1.o, G1.o], writes=[F3.o])
            op("pool", lambda e: e.tensor_tensor(out=xnew.t[0:n, :], in0=F3.t[0:n, :], in1=xs3.t[0:n, :], op=add),
               reads=[F3.o, xs3.o], writes=[xnew.o])
            norm_transpose3(xnew, n, A2c, B2c, h2T)
            for c in range(8):
                op("pe", lambda e, c=c: e.transpose(out=s0b[0:n, c * 128:(c + 1) * 128], in_=h2T.t[:, c, 0:n], identity=identb.t[:, :]),
                   reads=[h2T.o, identb.o], writes=[s0.o])
            op("act", lambda e: e.copy(out=h2tm.t[0:n, :], in_=s0b[0:n, :]), reads=[s0.o], writes=[h2tm.o])
            dma("sp", lambda e: e.dma_start(out=H2scr.ap()[oi * 128:oi * 128 + n, :], in_=h2tm.t[0:n, :]), reads=[h2tm.o], writes=[OH2], key="h2st")
            for c4, bank in enumerate((pA0, pA1, pB0, pB1)):
                fm_proj(bank, Wpq, c4 * 512, 4, h2T, n)
                op("act", lambda e, c4=c4, bank=bank: e.copy(out=qT.t[:, c4 * 4:(c4 + 1) * 4, 0:n], in_=v4(bank, n)), reads=[bank.o], writes=[qT.o])
            banks4 = (pA0, pA1, pB0, pB1)
            for c in range(16):
                bank = banks4[c // 4]
                op("pe", lambda e, c=c, bank=bank: e.matmul(bank.t[0:n, (c % 4) * 128:(c % 4 + 1) * 128], lhsT=qT.t[:, c, 0:n], rhs=keysT.t[:, c, :],
                                                            start=True, stop=True), reads=[qT.o, keysT.o], writes=[bank.o])
            for c in range(16):
                bank = banks4[c // 4]
                top16(bank.t[0:n, (c % 4) * 128:(c % 4 + 1) * 128], bank.o, n, 128, mx.t[0:n, c, :], ix.t[0:n, c, :], mx.o, ix.o)
            op("dve", lambda e: e.tensor_copy(out=ixf.t[0:n], in_=ix.t[0:n]), reads=[ix.o], writes=[ixf.o])

            def mxv(off, pat):
                base = mx.t[0:n, 0, 0:1]
                return bass.AP(base.tensor, base.offset + off, (base.ap[0],) + pat)

            def ixv(off, pat):
                base = ixf.t[0:n, 0, 0:1]
                return bass.AP(base.tensor, base.offset + off, (base.ap[0],) + pat)

            op("dve", lambda e: e.tensor_tensor(out=cand.t[0:n], in0=mxv(0, ((32, 8), (1, 16), (0, 16))), in1=mxv(16, ((32, 8), (0, 16), (1, 16))),
                                                op=add), reads=[mx.o], writes=[cand.o])
            for h in range(8):
                top16(cand.t[0:n, h].rearrange("p a b -> p (a b)"), cand.o, n, 256, ts_.t[0:n, h, :], pos.t[0:n, h, :], ts_.o, pos.o)
            t0 = bass.AP(ts_.t[0:n, 0, 0:1].tensor, ts_.t[0:n, 0, 0:1].offset, (ts_.t[0:n, 0, 0:1].ap[0], (16, 8), (0, 16)))
            op("dve", lambda e: e.tensor_tensor(out=ee.t[0:n], in0=ts_.t[0:n], in1=t0, op=sub_), reads=[ts_.o], writes=[ee.o])
            op("act", lambda e: e.activation(out=ee.t[0:n], in_=ee.t[0:n], func=AF.Exp), reads=[ee.o], writes=[ee.o])
            op("dve", lambda e: e.reduce_sum(out=esum.t[0:n, :], in_=ee.t[0:n], axis=AX.X), reads=[ee.o], writes=[esum.o])
            op("dve", lambda e: e.reciprocal(out=esum.t[0:n, :], in_=esum.t[0:n, :]), reads=[esum.o], writes=[esum.o])
            e0 = bass.AP(esum.t[0:n, 0:1].tensor, esum.t[0:n, 0:1].offset, (esum.t[0:n, 0:1].ap[0], (1, 8), (0, 16)))
            op("dve", lambda e: e.tensor_tensor(out=gg.t[0:n, :].rearrange("p (a b) -> p a b", a=8), in0=ee.t[0:n], in1=e0, op=mult),
               reads=[ee.o, esum.o], writes=[gg.o])
            op("dve", lambda e: e.tensor_scalar(out=piu.t[0:n], in0=pos.t[0:n], scalar1=4, scalar2=None, op0=ALU.logical_shift_right),
               reads=[pos.o], writes=[piu.o])
            op("dve", lambda e: e.tensor_scalar(out=pju.t[0:n], in0=pos.t[0:n], scalar1=15, scalar2=None, op0=ALU.bitwise_and),
               reads=[pos.o], writes=[pju.o])
            op("dve", lambda e: e.tensor_copy(out=pi_.t[0:n], in_=piu.t[0:n]), reads=[piu.o], writes=[pi_.o])
            op("dve", lambda e: e.tensor_copy(out=pj.t[0:n], in_=pju.t[0:n]), reads=[pju.o], writes=[pj.o])
            io_ = iota16.t[0:n, 0:1]
            iotav = bass.AP(io_.tensor, io_.offset, (io_.ap[0], (0, 8), (0, 16), (1, 16)))
            for (pp, off, sel) in ((pi_, 0, seli), (pj, 16, selj)):
                b_ = pp.t[0:n, 0, 0:1]
                pv = bass.AP(b_.tensor, b_.offset, (b_.ap[0], (16, 8), (1, 16), (0, 16)))
                op("dve", lambda e, pv=pv: e.tensor_tensor(out=cand.t[0:n], in0=pv, in1=iotav, op=ALU.is_equal),
                   reads=[pp.o, iota16.o], writes=[cand.o])
                op("dve", lambda e, off=off: e.tensor_tensor(out=cand.t[0:n], in0=cand.t[0:n], in1=ixv(off, ((32, 8), (0, 16), (1, 16))), op=mult),
                   reads=[cand.o, ixf.o], writes=[cand.o])
                op("dve", lambda e, sel=sel: e.reduce_sum(out=sel.t[0:n], in_=cand.t[0:n], axis=AX.X), reads=[cand.o], writes=[sel.o])
            op("dve", lambda e: e.scalar_tensor_tensor(out=eidx.t[0:n, :].rearrange("p (a b) -> p a b", a=8), in0=seli.t[0:n], scalar=128.0,
                                                       in1=selj.t[0:n], op0=mult, op1=add), reads=[seli.o, selj.o], writes=[eidx.o])
            op("pe", lambda e: e.transpose(out=s7.t[:, 0:n], in_=eidx.t[0:n, :], identity=identf.t[0:n, 0:n]), reads=[eidx.o, identf.o], writes=[s7.o])
            op("dve", lambda e: e.tensor_copy(out=eidxT.t[:, 0:n], in_=s7.t[:, 0:n]), reads=[s7.o], writes=[eidxT.o])
            op("pe", lambda e: e.transpose(out=s7.t[:, 128:128 + n], in_=gg.t[0:n, :], identity=identf.t[0:n, 0:n]), reads=[gg.o, identf.o], writes=[s7.o])
            op("act", lambda e: e.copy(out=gT.t[:, 0:n], in_=s7.t[:, 128:128 + n]), reads=[s7.o], writes=[gT.o])
            UVsrc = UVbf.ap().rearrange("e t d -> e (t d)")

            def st_gather(t):
                uv = uvb[t % NG]
                dma("pool", lambda e: e.indirect_dma_start(out=uv.t[:, :], out_offset=None, in_=UVsrc,
                                                           in_offset=bass.IndirectOffsetOnAxis(ap=eidxT.t[:, t:t + 1], axis=0)),
                    reads=[eidxT.o], writes=[uv.o], key=f"{uv.o.name}_{oi % 4}")

            def st_bcast(t):
                hb_ = hb[t % NH]
                src = bass.AP(H2scr, (oi * 128 + t) * D, ((0, 128), (1, D)))
                dma("sp", lambda e: e.dma_start(out=hb_.t[:, :], in_=src), reads=[OH2], writes=[hb_.o], key=f"{hb_.o.name}_{oi % 2}")

            def st_dot(t):
                uv, ac, gc, hb_ = uvb[t % NG], acol[t % NG], gcol[t % NG], hb[t % NH]
                op("dve", lambda e: e.scalar_tensor_tensor(out=dj.t[:, :], in0=uv.t[:, 0:D], scalar=1.0, in1=hb_.t[:, :],
                                                           op0=mult, op1=mult, accum_out=ac.t[:, 0:1]),
                   reads=[uv.o, hb_.o], writes=[ac.o])
                op("act", lambda e: e.activation(out=gc.t[:, 0:1], in_=ac.t[:, 0:1], func=AF.Gelu), reads=[ac.o], writes=[gc.o])

            def st_cm(t):
                gc, c_ = gcol[t % NG], cm[t % NG]
                op("dve", lambda e: e.tensor_scalar(out=c_.t[:, 0:n], in0=zwin.t[:, 127 - t:127 - t + n], scalar1=gc.t[:, 0:1],
                                                    scalar2=gT.t[:, t:t + 1], op0=mult, op1=mult),
                   reads=[zwin.o, gc.o, gT.o], writes=[c_.o])

            def st_acc(t):
                uv, c_ = uvb[t % NG], cm[t % NG]
                for half, bank in ((0, pB0), (1, pB1)):
                    op("pe", lambda e, half=half, bank=bank: e.matmul(bank.t[0:n, :], lhsT=c_.t[:, 0:n],
                                                                      rhs=uv.t[:, D + half * 512:D + (half + 1) * 512],
                                                                      start=(t == 0), stop=(t == n - 1)),
                       reads=[c_.o, uv.o], writes=[bank.o])

            for t in range(n + 2):
                if t < n:
                    st_gather(t)
                    st_bcast(t)
                    st_dot(t)
                if 1 <= t <= n:
                    st_cm(t - 1)
                if t >= 2:
                    st_acc(t - 2)
            op("dve", lambda e: e.tensor_tensor(out=F3.t[0:n, :], in0=pB[0:n, :], in1=G2.t[0:n, :], op=mult), reads=[pB0.o, pB1.o, G2.o], writes=[F3.o])
            op("pool", lambda e: e.tensor_tensor(out=xnew.t[0:n, :], in0=F3.t[0:n, :], in1=xnew.t[0:n, :], op=add), reads=[F3.o, xnew.o], writes=[xnew.o])
            op("act", lambda e: e.activation(out=F3.t[0:n, :], in_=xnew.t[0:n, :], func=AF.Square, accum_out=ss3.t[0:n, 0:1]), reads=[xnew.o], writes=[F3.o, ss3.o])
            op("act", lambda e: e.activation(out=ss3.t[0:n, 1:2], in_=ss3.t[0:n, 0:1], func=AF.Sqrt, scale=1.0 / D, bias=epsc.t[0:n, :]),
               reads=[ss3.o, epsc.o], writes=[ss3.o])
            op("dve", lambda e: e.reciprocal(out=ss3.t[0:n, 2:3], in_=ss3.t[0:n, 1:2]), reads=[ss3.o], writes=[ss3.o])
            op("dve", lambda e: e.scalar_tensor_tensor(out=F3.t[0:n, :], in0=xnew.t[0:n, :], scalar=ss3.t[0:n, 2:3], in1=FN.t[0:n, :], op0=mult, op1=mult),
               reads=[xnew.o, ss3.o, FN.o], writes=[F3.o])
            dst = y_s if sample else y_own[oi * 128:(oi + 1) * 128, :]
            dma("sp", lambda e: e.dma_start(out=dst, in_=F3.t[0:n, :]), reads=[F3.o], key="yout")

        for oi in range(32):
            p3_tile(oi, 128, False)
        load_rows(1)
        p3_tile(32, 32, True)
        P.wait_all_dma("sp")
        P.emit()
    return nc


def _t5_bucket(rel):
    nb = 16
    ret = 16 if rel > 0 else 0
    n = abs(rel)
    if n < 8:
        return ret + n
    nf = np.float32(max(n, 8))
    large = 8 + int(np.float32(np.log(nf / np.float32(8)) / np.float32(math.log(16.0)) * np.float32(8)))
    return ret + min(large, 15)


def _static_tables(r):
    shift = 3 - r
    inv = (1.0 / (10000.0 ** np.linspace(0.0, 1.0, 64, dtype=np.float32))).astype(np.float32)
    rot = np.zeros((NT + 1, 128, 256), np.float32)
    pos = (np.arange(NT * 128, dtype=np.int64) - shift * 512).clip(0).astype(np.float32)
    ang = (pos[:, None] * inv[None, :]).astype(np.float32).astype(np.float64)
    cos, sin = np.cos(ang).astype(np.float32), np.sin(ang).astype(np.float32)
    C = np.concatenate([cos, cos], 1).T.reshape(128, NT, 128)
    Sg = np.concatenate([-sin, sin], 1).T.reshape(128, NT, 128)
    rot[:NT, :, 0:128] = C.transpose(1, 0, 2)
    rot[:NT, :, 128:256] = Sg.transpose(1, 0, 2)
    ps = (1024 + np.arange(32)).astype(np.float32)
    angs = (ps[:, None] * inv[None, :]).astype(np.float32).astype(np.float64)
    cs, sn = np.cos(angs).astype(np.float32), np.sin(angs).astype(np.float32)
    rot[NT, :, 0:32] = np.concatenate([cs, cs], 1).T
    rot[NT, :, 128:160] = np.concatenate([-sn, sn], 1).T
    dtab = np.zeros((128, 1296), np.float32)
    sc = 128.0 ** -0.5
    for h in range(4):
        lg = math.log(GAMMAS[h])
        for (L, dto, xio, zeo) in ((128, 0, 512, 1024), (32, 1032, 1160, 1288)):
            n = np.arange(L, dtype=np.float64)
            diff = n[None, :] - n[:, None]
            DT = np.where(diff >= 0, np.exp(np.maximum(diff, 0) * lg), 0.0) * sc
            dtab[0:L, dto + h * L:dto + (h + 1) * L] = DT
            dtab[:, xio + h * L:xio + (h + 1) * L] = np.exp((n + 1.0) * lg)[None, :]
            dtab[0:L, zeo + h] = np.exp((L - 1.0 - n) * lg) * sc
    mcol = np.zeros((128, 1024), np.float32)
    for i in range(8):
        mcol[:, i * 128:i * 128 + 4 * shift] = NEG
    vsv = np.ones((128, 3), np.float32)
    vsv[:, 0:shift] = 0.0
    return rot, dtab, mcol, vsv


def kernel(x_prompt, x_sample, cache_k, cache_v, state_ret, c_prompt, c_sample, w_ada, b_ada, norm1, norm2, w_in,
           lam_q1, lam_k1, lam_q2, lam_k2, subln_a, subln_r, w_ba, w_br, w_o, rel_bias, w_pq, peer_keys, peer_u, peer_v,
           final_norm):
    f = lambda a: np.ascontiguousarray(np.asarray(a, dtype=np.float32))
    x_prompt, x_sample = f(x_prompt), f(x_sample)
    oht5 = np.zeros((32, 1280), np.float32)
    for i in range(1151):
        oht5[_t5_bucket(R0 - i), i] = 1.0
    zwin = np.zeros((128, 256), np.float32)
    zwin[:, 127] = 1.0
    shared = dict(
        w_ada=f(w_ada)[0], b_ada=f(b_ada)[0], norm1=f(norm1)[0], norm2=f(norm2)[0], w_in=f(w_in)[0],
        lamv=np.stack([f(lam_q1)[0], f(lam_k1)[0], f(lam_q2)[0], f(lam_k2)[0]]),
        subln=np.stack([f(subln_a)[0], f(subln_r)[0]]), w_ba=f(w_ba)[0], w_br=f(w_br)[0], w_o=f(w_o)[0],
        rel_bias=f(rel_bias), w_pq=f(w_pq)[0], peer_keys=f(peer_keys)[0].reshape(16, 128, 128),
        peer_u=f(peer_u)[0], peer_v=f(peer_v)[0], final_norm=f(final_norm), oht5=oht5,
        jrev=np.ascontiguousarray(np.eye(128, dtype=np.float32)[::-1]), ident=np.eye(128, dtype=np.float32), zwin=zwin,
        iota16=np.tile(np.arange(16, dtype=np.float32)[None, :], (128, 1)),
    )
    tabs = [_static_tables(r) for r in range(4)]
    in_maps = []
    for c in range(8):
        b, r = c // 4, c % 4
        shift = 3 - r
        xa = np.zeros((NT * 128, D), np.float32)
        xa[shift * 512:] = x_prompt[b, :NT * 128 - shift * 512]
        rot, dtab, mcol, vsv = tabs[r]
        m = dict(shared)
        m.update(x_all=xa, x_s=x_sample[c], cache_k=f(cache_k)[0, c].reshape(1024, 512), cache_v=f(cache_v)[0, c].reshape(1024, 512),
                 state_in=f(state_ret)[0, c], c_both=np.stack([f(c_prompt)[b], f(c_sample)[c]]),
                 rot=rot, dtab=dtab, mcol=mcol, vsv=vsv)
        in_maps.append(m)
    nc = build_nc()
    if STOP:
        return nc, in_maps
    res = run_bass_kernel_spmd(nc, in_maps, core_ids=list(range(8)))
    R = res.results
    y_prompt = np.zeros((2, 16384, D), np.float32)
    k_prompt = np.zeros((1, 2, 16384, 4, 128), np.float32)
    v_prompt = np.zeros((1, 2, 16384, 4, 128), np.float32)
    ret_prompt = np.zeros((1, 2, 4, 128, 128), np.float32)
    y_sample = np.zeros((8, 32, D), np.float32)
    k_sample = np.zeros((1, 8, 32, 4, 128), np.float32)
    v_sample = np.zeros((1, 8, 32, 4, 128), np.float32)
    ret_sample = np.zeros((1, 8, 4, 128, 128), np.float32)
    for c in range(8):
        b, r = c // 4, c % 4
        shift = 3 - r
        for oi, s in enumerate(OWN):
            t0 = s * 128 - shift * 512
            y_prompt[b, t0:t0 + 128] = R[c]["y_own"][oi * 128:(oi + 1) * 128]
            k_prompt[0, b, t0:t0 + 128] = R[c]["k_own"][oi * 128:(oi + 1) * 128].reshape(128, 4, 128)
            v_prompt[0, b, t0:t0 + 128] = R[c]["v_own"][oi * 128:(oi + 1) * 128].reshape(128, 4, 128)
        if r == 3:
            ret_prompt[0, b] = R[c]["ret_p"]
        y_sample[c] = R[c]["y_s"]
        k_sample[0, c] = R[c]["k_s"].reshape(32, 4, 128)
        v_sample[0, c] = R[c]["v_s"].reshape(32, 4, 128)
        ret_sample[0, c] = R[c]["ret_s"]
    return (y_prompt, y_sample, k_prompt, v_prompt, ret_prompt, k_sample, v_sample, ret_sample)
```

```python
import math
from contextlib import ExitStack
from concourse.bass_utils import run_bass_kernel_spmd

import numpy as np
import concourse.bass as bass
import concourse.mybir as mybir

F32 = mybir.dt.float32
BF16 = mybir.dt.bfloat16
I32 = mybir.dt.int32
U32 = mybir.dt.uint32
U16 = mybir.dt.uint16
ALU = mybir.AluOpType
AF = mybir.ActivationFunctionType
AX = mybir.AxisListType

SEM_WINDOW = 12000
SAME_ENGINE_SYNC = True


class Obj:
    __slots__ = ("name", "w", "r", "excl")

    def __init__(self, name):
        self.name = name
        self.w = None
        self.r = {}
        self.excl = False


class Prog:
    ENGS = ("pe", "act", "dve", "pool", "sp")

    def __init__(self, nc, stack):
        self.nc = nc
        self.stack = stack
        self.cnt = {e: 0 for e in self.ENGS}
        self.ops = {e: [] for e in self.ENGS}
        self.waited = {e: {} for e in self.ENGS}
        self.esems = {}
        self.dsems = {}
        self.dcount = {}
        self.nsem = 0
        self.keymap = {}
        self.next_phys = 0

    def _esem(self, eng, k):
        key = (eng, k)
        if key not in self.esems:
            self.esems[key] = self.stack.enter_context(self.nc.semaphore(f"s_{eng}_{k}"))
            self.nsem += 1
        return self.esems[key]

    def _dkey(self, key):
        if key not in self.keymap:
            name = f"D{self.next_phys}"
            self.next_phys += 1
            if name not in self.dsems:
                self.dsems[name] = self.stack.enter_context(self.nc.semaphore(f"d_{name}"))
                self.dcount[name] = 0
                self.nsem += 1
            self.keymap[key] = name
        return self.keymap[key]

    def _deps(self, eng, reads, writes):
        deps = {}

        def add(src, val):
            if val > deps.get(src, 0):
                deps[src] = val

        for o in reads:
            if o.w is not None:
                add(*o.w)
            if o.excl:
                for s, v in o.r.items():
                    if s != eng:
                        add(s, v)
        for o in writes:
            if o.w is not None:
                add(*o.w)
            for s, v in o.r.items():
                add(s, v)
        waits = []
        for src, val in deps.items():
            if src == eng:
                if eng == "pe" or not SAME_ENGINE_SYNC:
                    continue
            if self.waited[eng].get(src, 0) >= val:
                continue
            self.waited[eng][src] = val
            if src in self.ENGS:
                k = (val - 1) // SEM_WINDOW
                waits.append((self._esem(src, k), (val - 1) % SEM_WINDOW + 1))
            else:
                waits.append((self.dsems[src], val))
        return waits

    def _mark(self, ev, reads, writes):
        src, val = ev
        for o in reads:
            if val > o.r.get(src, 0):
                o.r[src] = val
        for o in writes:
            o.w = ev
            o.r = {}

    def op(self, eng, fn, reads=(), writes=()):
        waits = self._deps(eng, reads, writes)
        n = self.cnt[eng] + 1
        self.cnt[eng] = n
        k = (n - 1) // SEM_WINDOW
        sem = self._esem(eng, k)
        self.ops[eng].append((waits, fn, sem, 1))
        self._mark((eng, n), reads, writes)

    def dma(self, q, fn, reads=(), writes=(), key=None):
        waits = self._deps(q, reads, writes)
        name = self._dkey(key)
        sem = self.dsems[name]
        self.dcount[name] += 16
        self.ops[q].append((waits, fn, sem, 16))
        self._mark((name, self.dcount[name]), reads, writes)

    def wait_all_dma(self, q="sp"):
        waits = []
        for key, sem in self.dsems.items():
            if self.dcount[key] > self.waited[q].get(key, 0):
                waits.append((sem, self.dcount[key]))
        self.ops[q].append((waits, None, None, 0))

    def emit(self):
        nc = self.nc
        handles = {"pe": "tensor", "act": "scalar", "dve": "vector", "pool": "gpsimd", "sp": "sync"}
        with nc.Block() as block:
            for eng in self.ENGS:
                lst = self.ops[eng]

                def body(e, lst=lst):
                    for waits, fn, sem, inc in lst:
                        for s, v in waits:
                            e.wait_ge(s, v)
                        if fn is not None:
                            ins = fn(e)
                            ins.then_inc(sem, inc)

                getattr(block, handles[eng])(body)


def _barrier(self):
    snap = [(s, v) for s, v in list(self.cnt.items()) + list(self.dcount.items()) if v > 0]
    for e in self.ENGS:
        waits = []
        for src, val in snap:
            if src == e and e in ("pe", "sp"):
                continue
            if self.waited[e].get(src, 0) >= val:
                continue
            self.waited[e][src] = val
            if src in self.ENGS:
                k = (val - 1) // SEM_WINDOW
                waits.append((self._esem(src, k), (val - 1) % SEM_WINDOW + 1))
            else:
                waits.append((self.dsems[src], val))
        self.ops[e].append((waits, None, None, 0))
    self.keymap = {}
    self.next_phys = 0


Prog.barrier = _barrier


D = 1024
NT = 128
OWN = [16 * i + 12 + j for i in range(8) for j in range(4)]
HA = 4
EPS = 1e-6
QA, KA, VA, QR, KR, VR, GR, GA, GB = 0, 512, 1024, 1536, 2048, 2560, 3072, 3584, 4608
LAM_INIT = 0.8 - 0.6 * math.exp(-0.3 * 0)
GAMMAS = [1.0 - 2.0 ** (-5.0 - h) for h in range(4)]
R0 = 511
NEG = -30000.0
WB = 49152
AR = 39936
import os
STOP = os.environ.get("MK_STOP", "")


class T:
    def __init__(self, ap, name):
        self.t = ap
        self.o = Obj(name)


def _prod(s):
    r = 1
    for v in s:
        r *= v
    return r


class Carver:
    def __init__(self, buf, size):
        self.buf = buf
        self.size = size
        self.off = 0

    def reset(self):
        self.off = 0

    def get(self, shape, dt, name):
        n = _prod(shape[1:])
        sz = n * (2 if dt in (F32, I32, U32) else 1)
        sz = (sz + 15) // 16 * 16
        assert self.off + sz <= self.size, (name, self.off, sz, self.size)
        v = self.buf[0:shape[0], self.off:self.off + sz]
        self.off += sz
        if dt != BF16:
            v = v.bitcast(dt)
        v = v[:, 0:n]
        if len(shape) == 3:
            v = v.rearrange("p (a b) -> p a b", a=shape[1])
        elif len(shape) == 4:
            v = v.rearrange("p (a b c) -> p a b c", a=shape[1], b=shape[2])
        return T(v, name)


def bcast(ap2, dims):
    base = ap2.ap
    new = [base[0]]
    for d in dims:
        if d[0] == 'b':
            new.append((0, d[1]))
        else:
            new.append(base[1])
    return bass.AP(ap2.tensor, ap2.offset, tuple(new))


def build_nc():
    nc = bass.Bass("TRN2", target_bir_lowering=False)
    din = lambda name, shape, dt=F32: nc.dram_tensor(name, list(shape), dt, kind="ExternalInput")
    dout = lambda name, shape, dt=F32: nc.dram_tensor(name, list(shape), dt, kind="ExternalOutput")
    dscr = lambda name, shape, dt: nc.dram_tensor(name, list(shape), dt, kind="Internal")

    x_all = din("x_all", [NT * 128, D]).ap()
    x_s = din("x_s", [32, D]).ap()
    cache_k = din("cache_k", [1024, 512]).ap()
    cache_v = din("cache_v", [1024, 512]).ap()
    state_in = din("state_in", [4, 128, 128]).ap()
    c_both = din("c_both", [2, D]).ap()
    w_ada = din("w_ada", [D, 6 * D]).ap()
    b_ada = din("b_ada", [6 * D])
    norm1 = din("norm1", [D]).ap()
    norm2 = din("norm2", [D]).ap()
    w_in = din("w_in", [D, 5632]).ap()
    lamv = din("lamv", [4, 64])
    subln = din("subln", [2, 128]).ap()
    w_ba = din("w_ba", [512, D]).ap()
    w_br = din("w_br", [512, D]).ap()
    w_o = din("w_o", [D, D]).ap()
    rel_bias = din("rel_bias", [32, 4])
    w_pq = din("w_pq", [D, 2048]).ap()
    peer_keys = din("peer_keys", [16, 128, 128]).ap()
    peer_u = din("peer_u", [16384, D])
    peer_v = din("peer_v", [16384, D])
    final_norm = din("final_norm", [D])
    rot = din("rot", [NT + 1, 128, 256]).ap()
    dtab_d = din("dtab", [128, 1296]).ap()
    mcol_d = din("mcol", [128, 1024]).ap()
    vsv_d = din("vsv", [128, 3]).ap()
    oht5 = din("oht5", [32, 1280]).ap()
    jrev_d = din("jrev", [128, 128]).ap()
    ident_d = din("ident", [128, 128]).ap()
    zwin_d = din("zwin", [128, 256]).ap()
    iota_d = din("iota16", [128, 16]).ap()

    y_own = dout("y_own", [4096, D]).ap()
    k_own = dout("k_own", [4096, 512]).ap()
    v_own = dout("v_own", [4096, 512]).ap()
    ret_p = dout("ret_p", [4, 128, 128]).ap()
    y_s = dout("y_s", [32, D]).ap()
    k_s = dout("k_s", [32, 512]).ap()
    v_s = dout("v_s", [32, 512]).ap()
    ret_s = dout("ret_s", [4, 128, 128]).ap()

    Kscr = dscr("Kscr", [4, 128, 16384], BF16).ap()
    Vscr = dscr("Vscr", [4, 128, 128, 128], BF16).ap()
    Qscr = dscr("Qscr", [4, 128, 4096], BF16).ap()
    YAscr = dscr("YAscr", [4, 128, 4096], BF16).ap()
    YRscr = dscr("YRscr", [4, 128, 4096], BF16).ap()
    KSs = dscr("KSs", [4, 128, 1152], BF16).ap()
    VSs = dscr("VSs", [4, 128, 9, 128], BF16).ap()
    UVbf = dscr("UVbf", [16384, 2, D], BF16)
    H2scr = dscr("H2scr", [33 * 128, D], BF16)
    modscr = dscr("modscr", [2, 6 * D], F32)
    Gscr = dscr("Gscr", [4, 1280], F32)
    OK_, OV_, OQ_, OYA, OYR, OKS, OVS, OMOD, OG = [Obj(n) for n in
                                                  ("Kscr", "Vscr", "Qscr", "YAscr", "YRscr", "KSs", "VSs", "modscr", "Gscr")]
    Oout = Obj("outputs")

    st = ExitStack()
    with st:
        P = Prog(nc, st)
        op, dma = P.op, P.dma
        sbt = lambda name, shape, dt: T(st.enter_context(nc.sbuf_tensor("sb_" + name, list(shape), dt))[:], name)
        wbuf_t = st.enter_context(nc.sbuf_tensor("wbuf", [128, WB], BF16))
        arena_t = st.enter_context(nc.sbuf_tensor("arena", [128, AR], BF16))
        Owb = Obj("wbuf_weights")
        s0 = T(st.enter_context(nc.psum_tensor("ps0", [128, 512], F32))[:], "ps0")
        s7 = T(st.enter_context(nc.psum_tensor("ps7", [128, 512], F32))[:], "ps7")
        pairs = []
        for nm in "ABC":
            pt = st.enter_context(nc.psum_tensor("pp" + nm, [128, 1024], F32))
            pairs.append((pt, T(pt[:, 0:512], nm + "0"), T(pt[:, 512:1024], nm + "1")))
        (pA, pA0, pA1), (pB, pB0, pB1), (pC, pC0, pC1) = pairs
        for _b in (s0, s7, pA0, pA1, pB0, pB1, pC0, pC1):
            _b.o.excl = True

        identb = sbt("identb", [128, 128], BF16)
        identf = sbt("identf", [128, 128], F32)
        jrev = sbt("jrev", [128, 128], F32)
        onesb = sbt("onesb", [128, 128], BF16)
        zwin = sbt("zwin", [128, 256], F32)
        iota16 = sbt("iota16", [128, 16], F32)
        epsc = sbt("epsc", [128, 1], F32)
        cols = sbt("cols", [128, 2, 6, 8], F32)
        ncols = sbt("ncols", [128, 2, 8], F32)
        AB = sbt("AB", [128, 2, 4, 8], F32)
        ABv = sbt("ABv", [128, 3, 2, 8], F32)
        vsv = sbt("vsv", [128, 3], F32)
        lamc = sbt("lamc", [128, 8], F32)
        sub = sbt("sub", [128, 2], F32)
        G1 = sbt("G1", [128, D], F32)
        G2 = sbt("G2", [128, D], F32)
        FN = sbt("FN", [128, D], F32)
        S = sbt("S", [128, 4, 128], F32)
        Sb = sbt("Sb", [128, 4, 128], BF16)
        Ss = sbt("Ss", [128, 4, 128], F32)
        Ssb = sbt("Ssb", [128, 4, 128], BF16)
        keysT = sbt("keysT", [128, 16, 128], BF16)
        dtab = sbt("dtab", [128, 1296], F32)
        qs_T = sbt("qs_T", [128, 4, 32], BF16)
        yas_T = sbt("yas_T", [128, 4, 32], BF16)
        yrs_T = sbt("yrs_T", [128, 4, 32], BF16)

        A = Carver(arena_t, AR)
        W = Carver(wbuf_t, WB)

        mult, add, sub_ = ALU.mult, ALU.add, ALU.subtract

        def ld(dst, src, key, reads=()):
            dma("sp", lambda e: e.dma_start(out=dst.t if isinstance(dst, T) else dst, in_=src),
                reads=list(reads), writes=[dst.o], key=key)

        def ld_slow(dst_ap, dst_obj, src, key, reads=()):
            dma("sp", lambda e: e.dma_start(out=dst_ap, in_=src, allow_slow_non_contiguous=True),
                reads=list(reads), writes=[dst_obj], key=key)

        ld(identf, ident_d, "c0")
        ld(jrev, jrev_d, "c1")
        ld(zwin, zwin_d, "c2")
        ld(iota16, iota_d, "c3")
        ld(dtab, dtab_d, "c4")
        ld(vsv, vsv_d, "c5")
        op("pool", lambda e: e.memset(onesb.t[:], 1.0), writes=[onesb.o])
        op("pool", lambda e: e.memset(epsc.t[:], EPS), writes=[epsc.o])
        op("dve", lambda e: e.tensor_copy(out=identb.t[:], in_=identf.t[:]), reads=[identf.o], writes=[identb.o])
        op("pool", lambda e: e.memset(S.t[:], 0.0), writes=[S.o])
        op("pool", lambda e: e.memset(Sb.t[:], 0.0), writes=[Sb.o])

        A.reset()
        cT = A.get([128, 8, 2], F32, "cT")
        for g in range(2):
            ld_slow(cT.t[:, :, g], cT.o, c_both[g].rearrange("(k p) -> p k", p=128), "cT")
        op("act", lambda e: e.activation(out=cT.t[:], in_=cT.t[:], func=AF.Silu), reads=[cT.o], writes=[cT.o])
        wst = [A.get([128, 8, 512], F32, f"wst{i}") for i in range(2)]
        modsb = W.get([2, 6 * D], F32, "modsb")
        badd = W.get([2, 6 * D], F32, "badd")
        ld(badd, bass.AP(b_ada, 0, ((0, 2), (1, 6 * D))), "badd")
        w_ada_v = w_ada.rearrange("(k p) n -> p k n", p=128)
        for cg in range(12):
            ws = wst[cg % 2]
            ld(ws, w_ada_v[:, :, cg * 512:(cg + 1) * 512], f"wst{cg % 2}")
            bank = pA0 if cg % 2 == 0 else pA1
            for k in range(8):
                op("pe", lambda e, k=k, ws=ws, bank=bank: e.matmul(bank.t[0:2, :], lhsT=cT.t[:, k, :], rhs=ws.t[:, k, :],
                                                                   start=(k == 0), stop=(k == 7)),
                   reads=[cT.o, ws.o], writes=[bank.o])
            op("dve", lambda e, cg=cg, bank=bank: e.tensor_tensor(out=modsb.t[:, cg * 512:(cg + 1) * 512], in0=bank.t[0:2, :],
                                                                  in1=badd.t[:, cg * 512:(cg + 1) * 512], op=add),
               reads=[bank.o, badd.o], writes=[modsb.o])
        dma("pool", lambda e: e.dma_start(out=modscr.ap(), in_=modsb.t[:]), reads=[modsb.o], writes=[OMOD], key="modscr")
        for g in range(2):
            for v in range(6):
                src = bass.AP(modscr, g * 6 * D + v * D, ((1, 128), (128, 8)))
                ld_slow(cols.t[:, g, v, :], cols.o, src, "cols", reads=[OMOD])
        ld_slow(ncols.t[:, 0, :], ncols.o, norm1.rearrange("(k p) -> p k", p=128), "ncols")
        ld_slow(ncols.t[:, 1, :], ncols.o, norm2.rearrange("(k p) -> p k", p=128), "ncols")
        for g in range(2):
            for j, (vsc, vsh) in enumerate(((1, 0), (4, 3))):
                op("dve", lambda e, g=g, j=j, vsc=vsc: e.scalar_tensor_tensor(
                    out=AB.t[:, g, 2 * j, :], in0=cols.t[:, g, vsc, :], scalar=1.0, in1=ncols.t[:, j, :], op0=add, op1=mult),
                   reads=[cols.o, ncols.o], writes=[AB.o])
                op("dve", lambda e, g=g, j=j, vsh=vsh: e.tensor_copy(out=AB.t[:, g, 2 * j + 1, :], in_=cols.t[:, g, vsh, :]),
                   reads=[cols.o], writes=[AB.o])
        for gi in range(3):
            for j in range(2):
                op("dve", lambda e, gi=gi, j=j: e.tensor_scalar(out=ABv.t[:, gi, j, :], in0=AB.t[:, 0, j, :],
                                                                scalar1=vsv.t[:, gi:gi + 1], scalar2=None, op0=mult),
                   reads=[AB.o, vsv.o], writes=[ABv.o])

        def load_rows(g):
            ld(G1, bass.AP(modscr, g * 6 * D + 2 * D, ((0, 128), (1, D))), "G1", reads=[OMOD])
            ld(G2, bass.AP(modscr, g * 6 * D + 5 * D, ((0, 128), (1, D))), "G2", reads=[OMOD])

        ld(FN, bass.AP(final_norm, 0, ((0, 128), (1, D))), "FN")
        lq = A.get([128, 4, 64], F32, "lq")
        ld(lq, bass.AP(lamv, 0, ((0, 128), (1, 256))), "lq")
        ljunk = A.get([128, 64], F32, "ljunk")
        for i in range(2):
            op("dve", lambda e, i=i: e.scalar_tensor_tensor(out=ljunk.t[:], in0=lq.t[:, 2 * i, :], scalar=1.0, in1=lq.t[:, 2 * i + 1, :],
                                                            op0=mult, op1=mult, accum_out=lamc.t[:, i:i + 1]),
               reads=[lq.o], writes=[ljunk.o, lamc.o])
        op("act", lambda e: e.activation(out=lamc.t[:, 2:4], in_=lamc.t[:, 0:2], func=AF.Exp), reads=[lamc.o], writes=[lamc.o])
        op("dve", lambda e: e.tensor_tensor(out=lamc.t[:, 4:5], in0=lamc.t[:, 3:4], in1=lamc.t[:, 2:3], op=sub_),
           reads=[lamc.o], writes=[lamc.o])
        op("dve", lambda e: e.tensor_scalar(out=lamc.t[:, 5:6], in0=lamc.t[:, 4:5], scalar1=-LAM_INIT, scalar2=None, op0=add),
           reads=[lamc.o], writes=[lamc.o])
        nlam = lamc.t[:, 5:6]
        ld_slow(sub.t[:, :], sub.o, subln.rearrange("a p -> p a"), "sub")
        op("dve", lambda e: e.tensor_scalar(out=sub.t[:, 0:1], in0=sub.t[:, 0:1], scalar1=1.0 - LAM_INIT, scalar2=None, op0=mult),
           reads=[sub.o], writes=[sub.o])
        rb = A.get([32, 4], F32, "rb")
        rb15 = A.get([32, 4], F32, "rb15")
        oh = A.get([32, 1280], F32, "oh")
        gsb = A.get([4, 1280], F32, "gsb")
        ld(rb, rel_bias.ap(), "rb")
        ld(rb15, bass.AP(rel_bias, 15 * 4, ((0, 32), (1, 4))), "rb15")
        ld(oh, oht5, "oh")
        op("dve", lambda e: e.tensor_tensor(out=rb.t[:], in0=rb.t[:], in1=rb15.t[:], op=sub_), reads=[rb.o, rb15.o], writes=[rb.o])
        for i, (a, b) in enumerate(((0, 512), (512, 1024), (1024, 1280))):
            op("pe", lambda e, a=a, b=b: e.matmul(pB0.t[0:4, 0:b - a], lhsT=rb.t[:, :], rhs=oh.t[:, a:b], start=True, stop=True),
               reads=[rb.o, oh.o], writes=[pB0.o])
            op("act", lambda e, a=a, b=b: e.copy(out=gsb.t[:, a:b], in_=pB0.t[0:4, 0:b - a]), reads=[pB0.o], writes=[gsb.o])
        dma("pool", lambda e: e.dma_start(out=Gscr.ap(), in_=gsb.t[:]), reads=[gsb.o], writes=[OG], key="gscr")
        kst = W.get([128, 16, 128], F32, "kst")
        ksb = W.get([128, 16, 128], BF16, "ksb")
        ld(kst, peer_keys.rearrange("c n d -> n c d"), "kst")
        op("pool", lambda e: e.tensor_copy(out=ksb.t[:], in_=kst.t[:]), reads=[kst.o], writes=[ksb.o])
        s0b = s0.t[:].bitcast(BF16)
        for c4 in range(4):
            for j in range(4):
                c = c4 * 4 + j
                op("pe", lambda e, c=c, j=j: e.transpose(out=s0b[:, j * 128:(j + 1) * 128], in_=ksb.t[:, c, :], identity=identb.t[:]),
                   reads=[ksb.o, identb.o], writes=[s0.o])
            op("dve", lambda e, c4=c4: e.tensor_copy(out=keysT.t[:, c4 * 4:(c4 + 1) * 4, :],
                                                     in_=s0b[:, 0:512].rearrange("p (a b) -> p a b", a=4)),
               reads=[s0.o], writes=[keysT.o])
        cvi = [W.get([128, 2, D], F32, f"cvi{i}") for i in range(3)]
        cvo = [W.get([128, 2, D], BF16, f"cvo{i}") for i in range(3)]
        ci_ = 0
        for ti_, tab in enumerate((peer_u, peer_v)):
            tv = tab.ap().rearrange("(p r) d -> p r d", p=128)
            dv = UVbf.ap().rearrange("(p r) t d -> p r t d", p=128)[:, :, ti_, :]
            for rr in range(0, 128, 2):
                a_, b_ = cvi[ci_ % 3], cvo[ci_ % 3]
                ld(a_, tv[:, rr:rr + 2, :], a_.o.name)
                eng = ("dve", "act", "pool")[ci_ % 3]
                if eng == "act":
                    op("act", lambda e, a_=a_, b_=b_: e.copy(out=b_.t[:], in_=a_.t[:]), reads=[a_.o], writes=[b_.o])
                else:
                    op(eng, lambda e, a_=a_, b_=b_: e.tensor_copy(out=b_.t[:], in_=a_.t[:]), reads=[a_.o], writes=[b_.o])
                dma("pool" if ci_ % 2 == 0 else "sp", lambda e, b_=b_, dv=dv, rr=rr: e.dma_start(out=dv[:, rr:rr + 2, :], in_=b_.t[:]),
                    reads=[b_.o], key=b_.o.name)
                ci_ += 1
        P.barrier()
        if STOP == "p0":
            P.wait_all_dma("sp")
            P.emit()
            return nc

        def load_weights(specs, stage):
            for i, (dst, src, K, n) in enumerate(specs):
                sg = stage[i % len(stage)]
                dma("sp", lambda e, sg=sg, src=src, K=K, n=n: e.dma_start(out=sg.t[:, 0:K, 0:n], in_=src),
                    writes=[sg.o], key=sg.o.name)
                eng = "pool" if i % 2 == 0 else "dve"
                op(eng, lambda e, sg=sg, dst=dst, K=K, n=n: e.tensor_copy(out=dst, in_=sg.t[:, 0:K, 0:n]),
                   reads=[sg.o], writes=[Owb])

        def wspecs(dst3, src2, K, ncol, src_cols=None, piece=256):
            out = []
            sv = src2.rearrange("(k p) n -> p k n", p=128)
            for a in range(0, ncol, piece):
                n = min(piece, ncol - a)
                out.append((dst3[:, :, a:a + n], sv[:, :, src_cols + a:src_cols + a + n], K, n))
            return out

        A.reset()
        W.reset()
        Wall = W.get([128, 8, 2560], BF16, "Wall").t
        Wown = W.get([128, 8, 2048], BF16, "Wown").t
        stage = [A.get([128, 8, 256], F32, f"stg{i}") for i in range(2)]
        specs = []
        specs += wspecs(Wall[:, :, 0:512], w_in, 8, 512, KA)
        specs += wspecs(Wall[:, :, 512:1024], w_in, 8, 512, KR)
        specs += wspecs(Wall[:, :, 1536:2048], w_in, 8, 512, VA)
        specs += wspecs(Wall[:, :, 2048:2560], w_in, 8, 512, VR)
        specs += wspecs(Wown[:, :, 0:512], w_in, 8, 512, QA)
        specs += wspecs(Wown[:, :, 512:1024], w_in, 8, 512, QR)
        specs += wspecs(Wown[:, :, 1536:2048], w_in, 8, 512, GR)
        for h in range(4):
            specs += wspecs(Wall[:, :, 1024 + h * 128:1024 + h * 128 + 64], w_in, 8, 64, KR + h * 128 + 64)
            specs += wspecs(Wall[:, :, 1024 + h * 128 + 64:1024 + h * 128 + 128], w_in, 8, 64, KR + h * 128)
            specs += wspecs(Wown[:, :, 1024 + h * 128:1024 + h * 128 + 64], w_in, 8, 64, QR + h * 128 + 64)
            specs += wspecs(Wown[:, :, 1024 + h * 128 + 64:1024 + h * 128 + 128], w_in, 8, 64, QR + h * 128)
        load_weights(specs, stage)

        xs = [A.get([128, D], F32, f"xs{i}") for i in range(2)]
        F1 = A.get([128, D], F32, "F1")
        xn = A.get([128, D], BF16, "xn")
        hTs = [A.get([128, 8, 128], BF16, f"hT{i}") for i in range(2)]
        ss = A.get([128, 4], F32, "ss")
        rots = [A.get([128, 256], F32, f"rot{i}") for i in range(2)]
        kaT = [A.get([128, 4, 128], BF16, f"kaT{i}") for i in range(2)]
        vab = [A.get([128, 512], BF16, f"vab{i}") for i in range(2)]
        kout = [A.get([128, 512], F32, f"kout{i}") for i in range(2)]
        vout = [A.get([128, 512], F32, f"vout{i}") for i in range(2)]
        t1 = A.get([128, 4, 128], F32, "t1")
        t2 = A.get([128, 4, 128], F32, "t2")
        krot = A.get([128, 4, 128], BF16, "krot")
        krz = A.get([128, 4, 128], BF16, "krz")
        vrb = A.get([128, 512], BF16, "vrb")
        qb = [A.get([128, 4, 128], BF16, f"qb{i}") for i in range(2)]
        qrf = A.get([128, 4, 128], F32, "qrf")
        qrb = A.get([128, 4, 128], BF16, "qrb")
        qxb = A.get([128, 4, 128], BF16, "qxb")
        PTr = A.get([128, 4, 128], BF16, "PTr")
        orr = A.get([128, 4, 128], F32, "orr")
        sq = A.get([128, 512], BF16, "sq")
        sd = A.get([128, 512], F32, "sd")
        gsl = A.get([128, 4, 128], F32, "gsl")
        yrb = [A.get([128, 4, 128], BF16, f"yrb{i}") for i in range(2)]
        ckf = A.get([128, 512], F32, "ckf")
        ckb = A.get([128, 512], BF16, "ckb")

        def norm_transpose(xt, n, Acol, Bcol, AcolO, hT):
            op("act", lambda e: e.activation(out=F1.t[0:n, :], in_=xt.t[0:n, :], func=AF.Square, accum_out=ss.t[0:n, 0:1]),
               reads=[xt.o], writes=[F1.o, ss.o])
            op("act", lambda e: e.activation(out=ss.t[0:n, 1:2], in_=ss.t[0:n, 0:1], func=AF.Sqrt, scale=1.0 / D, bias=epsc.t[0:n, :]),
               reads=[ss.o, epsc.o], writes=[ss.o])
            op("dve", lambda e: e.reciprocal(out=ss.t[0:n, 2:3], in_=ss.t[0:n, 1:2]), reads=[ss.o], writes=[ss.o])
            op("dve", lambda e: e.tensor_scalar(out=xn.t[0:n, :], in0=xt.t[0:n, :], scalar1=ss.t[0:n, 2:3], scalar2=None, op0=mult),
               reads=[xt.o, ss.o], writes=[xn.o])
            for c in range(8):
                op("pe", lambda e, c=c: e.transpose(out=s0b[:, c * 128:c * 128 + n], in_=xn.t[0:n, c * 128:(c + 1) * 128],
                                                    identity=identb.t[0:n, 0:n]),
                   reads=[xn.o, identb.o], writes=[s0.o])
            for c in range(8):
                op("dve" if c % 2 == 0 else "act",
                   (lambda e, c=c: e.tensor_scalar(out=hT.t[:, c, 0:n], in0=s0b[:, c * 128:c * 128 + n],
                                                   scalar1=Acol[:, c:c + 1], scalar2=Bcol[:, c:c + 1], op0=mult, op1=add))
                   if c % 2 == 0 else
                   (lambda e, c=c: e.activation(out=hT.t[:, c, 0:n], in_=s0b[:, c * 128:c * 128 + n], func=AF.Identity,
                                                scale=Acol[:, c:c + 1], bias=Bcol[:, c:c + 1])),
                   reads=[s0.o, AcolO], writes=[hT.o])

        def fm_proj(bank, Wt, col0, nchunks, hT, n, reads_extra=()):
            for j in range(nchunks):
                for k in range(8):
                    op("pe", lambda e, j=j, k=k: e.matmul(bank.t[:, j * 128:j * 128 + n],
                                                          lhsT=Wt[:, k, col0 + j * 128:col0 + (j + 1) * 128],
                                                          rhs=hT.t[:, k, 0:n], start=(k == 0), stop=(k == 7)),
                       reads=[hT.o, Owb], writes=[bank.o])

        def tm_proj(bank, Wt, col0, hT, n):
            for k in range(8):
                op("pe", lambda e, k=k: e.matmul(bank.t[0:n, :], lhsT=hT.t[:, k, 0:n], rhs=Wt[:, k, col0:col0 + 512],
                                                 start=(k == 0), stop=(k == 7)),
                   reads=[hT.o, Owb], writes=[bank.o])

        def v4(bank, n):
            return bank.t[:, :].rearrange("p (a b) -> p a b", a=4)[:, :, 0:n]

        def rotate(bx, bxs, rt, n, out_f32):
            Cb = bcast(rt.t[:, 0:n], [('b', 4), ('x',)])
            Sb_ = bcast(rt.t[:, 128:128 + n], [('b', 4), ('x',)])
            op("dve", lambda e: e.tensor_tensor(out=t1.t[:, :, 0:n], in0=v4(bx, n), in1=Cb, op=mult),
               reads=[bx.o, rt.o], writes=[t1.o])
            op("dve", lambda e: e.tensor_tensor(out=t2.t[:, :, 0:n], in0=v4(bxs, n), in1=Sb_, op=mult),
               reads=[bxs.o, rt.o], writes=[t2.o])
            op("pool", lambda e: e.tensor_tensor(out=out_f32.t[:, :, 0:n], in0=t1.t[:, :, 0:n], in1=t2.t[:, :, 0:n], op=add),
               reads=[t1.o, t2.o], writes=[out_f32.o])

        def p1_tile(s, n, own_idx, sample):
            slot = s % 2
            xt = xs[slot]
            hT = hTs[slot]
            rt = rots[slot]
            g = 1 if sample else 0
            if sample:
                dma("sp", lambda e: e.dma_start(out=xt.t[0:32, :], in_=x_s), writes=[xt.o], key=f"xs{slot}")
            else:
                ld(xt, x_all[s * 128:(s + 1) * 128, :], f"xs{slot}")
            ld(rt, rot[NT if sample else s], f"rot{slot}")
            if (not sample) and s < 12:
                Acol, Bcol, AO = ABv.t[:, s // 4, 0, :], ABv.t[:, s // 4, 1, :], ABv.o
            else:
                Acol, Bcol, AO = AB.t[:, g, 0, :], AB.t[:, g, 1, :], AB.o
            norm_transpose(xt, n, Acol, Bcol, AO, hT)
            St, Sbt = (Ss, Ssb) if sample else (S, Sb)
            dto = 1032 if sample else 0
            xio = 1160 if sample else 512
            zeo = 1288 if sample else 1024
            dtw = 32 if sample else 128
            fm_proj(pA0, Wall, 0, 4, hT, n)
            kt = kaT[slot]
            op("act", lambda e: e.copy(out=kt.t[:, :, 0:n], in_=v4(pA0, n)), reads=[pA0.o], writes=[kt.o])
            if sample:
                dma("pool", lambda e: e.dma_start(out=KSs[:, :, 1024:1056].rearrange("h p t -> p h t"), in_=kt.t[:, :, 0:32]),
                    reads=[kt.o], key=f"kaT{slot}")
            else:
                dma("pool", lambda e: e.dma_start(out=Kscr[:, :, s * 128:(s + 1) * 128].rearrange("h p t -> p h t"), in_=kt.t[:]),
                    reads=[kt.o], key=f"kaT{slot}")
            tm_proj(pB1, Wall, 1536, hT, n)
            vb_ = vab[slot]
            op("act", lambda e: e.copy(out=vb_.t[0:n, :], in_=pB1.t[0:n, :]), reads=[pB1.o], writes=[vb_.o])
            if sample:
                dma("pool", lambda e: e.dma_start(out=VSs[:, 0:32, 8, :].rearrange("h p d -> p h d"),
                                                  in_=vb_.t[0:32, :].rearrange("p (h d) -> p h d", h=4)),
                    reads=[vb_.o], key=f"vab{slot}")
            else:
                dma("pool", lambda e: e.dma_start(out=Vscr[:, :, s, :].rearrange("h p d -> p h d"),
                                                  in_=vb_.t[:, :].rearrange("p (h d) -> p h d", h=4)),
                    reads=[vb_.o], key=f"vab{slot}")
            if own_idx is not None:
                vo = vout[own_idx % 2]
                op("dve", lambda e: e.tensor_copy(out=vo.t[0:n, :], in_=pB1.t[0:n, :]), reads=[pB1.o], writes=[vo.o])
                dst = v_s if sample else v_own[own_idx * 128:(own_idx + 1) * 128, :]
                dma("pool", lambda e, dst=dst: e.dma_start(out=dst, in_=vo.t[0:n, :]), reads=[vo.o], key=f"vout{own_idx % 2}")
                tm_proj(pC1, Wall, 0, hT, n)
                ko = kout[own_idx % 2]
                op("act", lambda e: e.copy(out=ko.t[0:n, :], in_=pC1.t[0:n, :]), reads=[pC1.o], writes=[ko.o])
                dst = k_s if sample else k_own[own_idx * 128:(own_idx + 1) * 128, :]
                dma("pool", lambda e, dst=dst: e.dma_start(out=dst, in_=ko.t[0:n, :]), reads=[ko.o], key=f"kout{own_idx % 2}")
            fm_proj(pA1, Wall, 512, 4, hT, n)
            fm_proj(pB0, Wall, 1024, 4, hT, n)
            rotate(pA1, pB0, rt, n, qrf)
            op("act", lambda e: e.copy(out=krot.t[:, :, 0:n], in_=qrf.t[:, :, 0:n]), reads=[qrf.o], writes=[krot.o])
            for h in range(4):
                op("pe", lambda e, h=h: e.transpose(out=s0b[0:n, h * 128:(h + 1) * 128], in_=krot.t[:, h, 0:n], identity=identb.t[:]),
                   reads=[krot.o, identb.o], writes=[s0.o])
            for h in range(4):
                op("dve", lambda e, h=h: e.tensor_scalar(out=krz.t[0:n, h, :], in0=s0b[0:n, h * 128:(h + 1) * 128],
                                                         scalar1=dtab.t[0:n, zeo + h:zeo + h + 1], scalar2=None, op0=mult),
                   reads=[s0.o, dtab.o], writes=[krz.o])
            tm_proj(pC0, Wall, 2048, hT, n)
            op("act", lambda e: e.copy(out=vrb.t[0:n, :], in_=pC0.t[0:n, :]), reads=[pC0.o], writes=[vrb.o])
            if own_idx is not None and int(os.environ.get("MK_CUT", "99")) >= 1:
                fm_proj(pA0, Wown, 0, 4, hT, n)
                q_ = qb[own_idx % 2]
                if sample:
                    op("act", lambda e: e.activation(out=qs_T.t[:, :, :], in_=v4(pA0, 32), func=AF.Copy, scale=0.125),
                       reads=[pA0.o], writes=[qs_T.o])
                else:
                    op("act", lambda e: e.activation(out=q_.t[:], in_=v4(pA0, 128), func=AF.Copy, scale=0.125),
                       reads=[pA0.o], writes=[q_.o])
                    dma("pool", lambda e: e.dma_start(out=Qscr[:, :, own_idx * 128:(own_idx + 1) * 128].rearrange("h p t -> p h t"),
                                                      in_=q_.t[:]), reads=[q_.o], key=f"qb{own_idx % 2}")
                if int(os.environ.get("MK_CUT", "99")) >= 2:
                    fm_proj(pA1, Wown, 512, 4, hT, n)
                    fm_proj(pB0, Wown, 1024, 4, hT, n)
                    rotate(pA1, pB0, rt, n, qrf)
                    op("act", lambda e: e.copy(out=qrb.t[:, :, 0:n], in_=qrf.t[:, :, 0:n]), reads=[qrf.o], writes=[qrb.o])
                    XIv = dtab.t[:, xio:xio + 4 * dtw].rearrange("p (a b) -> p a b", a=4)
                    op("pool", lambda e: e.tensor_tensor(out=qxb.t[:, :, 0:n], in0=qrf.t[:, :, 0:n], in1=XIv[:, :, 0:n], op=mult),
                       reads=[qrf.o, dtab.o], writes=[qxb.o])
                    for h in range(4):
                        op("pe", lambda e, h=h: e.matmul(pC1.t[0:n, h * 128:h * 128 + n], lhsT=krot.t[:, h, 0:n], rhs=qrb.t[:, h, 0:n],
                                                         start=True, stop=True), reads=[krot.o, qrb.o], writes=[pC1.o])
                    DTv = dtab.t[:, dto:dto + 4 * dtw].rearrange("p (a b) -> p a b", a=4)
                    op("dve", lambda e: e.tensor_tensor(out=PTr.t[0:n, :, 0:n], in0=v4(pC1, n)[0:n], in1=DTv[0:n, :, 0:n], op=mult),
                       reads=[pC1.o, dtab.o], writes=[PTr.o])
                    for h in range(4):
                        op("pe", lambda e, h=h: e.matmul(s7.t[:, h * 128:h * 128 + n], lhsT=vrb.t[0:n, h * 128:(h + 1) * 128],
                                                         rhs=PTr.t[0:n, h, 0:n], start=True, stop=False),
                           reads=[vrb.o, PTr.o], writes=[s7.o])
                        op("pe", lambda e, h=h: e.matmul(s7.t[:, h * 128:h * 128 + n], lhsT=Sbt.t[:, h, :],
                                                         rhs=qxb.t[:, h, 0:n], start=False, stop=True),
                           reads=[Sbt.o, qxb.o], writes=[s7.o])
                    op("act", lambda e: e.copy(out=orr.t[:, :, 0:n], in_=v4(s7, n)), reads=[s7.o], writes=[orr.o])
                    op("act", lambda e: e.activation(out=sq.t[:, 0:512].rearrange("p (a b) -> p a b", a=4)[:, :, 0:n],
                                                     in_=orr.t[:, :, 0:n], func=AF.Square), reads=[orr.o], writes=[sq.o])
                    for h in range(4):
                        op("pe", lambda e, h=h: e.matmul(pC1.t[:, h * 128:h * 128 + n], lhsT=onesb.t[:, :],
                                                         rhs=sq.t[:, h * 128:h * 128 + n], start=True, stop=True),
                           reads=[onesb.o, sq.o], writes=[pC1.o])
                    sdv = sd.t[:, :].rearrange("p (a b) -> p a b", a=4)[:, :, 0:n]
                    op("act", lambda e: e.activation(out=sdv, in_=v4(pC1, n), func=AF.Sqrt, scale=1.0 / 128, bias=epsc.t[:, :]),
                       reads=[pC1.o, epsc.o], writes=[sd.o])
                    op("dve", lambda e: e.reciprocal(out=sdv, in_=sdv), reads=[sd.o], writes=[sd.o])
                    op("dve", lambda e: e.scalar_tensor_tensor(out=orr.t[:, :, 0:n], in0=orr.t[:, :, 0:n], scalar=sub.t[:, 1:2],
                                                               in1=sdv, op0=mult, op1=mult),
                       reads=[orr.o, sub.o, sd.o], writes=[orr.o])
                    fm_proj(pA0, Wown, 1536, 4, hT, n)
                    op("act", lambda e: e.activation(out=gsl.t[:, :, 0:n], in_=v4(pA0, n), func=AF.Silu), reads=[pA0.o], writes=[gsl.o])
                    if sample:
                        op("pool", lambda e: e.tensor_tensor(out=yrs_T.t[:, :, :], in0=orr.t[:, :, 0:32], in1=gsl.t[:, :, 0:32], op=mult),
                           reads=[orr.o, gsl.o], writes=[yrs_T.o])
                    else:
                        y_ = yrb[own_idx % 2]
                        op("pool", lambda e: e.tensor_tensor(out=y_.t[:], in0=orr.t[:], in1=gsl.t[:], op=mult),
                           reads=[orr.o, gsl.o], writes=[y_.o])
                        dma("pool", lambda e: e.dma_start(out=YRscr[:, :, own_idx * 128:(own_idx + 1) * 128].rearrange("h p t -> p h t"),
                                                          in_=y_.t[:]), reads=[y_.o], key=f"yrb{own_idx % 2}")
            L = 32 if sample else 128
            for h in range(4):
                op("pe", lambda e, h=h: e.matmul(s7.t[:, h * 128:(h + 1) * 128], lhsT=krz.t[0:n, h, :],
                                                 rhs=vrb.t[0:n, h * 128:(h + 1) * 128], start=True, stop=True),
                   reads=[krz.o, vrb.o], writes=[s7.o])
            for h in range(4):
                op("dve", lambda e, h=h: e.scalar_tensor_tensor(out=St.t[:, h, :], in0=St.t[:, h, :], scalar=float(GAMMAS[h] ** L),
                                                                in1=s7.t[:, h * 128:(h + 1) * 128], op0=mult, op1=add),
                   reads=[St.o, s7.o], writes=[St.o])
            op("pool", lambda e: e.tensor_copy(out=Sbt.t[:], in_=St.t[:]), reads=[St.o], writes=[Sbt.o])

        _tiles = [int(v) for v in os.environ["MK_TILES"].split(",")] if os.environ.get("MK_TILES") else list(range(NT))
        for s in _tiles:
            p1_tile(s, 128, OWN.index(s) if s in OWN else None, False)
        dma("pool", lambda e: e.dma_start(out=ret_p.rearrange("h k v -> k h v"), in_=S.t[:]), reads=[S.o], key="retp")

        if os.environ.get("MK_NOSAMPLE"):
            P.barrier()
            P.wait_all_dma("sp")
            P.emit()
            return nc
        ld(Ss, state_in.rearrange("h k v -> k h v"), "Ss")
        op("pool", lambda e: e.tensor_copy(out=Ssb.t[:], in_=Ss.t[:]), reads=[Ss.o], writes=[Ssb.o])
        for blk in range(8):
            ld(ckf, cache_k[blk * 128:(blk + 1) * 128, :], "ckf")
            op("pool", lambda e: e.tensor_copy(out=ckb.t[:], in_=ckf.t[:]), reads=[ckf.o], writes=[ckb.o])
            for h in range(4):
                op("pe", lambda e, h=h: e.transpose(out=s0b[:, h * 128:(h + 1) * 128], in_=ckb.t[:, h * 128:(h + 1) * 128],
                                                    identity=identb.t[:]), reads=[ckb.o, identb.o], writes=[s0.o])
            kt = kaT[blk % 2]
            op("act", lambda e, kt=kt: e.copy(out=kt.t[:], in_=s0b[:, 0:512].rearrange("p (a b) -> p a b", a=4)),
               reads=[s0.o], writes=[kt.o])
            dma("pool", lambda e, kt=kt, blk=blk: e.dma_start(out=KSs[:, :, blk * 128:(blk + 1) * 128].rearrange("h p t -> p h t"),
                                                             in_=kt.t[:]), reads=[kt.o], key=f"kaT{blk % 2}")
            ld(ckf, cache_v[blk * 128:(blk + 1) * 128, :], "ckf")
            vb_ = vab[blk % 2]
            op("pool", lambda e, vb_=vb_: e.tensor_copy(out=vb_.t[:], in_=ckf.t[:]), reads=[ckf.o], writes=[vb_.o])
            dma("pool", lambda e, vb_=vb_, blk=blk: e.dma_start(out=VSs[:, :, blk, :].rearrange("h p d -> p h d"),
                                                               in_=vb_.t[:, :].rearrange("p (h d) -> p h d", h=4)),
                reads=[vb_.o], key=f"vab{blk % 2}")
        p1_tile(NT, 32, 32, True)
        dma("pool", lambda e: e.dma_start(out=ret_s.rearrange("h k v -> k h v"), in_=Ss.t[:]), reads=[Ss.o], key="rets")
        P.barrier()
        if STOP == "p1":
            P.wait_all_dma("sp")
            P.emit()
            return nc

        W.reset()
        Kt = [W.get([128, 1024], BF16, f"Kt{i}") for i in range(2)]
        Vc = [W.get([128, 8, 128], BF16, f"Vc{i}") for i in range(2)]
        Qt = [W.get([128, 512], BF16, f"Qt{i}") for i in range(2)]
        PT = [W.get([128, 512], BF16, f"PT{i}") for i in range(6)]
        sacc = [W.get([128, 512], F32, f"sacc{i}") for i in range(2)]
        onesf = W.get([128, 128], F32, "onesf")
        shi = W.get([128, 512], BF16, "shi")
        slo = W.get([128, 512], BF16, "slo")
        tmpf = [W.get([128, 512], F32, f"tmpf{i}") for i in range(2)]
        BT = [W.get([128, 512], F32, f"BT{i}") for i in range(5)]
        Hk = W.get([128, 512], F32, "Hk")
        mcol = W.get([128, 1024], F32, "mcol")
        fR = W.get([128, 512], F32, "fR")
        fo0 = W.get([128, 512], F32, "fo0")
        fo1 = W.get([128, 512], F32, "fo1")
        foa = W.get([128, 512], F32, "foa")
        fsd = W.get([128, 512], F32, "fsd")
        fsq = W.get([128, 512], BF16, "fsq")
        yab = [W.get([128, 512], BF16, f"yab{i}") for i in range(2)]
        zc = W.get([128, 1], F32, "zc")
        ld(mcol, mcol_d, "mcol")
        op("pool", lambda e: e.memset(zc.t[:], 0.0), writes=[zc.o])
        op("pool", lambda e: e.memset(onesf.t[:], 1.0), writes=[onesf.o])
        STb = [s0, s7, pC0, pC1]
        cnt = {"st": 0, "pt": 0, "ch": 0, "q": 0, "tf": 0, "ya": 0}

        def attend(h, Qap, Qo, nq, blocks, out_cb):
            accO = [pA0, pB0]
            accL = [pA1, pB1]
            nb = len(blocks)
            pend = {}

            def stage1(bi):
                b = blocks[bi]
                nk = b["nk"]
                if b.get("pre") is not None:
                    b["pre"]()
                Kap, Ko = b["K"]
                stbs, pts = [], []
                for m in range(2):
                    stb = STb[cnt["st"] % 4]; cnt["st"] += 1
                    stbs.append(stb)
                    op("pe", lambda e, m=m, stb=stb: e.matmul(stb.t[0:nk, 0:nq], lhsT=Kap[m * 64:(m + 1) * 64, :],
                                                              rhs=Qap[m * 64:(m + 1) * 64, :], start=True, stop=True),
                       reads=[Ko, Qo], writes=[stb.o])
                for m in range(2):
                    stb = stbs[m]
                    pt = PT[cnt["pt"] % 6]; cnt["pt"] += 1
                    pts.append(pt)
                    if b["bias"] is not None:
                        bap, bo = b["bias"]
                        tf = tmpf[cnt["tf"] % 2]; cnt["tf"] += 1
                        op("dve", lambda e, tf=tf, stb=stb: e.tensor_tensor(out=tf.t[0:nk, 0:nq], in0=stb.t[0:nk, 0:nq], in1=bap, op=add),
                           reads=[stb.o, bo], writes=[tf.o])
                        src, so = tf.t[0:nk, 0:nq], tf.o
                    else:
                        src, so = stb.t[0:nk, 0:nq], stb.o
                    op("act", lambda e, pt=pt, src=src: e.activation(out=pt.t[0:nk, 0:nq], in_=src, func=AF.Exp, bias=b["mc"][0:nk, :]),
                       reads=[so, mcol.o, zc.o], writes=[pt.o])
                    for (r0, r1, c0, c1) in b["zero"]:
                        op("pool", lambda e, pt=pt, r0=r0, r1=r1, c0=c0, c1=c1: e.memset(pt.t[r0:r1, c0:c1], 0.0), writes=[pt.o])
                pend[bi] = pts

            def stage2(bi):
                b = blocks[bi]
                nk = b["nk"]
                pts = pend.pop(bi)
                Vap, Vo = b["V"]
                for m in range(2):
                    pt = pts[m]
                    O_ = accO[m]
                    op("pe", lambda e, pt=pt, O_=O_: e.matmul(O_.t[:, 0:nq], lhsT=Vap, rhs=pt.t[0:nk, 0:nq], start=(bi == 0), stop=(bi == nb - 1)),
                       reads=[Vo, pt.o], writes=[O_.o])
                for m in range(2):
                    pt = pts[m]
                    sa = sacc[m]
                    if bi == 0:
                        op("dve", lambda e, pt=pt, sa=sa: e.tensor_copy(out=sa.t[0:nk, 0:nq], in_=pt.t[0:nk, 0:nq]), reads=[pt.o], writes=[sa.o])
                    else:
                        op("dve", lambda e, pt=pt, sa=sa: e.tensor_tensor(out=sa.t[0:nk, 0:nq], in0=sa.t[0:nk, 0:nq], in1=pt.t[0:nk, 0:nq], op=add),
                           reads=[pt.o, sa.o], writes=[sa.o])

            for bi in range(nb + 1):
                if bi < nb:
                    stage1(bi)
                if bi >= 1:
                    stage2(bi - 1)
            for m, fo in ((0, fo0), (1, fo1)):
                O_, L_ = accO[m], accL[m]
                sa = sacc[m]
                op("dve", lambda e, sa=sa: e.tensor_copy(out=shi.t[:, 0:nq], in_=sa.t[:, 0:nq]), reads=[sa.o], writes=[shi.o])
                op("dve", lambda e, sa=sa: e.tensor_tensor(out=slo.t[:, 0:nq], in0=sa.t[:, 0:nq], in1=shi.t[:, 0:nq], op=sub_),
                   reads=[sa.o, shi.o], writes=[slo.o])
                op("pe", lambda e, L_=L_: e.matmul(L_.t[:, 0:nq], lhsT=onesb.t[:, :], rhs=shi.t[:, 0:nq], start=True, stop=False),
                   reads=[onesb.o, shi.o], writes=[L_.o])
                op("pe", lambda e, L_=L_: e.matmul(L_.t[:, 0:nq], lhsT=onesb.t[:, :], rhs=slo.t[:, 0:nq], start=False, stop=True),
                   reads=[onesb.o, slo.o], writes=[L_.o])
                op("dve", lambda e, L_=L_: e.reciprocal(out=fR.t[:, 0:nq], in_=L_.t[:, 0:nq]), reads=[L_.o], writes=[fR.o])
                op("dve", lambda e, O_=O_, fo=fo: e.tensor_tensor(out=fo.t[:, 0:nq], in0=O_.t[:, 0:nq], in1=fR.t[:, 0:nq], op=mult),
                   reads=[O_.o, fR.o], writes=[fo.o])
            op("dve", lambda e: e.scalar_tensor_tensor(out=foa.t[:, 0:nq], in0=fo1.t[:, 0:nq], scalar=nlam, in1=fo0.t[:, 0:nq],
                                                       op0=mult, op1=add), reads=[fo0.o, fo1.o, lamc.o], writes=[foa.o])
            op("act", lambda e: e.activation(out=fsq.t[:, 0:nq], in_=foa.t[:, 0:nq], func=AF.Square), reads=[foa.o], writes=[fsq.o])
            op("pe", lambda e: e.matmul(pC1.t[:, 0:nq], lhsT=onesb.t[:, :], rhs=fsq.t[:, 0:nq], start=True, stop=True),
               reads=[onesb.o, fsq.o], writes=[pC1.o])
            op("act", lambda e: e.activation(out=fsd.t[:, 0:nq], in_=pC1.t[:, 0:nq], func=AF.Sqrt, scale=1.0 / 128, bias=epsc.t[:, :]),
               reads=[pC1.o, epsc.o], writes=[fsd.o])
            op("dve", lambda e: e.reciprocal(out=fsd.t[:, 0:nq], in_=fsd.t[:, 0:nq]), reads=[fsd.o], writes=[fsd.o])
            out_cb()

        for h in range(4):
            for j in range(-1, 4):
                src = bass.AP(Gscr, h * 1280 + (R0 - 127 - 128 * j), ((1, 128), (1, 512)))
                ld(Hk, src, "Hk", reads=[OG])
                op("pe", lambda e: e.matmul(pC1.t[:, :], lhsT=jrev.t[:, :], rhs=Hk.t[:, :], start=True, stop=True),
                   reads=[jrev.o, Hk.o], writes=[pC1.o])
                bt = BT[j + 1]
                op("act", lambda e, bt=bt: e.copy(out=bt.t[:], in_=pC1.t[:, :]), reads=[pC1.o], writes=[bt.o])
            for i in range(8):
                sg = 4 * i + 3
                nblk = 4 * sg + 4
                q_ = Qt[cnt["q"] % 2]; cnt["q"] += 1
                ld(q_, Qscr[h, :, i * 512:(i + 1) * 512], q_.o.name, reads=[OQ_])
                blocks = []
                base = cnt["ch"]
                nch = nblk // 8
                cnt["ch"] += nch

                def issue(ci, h=h, base=base):
                    kc_ = Kt[(base + ci) % 2]; vc_ = Vc[(base + ci) % 2]
                    ld(kc_, Kscr[h, :, ci * 1024:(ci + 1) * 1024], kc_.o.name, reads=[OK_])
                    ld(vc_, Vscr[h, :, ci * 8:(ci + 1) * 8, :], vc_.o.name, reads=[OV_])

                for kb in range(nblk):
                    ci = kb // 8
                    kc = Kt[(base + ci) % 2]; vc = Vc[(base + ci) % 2]
                    kl = kb % 8
                    j = kb - 4 * sg
                    b = dict(K=(kc.t[:, kl * 128:(kl + 1) * 128], kc.o), V=(vc.t[:, kl, :], vc.o), nk=128,
                             bias=None, mc=mcol.t[:, i * 128 + kb:i * 128 + kb + 1], zero=[], pre=None)
                    if kb == 0:
                        b["pre"] = (lambda issue=issue, nch=nch: [issue(c_) for c_ in range(min(2, nch))])
                    elif kl == 0 and ci + 1 < nch:
                        b["pre"] = (lambda issue=issue, ci=ci: issue(ci + 1))
                    if j >= -1:
                        b["bias"] = (BT[j + 1].t[:, :], BT[j + 1].o)
                    if j >= 0:
                        if j > 0:
                            b["zero"].append((0, 128, 0, 128 * j))
                        b["zero"].append((64, 128, 128 * j, 128 * j + 64))
                    blocks.append(b)

                def out_cb(i=i, h=h):
                    ya = yab[cnt["ya"] % 2]; cnt["ya"] += 1
                    op("dve", lambda e: e.scalar_tensor_tensor(out=ya.t[:, :], in0=foa.t[:, :], scalar=sub.t[:, 0:1], in1=fsd.t[:, :],
                                                               op0=mult, op1=mult), reads=[foa.o, sub.o, fsd.o], writes=[ya.o])
                    dma("pool", lambda e: e.dma_start(out=YAscr[h, :, i * 512:(i + 1) * 512], in_=ya.t[:, :]),
                        reads=[ya.o], key=ya.o.name)

                attend(h, q_.t[:, :], q_.o, 512, blocks, out_cb)
            kc = Kt[cnt["ch"] % 2]; vc = Vc[cnt["ch"] % 2]; cnt["ch"] += 1
            ld(kc, KSs[h, :, 0:1024], kc.o.name, reads=[OKS])
            ld(vc, VSs[h, :, 0:8, :], vc.o.name, reads=[OVS])
            kc2 = Kt[cnt["ch"] % 2]; vc2 = Vc[cnt["ch"] % 2]; cnt["ch"] += 1
            dma("sp", lambda e, kc2=kc2, h=h: e.dma_start(out=kc2.t[:, 0:32], in_=KSs[h, :, 1024:1056]), reads=[OKS], writes=[kc2.o],
                key=kc2.o.name)
            dma("sp", lambda e, vc2=vc2, h=h: e.dma_start(out=vc2.t[0:32, 0, :], in_=VSs[h, 0:32, 8, :]), reads=[OVS], writes=[vc2.o],
                key=vc2.o.name)
            blocks = []
            for kb in range(8):
                b = dict(K=(kc.t[:, kb * 128:(kb + 1) * 128], kc.o), V=(vc.t[:, kb, :], vc.o), nk=128, bias=None, mc=zc.t[:, 0:1], zero=[])
                if kb == 7:
                    b["bias"] = (BT[0].t[:, 0:32], BT[0].o)
                blocks.append(b)
            blocks.append(dict(K=(kc2.t[:, 0:32], kc2.o), V=(vc2.t[0:32, 0, :], vc2.o), nk=32,
                               bias=(BT[1].t[0:32, 0:32], BT[1].o), mc=zc.t[:, 0:1], zero=[]))

            def out_cb_s(h=h):
                op("dve", lambda e: e.scalar_tensor_tensor(out=yas_T.t[:, h, :], in0=foa.t[:, 0:32], scalar=sub.t[:, 0:1],
                                                           in1=fsd.t[:, 0:32], op0=mult, op1=mult),
                   reads=[foa.o, sub.o, fsd.o], writes=[yas_T.o])

            attend(h, qs_T.t[:, h, :], qs_T.o, 32, blocks, out_cb_s)
        P.barrier()
        if STOP == "p2":
            P.wait_all_dma("sp")
            P.emit()
            return nc

        A.reset()
        W.reset()
        Wg = W.get([128, 8, 2048], BF16, "Wg").t
        Wo = W.get([128, 8, 1024], BF16, "Wo").t
        Wpq = W.get([128, 8, 2048], BF16, "Wpq").t
        Wba = W.get([128, 4, 1024], BF16, "Wba").t
        Wbr = W.get([128, 4, 1024], BF16, "Wbr").t
        craw = A.get([128, 2, 8, 128], F32, "cand")
        stage3 = []
        for i in range(2):
            sg_ = T(craw.t[:, i], "cand")
            sg_.o = craw.o
            stage3.append(sg_)
        NG = 5
        uvb = [A.get([128, 2 * D], BF16, f"uv{i}") for i in range(NG)]
        acol = [A.get([128, 1], F32, f"acol{i}") for i in range(NG)]
        gcol = [A.get([128, 1], F32, f"gcol{i}") for i in range(NG)]
        specs = []
        specs += wspecs(Wg, w_in, 8, 2048, GA, piece=128)
        specs += wspecs(Wo, w_o, 8, 1024, 0, piece=128)
        specs += wspecs(Wpq, w_pq, 8, 2048, 0, piece=128)
        specs += wspecs(Wba, w_ba, 4, 1024, 0, piece=128)
        specs += wspecs(Wbr, w_br, 4, 1024, 0, piece=128)
        load_weights(specs, stage3)
        xs3 = A.get([128, D], F32, "xs3")
        xnew = A.get([128, D], F32, "xnew")
        F3 = A.get([128, D], F32, "F1b")
        xn3 = A.get([128, D], BF16, "xnb")
        hT3 = A.get([128, 8, 128], BF16, "hT3")
        h2T = A.get([128, 8, 128], BF16, "h2T")
        ss3 = A.get([128, 4], F32, "ss3")
        sga = A.get([128, 8, 128], BF16, "sga")
        sgb = A.get([128, 8, 128], BF16, "sgb")
        yain = A.get([128, 4, 128], BF16, "yain")
        yrin = A.get([128, 4, 128], BF16, "yrin")
        u1 = A.get([128, 4, 128], F32, "u1")
        u2 = A.get([128, 4, 128], F32, "u2")
        yT = A.get([128, 8, 128], BF16, "yT")
        h2tm = A.get([128, D], BF16, "h2tm")
        qT = A.get([128, 16, 128], BF16, "qT")
        wk = A.get([128, 256], F32, "wk")
        mx = A.get([128, 16, 16], F32, "mx")
        ix = A.get([128, 16, 16], U32, "ix")
        ixf = A.get([128, 16, 16], F32, "ixf")
        cand = T(craw.t.rearrange("p a b c -> p (a b c)").rearrange("p (a b c) -> p a b c", a=8, b=16), "cand")
        cand.o = craw.o
        ts_ = A.get([128, 8, 16], F32, "ts")
        pos = A.get([128, 8, 16], U32, "pos")
        piu = A.get([128, 8, 16], U32, "piu")
        pju = A.get([128, 8, 16], U32, "pju")
        pj = A.get([128, 8, 16], F32, "pj")
        pi_ = A.get([128, 8, 16], F32, "pi")
        seli = A.get([128, 8, 16], F32, "seli")
        selj = A.get([128, 8, 16], F32, "selj")
        eidx = A.get([128, 128], F32, "eidx")
        ee = A.get([128, 8, 16], F32, "ee")
        esum = A.get([128, 8], F32, "esum")
        gg = A.get([128, 128], F32, "gg")
        eidxT = A.get([128, 128], I32, "eidxT")
        gT = A.get([128, 128], F32, "gT")
        cm = [A.get([128, 128], BF16, f"cm{i}") for i in range(NG)]
        s0b = s0.t[:].bitcast(BF16)
        load_rows(0)

        def norm_transpose3(xt, n, Acol, Bcol, hT):
            op("act", lambda e: e.activation(out=F3.t[0:n, :], in_=xt.t[0:n, :], func=AF.Square, accum_out=ss3.t[0:n, 0:1]),
               reads=[xt.o], writes=[F3.o, ss3.o])
            op("act", lambda e: e.activation(out=ss3.t[0:n, 1:2], in_=ss3.t[0:n, 0:1], func=AF.Sqrt, scale=1.0 / D, bias=epsc.t[0:n, :]),
               reads=[ss3.o, epsc.o], writes=[ss3.o])
            op("dve", lambda e: e.reciprocal(out=ss3.t[0:n, 2:3], in_=ss3.t[0:n, 1:2]), reads=[ss3.o], writes=[ss3.o])
            op("dve", lambda e: e.tensor_scalar(out=xn3.t[0:n, :], in0=xt.t[0:n, :], scalar1=ss3.t[0:n, 2:3], scalar2=None, op0=mult),
               reads=[xt.o, ss3.o], writes=[xn3.o])
            for c in range(8):
                op("pe", lambda e, c=c: e.transpose(out=s0b[:, c * 128:c * 128 + n], in_=xn3.t[0:n, c * 128:(c + 1) * 128],
                                                    identity=identb.t[0:n, 0:n]), reads=[xn3.o, identb.o], writes=[s0.o])
            for c in range(8):
                op("dve", lambda e, c=c: e.tensor_scalar(out=hT.t[:, c, 0:n], in0=s0b[:, c * 128:c * 128 + n],
                                                         scalar1=Acol[:, c:c + 1], scalar2=Bcol[:, c:c + 1], op0=mult, op1=add),
                   reads=[s0.o, AB.o], writes=[hT.o])

        def top16(src_ap, src_o, n, width, out_v, out_i, vo, io):
            op("dve", lambda e: e.max(out=out_v[:, 0:8], in_=src_ap), reads=[src_o], writes=[vo])
            op("dve", lambda e: e.match_replace(out=wk.t[0:n, 0:width], in_to_replace=out_v[:, 0:8], in_values=src_ap, imm_value=-1e30),
               reads=[src_o, vo], writes=[wk.o])
            op("dve", lambda e: e.max(out=out_v[:, 8:16], in_=wk.t[0:n, 0:width]), reads=[wk.o], writes=[vo])
            op("dve", lambda e: e.max_index(out=out_i[:, 0:8], in_max=out_v[:, 0:8], in_values=src_ap), reads=[src_o, vo], writes=[io])
            op("dve", lambda e: e.max_index(out=out_i[:, 8:16], in_max=out_v[:, 8:16], in_values=wk.t[0:n, 0:width]),
               reads=[wk.o, vo], writes=[io])

        def p3_tile(oi, n, sample):
            g = 1 if sample else 0
            A1c, B1c, A2c, B2c = (AB.t[:, g, k, :] for k in range(4))
            if sample:
                dma("sp", lambda e: e.dma_start(out=xs3.t[0:32, :], in_=x_s), writes=[xs3.o], key="xs3")
                ya_ap, yr_ap, ya_o, yr_o = yas_T.t, yrs_T.t, yas_T.o, yrs_T.o
            else:
                s = OWN[oi]
                ld(xs3, x_all[s * 128:(s + 1) * 128, :], "xs3")
                ld(yain, YAscr[:, :, oi * 128:(oi + 1) * 128].rearrange("h p t -> p h t"), "yain", reads=[OYA])
                ld(yrin, YRscr[:, :, oi * 128:(oi + 1) * 128].rearrange("h p t -> p h t"), "yrin", reads=[OYR])
                ya_ap, yr_ap, ya_o, yr_o = yain.t, yrin.t, yain.o, yrin.o
            norm_transpose3(xs3, n, A1c, B1c, hT3)
            for gi, sgt in ((0, sga), (1, sgb)):
                for half, bank in ((0, pA0), (1, pA1)):
                    fm_proj(bank, Wg, gi * 1024 + half * 512, 4, hT3, n)
                    op("act", lambda e, sgt=sgt, half=half, bank=bank: e.activation(out=sgt.t[:, half * 4:(half + 1) * 4, 0:n], in_=v4(bank, n),
                                                                                    func=AF.Sigmoid), reads=[bank.o], writes=[sgt.o])
            for half, (bA, bR) in ((0, (pB0, pC0)), (1, (pB1, pC1))):
                for j in range(4):
                    jj = half * 4 + j
                    for h in range(4):
                        op("pe", lambda e, j=j, jj=jj, h=h, bA=bA: e.matmul(bA.t[:, j * 128:j * 128 + n], lhsT=Wba[:, h, jj * 128:(jj + 1) * 128],
                                                                             rhs=ya_ap[:, h, 0:n], start=(h == 0), stop=(h == 3)),
                           reads=[Owb, ya_o], writes=[bA.o])
                    for h in range(4):
                        op("pe", lambda e, j=j, jj=jj, h=h, bR=bR: e.matmul(bR.t[:, j * 128:j * 128 + n], lhsT=Wbr[:, h, jj * 128:(jj + 1) * 128],
                                                                             rhs=yr_ap[:, h, 0:n], start=(h == 0), stop=(h == 3)),
                           reads=[Owb, yr_o], writes=[bR.o])
                op("dve", lambda e, half=half, bA=bA: e.tensor_tensor(out=u1.t[:, :, 0:n], in0=v4(bA, n), in1=sga.t[:, half * 4:(half + 1) * 4, 0:n],
                                                                      op=mult), reads=[bA.o, sga.o], writes=[u1.o])
                op("dve", lambda e, half=half, bR=bR: e.tensor_tensor(out=u2.t[:, :, 0:n], in0=v4(bR, n), in1=sgb.t[:, half * 4:(half + 1) * 4, 0:n],
                                                                      op=mult), reads=[bR.o, sgb.o], writes=[u2.o])
                op("pool", lambda e, half=half: e.tensor_tensor(out=yT.t[:, half * 4:(half + 1) * 4, 0:n], in0=u1.t[:, :, 0:n], in1=u2.t[:, :, 0:n],
                                                                op=add), reads=[u1.o, u2.o], writes=[yT.o])
            for half, bank in ((0, pA0), (1, pA1)):
                for k in range(8):
                    op("pe", lambda e, k=k, half=half, bank=bank: e.matmul(bank.t[0:n, :], lhsT=yT.t[:, k, 0:n], rhs=Wo[:, k, half * 512:(half + 1) * 512],
                                                                           start=(k == 0), stop=(k == 7)), reads=[yT.o, Owb], writes=[bank.o])
            op("dve", lambda e: e.tensor_tensor(out=F3.t[0:n, :], in0=pA[0:n, :], in1=G1.t[0:n, :], op=mult),
               reads=[pA0.o, pA1.o, G1.o], writes=[F3.o])
            op("pool", lambda e: e.tensor_tensor(out=xnew.t[0:n, :], in0=F3.t[0:n, :], in1=xs3.t[0:n, :], op=add),
               reads=[F3.o, xs3.o], writes=[xnew.o])
            norm_transpose3(xnew, n, A2c, B2c, h2T)
            for c in range(8):
                op("pe", lambda e, c=c: e.transpose(out=s0b[0:n, c * 128:(c + 1) * 128], in_=h2T.t[:, c, 0:n], identity=identb.t[:, :]),
                   reads=[h2T.o, identb.o], writes=[s0.o])
            op("act", lambda e: e.copy(out=h2tm.t[0:n, :], in_=s0b[0:n, :]), reads=[s0.o], writes=[h2tm.o])
            for c4, bank in enumerate((pA0, pA1, pB0, pB1)):
                fm_proj(bank, Wpq, c4 * 512, 4, h2T, n)
                op("act", lambda e, c4=c4, bank=bank: e.copy(out=qT.t[:, c4 * 4:(c4 + 1) * 4, 0:n], in_=v4(bank, n)), reads=[bank.o], writes=[qT.o])
            banks4 = (pA0, pA1, pB0, pB1)
            for c in range(16):
                bank = banks4[c // 4]
                op("pe", lambda e, c=c, bank=bank: e.matmul(bank.t[0:n, (c % 4) * 128:(c % 4 + 1) * 128], lhsT=qT.t[:, c, 0:n], rhs=keysT.t[:, c, :],
                                                            start=True, stop=True), reads=[qT.o, keysT.o], writes=[bank.o])
            for c in range(16):
                bank = banks4[c // 4]
                top16(bank.t[0:n, (c % 4) * 128:(c % 4 + 1) * 128], bank.o, n, 128, mx.t[0:n, c, :], ix.t[0:n, c, :], mx.o, ix.o)
            op("dve", lambda e: e.tensor_copy(out=ixf.t[0:n], in_=ix.t[0:n]), reads=[ix.o], writes=[ixf.o])

            def mxv(off, pat):
                base = mx.t[0:n, 0, 0:1]
                return bass.AP(base.tensor, base.offset + off, (base.ap[0],) + pat)

            def ixv(off, pat):
                base = ixf.t[0:n, 0, 0:1]
                return bass.AP(base.tensor, base.offset + off, (base.ap[0],) + pat)

            op("dve", lambda e: e.tensor_tensor(out=cand.t[0:n], in0=mxv(0, ((32, 8), (1, 16), (0, 16))), in1=mxv(16, ((32, 8), (0, 16), (1, 16))),
                                                op=add), reads=[mx.o], writes=[cand.o])
            for h in range(8):
                top16(cand.t[0:n, h].rearrange("p a b -> p (a b)"), cand.o, n, 256, ts_.t[0:n, h, :], pos.t[0:n, h, :], ts_.o, pos.o)
            t0 = bass.AP(ts_.t[0:n, 0, 0:1].tensor, ts_.t[0:n, 0, 0:1].offset, (ts_.t[0:n, 0, 0:1].ap[0], (16, 8), (0, 16)))
            op("dve", lambda e: e.tensor_tensor(out=ee.t[0:n], in0=ts_.t[0:n], in1=t0, op=sub_), reads=[ts_.o], writes=[ee.o])
            op("act", lambda e: e.activation(out=ee.t[0:n], in_=ee.t[0:n], func=AF.Exp), reads=[ee.o], writes=[ee.o])
            op("dve", lambda e: e.reduce_sum(out=esum.t[0:n, :], in_=ee.t[0:n], axis=AX.X), reads=[ee.o], writes=[esum.o])
            op("dve", lambda e: e.reciprocal(out=esum.t[0:n, :], in_=esum.t[0:n, :]), reads=[esum.o], writes=[esum.o])
            e0 = bass.AP(esum.t[0:n, 0:1].tensor, esum.t[0:n, 0:1].offset, (esum.t[0:n, 0:1].ap[0], (1, 8), (0, 16)))
            op("dve", lambda e: e.tensor_tensor(out=gg.t[0:n, :].rearrange("p (a b) -> p a b", a=8), in0=ee.t[0:n], in1=e0, op=mult),
               reads=[ee.o, esum.o], writes=[gg.o])
            op("dve", lambda e: e.tensor_scalar(out=piu.t[0:n], in0=pos.t[0:n], scalar1=4, scalar2=None, op0=ALU.logical_shift_right),
               reads=[pos.o], writes=[piu.o])
            op("dve", lambda e: e.tensor_scalar(out=pju.t[0:n], in0=pos.t[0:n], scalar1=15, scalar2=None, op0=ALU.bitwise_and),
               reads=[pos.o], writes=[pju.o])
            op("dve", lambda e: e.tensor_copy(out=pi_.t[0:n], in_=piu.t[0:n]), reads=[piu.o], writes=[pi_.o])
            op("dve", lambda e: e.tensor_copy(out=pj.t[0:n], in_=pju.t[0:n]), reads=[pju.o], writes=[pj.o])
            io_ = iota16.t[0:n, 0:1]
            iotav = bass.AP(io_.tensor, io_.offset, (io_.ap[0], (0, 8), (0, 16), (1, 16)))
            for (pp, off, sel) in ((pi_, 0, seli), (pj, 16, selj)):
                b_ = pp.t[0:n, 0, 0:1]
                pv = bass.AP(b_.tensor, b_.offset, (b_.ap[0], (16, 8), (1, 16), (0, 16)))
                op("dve", lambda e, pv=pv: e.tensor_tensor(out=cand.t[0:n], in0=pv, in1=iotav, op=ALU.is_equal),
                   reads=[pp.o, iota16.o], writes=[cand.o])
                op("dve", lambda e, off=off: e.tensor_tensor(out=cand.t[0:n], in0=cand.t[0:n], in1=ixv(off, ((32, 8), (0, 16), (1, 16))), op=mult),
                   reads=[cand.o, ixf.o], writes=[cand.o])
                op("dve", lambda e, sel=sel: e.reduce_sum(out=sel.t[0:n], in_=cand.t[0:n], axis=AX.X), reads=[cand.o], writes=[sel.o])
            op("dve", lambda e: e.scalar_tensor_tensor(out=eidx.t[0:n, :].rearrange("p (a b) -> p a b", a=8), in0=seli.t[0:n], scalar=128.0,
                                                       in1=selj.t[0:n], op0=mult, op1=add), reads=[seli.o, selj.o], writes=[eidx.o])
            op("pe", lambda e: e.transpose(out=s7.t[:, 0:n], in_=eidx.t[0:n, :], identity=identf.t[0:n, 0:n]), reads=[eidx.o, identf.o], writes=[s7.o])
            op("dve", lambda e: e.tensor_copy(out=eidxT.t[:, 0:n], in_=s7.t[:, 0:n]), reads=[s7.o], writes=[eidxT.o])
            op("pe", lambda e: e.transpose(out=s7.t[:, 128:128 + n], in_=gg.t[0:n, :], identity=identf.t[0:n, 0:n]), reads=[gg.o, identf.o], writes=[s7.o])
            op("act", lambda e: e.copy(out=gT.t[:, 0:n], in_=s7.t[:, 128:128 + n]), reads=[s7.o], writes=[gT.o])
            UVsrc = UVbf.ap().rearrange("e t d -> e (t d)")

            def st_gather(t):
                uv = uvb[t % NG]
                dma("pool", lambda e: e.indirect_dma_start(out=uv.t[:, :], out_offset=None, in_=UVsrc,
                                                           in_offset=bass.IndirectOffsetOnAxis(ap=eidxT.t[:, t:t + 1], axis=0)),
                    reads=[eidxT.o], writes=[uv.o], key=f"{uv.o.name}_{oi % 4}")

            def pair(t):
                return (pC, pC0, pC1) if t % 2 == 0 else (pA, pA0, pA1)

            def st_bcast(t):
                idc = identb.t[0:n, t:t + 1]
                selT = bass.AP(idc.tensor, idc.offset, (idc.ap[0], (0, 128)))
                pX, pX0, pX1 = pair(t)
                for half in range(2):
                    op("pe", lambda e, half=half: e.matmul(pX[:, half * 512:(half + 1) * 512], lhsT=selT,
                                                           rhs=h2tm.t[0:n, half * 512:(half + 1) * 512], start=True, stop=True),
                       reads=[identb.o, h2tm.o], writes=[pX0.o if half == 0 else pX1.o])

            def st_dot(t):
                uv, ac, gc = uvb[t % NG], acol[t % NG], gcol[t % NG]
                pX, pX0, pX1 = pair(t)
                op("dve", lambda e: e.scalar_tensor_tensor(out=F3.t[:, :], in0=uv.t[:, 0:D], scalar=1.0, in1=pX[:, :],
                                                           op0=mult, op1=mult, accum_out=ac.t[:, 0:1]),
                   reads=[uv.o, pX0.o, pX1.o], writes=[F3.o, ac.o])
                op("act", lambda e: e.activation(out=gc.t[:, 0:1], in_=ac.t[:, 0:1], func=AF.Gelu), reads=[ac.o], writes=[gc.o])

            def st_cm(t):
                gc, c_ = gcol[t % NG], cm[t % NG]
                op("dve", lambda e: e.tensor_scalar(out=c_.t[:, 0:n], in0=zwin.t[:, 127 - t:127 - t + n], scalar1=gc.t[:, 0:1],
                                                    scalar2=gT.t[:, t:t + 1], op0=mult, op1=mult),
                   reads=[zwin.o, gc.o, gT.o], writes=[c_.o])

            def st_acc(t):
                uv, c_ = uvb[t % NG], cm[t % NG]
                for half, bank in ((0, pB0), (1, pB1)):
                    op("pe", lambda e, half=half, bank=bank: e.matmul(bank.t[0:n, :], lhsT=c_.t[:, 0:n],
                                                                      rhs=uv.t[:, D + half * 512:D + (half + 1) * 512],
                                                                      start=(t == 0), stop=(t == n - 1)),
                       reads=[c_.o, uv.o], writes=[bank.o])

            for t in range(n + 2):
                if t < n:
                    st_gather(t)
                    st_bcast(t)
                    st_dot(t)
                if 1 <= t <= n:
                    st_cm(t - 1)
                if t >= 2:
                    st_acc(t - 2)
            op("dve", lambda e: e.tensor_tensor(out=F3.t[0:n, :], in0=pB[0:n, :], in1=G2.t[0:n, :], op=mult), reads=[pB0.o, pB1.o, G2.o], writes=[F3.o])
            op("pool", lambda e: e.tensor_tensor(out=xnew.t[0:n, :], in0=F3.t[0:n, :], in1=xnew.t[0:n, :], op=add), reads=[F3.o, xnew.o], writes=[xnew.o])
            op("act", lambda e: e.activation(out=F3.t[0:n, :], in_=xnew.t[0:n, :], func=AF.Square, accum_out=ss3.t[0:n, 0:1]), reads=[xnew.o], writes=[F3.o, ss3.o])
            op("act", lambda e: e.activation(out=ss3.t[0:n, 1:2], in_=ss3.t[0:n, 0:1], func=AF.Sqrt, scale=1.0 / D, bias=epsc.t[0:n, :]),
               reads=[ss3.o, epsc.o], writes=[ss3.o])
            op("dve", lambda e: e.reciprocal(out=ss3.t[0:n, 2:3], in_=ss3.t[0:n, 1:2]), reads=[ss3.o], writes=[ss3.o])
            op("dve", lambda e: e.scalar_tensor_tensor(out=F3.t[0:n, :], in0=xnew.t[0:n, :], scalar=ss3.t[0:n, 2:3], in1=FN.t[0:n, :], op0=mult, op1=mult),
               reads=[xnew.o, ss3.o, FN.o], writes=[F3.o])
            dst = y_s if sample else y_own[oi * 128:(oi + 1) * 128, :]
            dma("sp", lambda e: e.dma_start(out=dst, in_=F3.t[0:n, :]), reads=[F3.o], key="yout")

        for oi in range(32):
            p3_tile(oi, 128, False)
        load_rows(1)
        p3_tile(32, 32, True)
        P.wait_all_dma("sp")
        P.emit()
    return nc


def _t5_bucket(rel):
    nb = 16
    ret = 16 if rel > 0 else 0
    n = abs(rel)
    if n < 8:
        return ret + n
    nf = np.float32(max(n, 8))
    large = 8 + int(np.float32(np.log(nf / np.float32(8)) / np.float32(math.log(16.0)) * np.float32(8)))
    return ret + min(large, 15)


def _static_tables(r):
    shift = 3 - r
    inv = (1.0 / (10000.0 ** np.linspace(0.0, 1.0, 64, dtype=np.float32))).astype(np.float32)
    rot = np.zeros((NT + 1, 128, 256), np.float32)
    pos = (np.arange(NT * 128, dtype=np.int64) - shift * 512).clip(0).astype(np.float32)
    ang = (pos[:, None] * inv[None, :]).astype(np.float32).astype(np.float64)
    cos, sin = np.cos(ang).astype(np.float32), np.sin(ang).astype(np.float32)
    C = np.concatenate([cos, cos], 1).T.reshape(128, NT, 128)
    Sg = np.concatenate([-sin, sin], 1).T.reshape(128, NT, 128)
    rot[:NT, :, 0:128] = C.transpose(1, 0, 2)
    rot[:NT, :, 128:256] = Sg.transpose(1, 0, 2)
    ps = (1024 + np.arange(32)).astype(np.float32)
    angs = (ps[:, None] * inv[None, :]).astype(np.float32).astype(np.float64)
    cs, sn = np.cos(angs).astype(np.float32), np.sin(angs).astype(np.float32)
    rot[NT, :, 0:32] = np.concatenate([cs, cs], 1).T
    rot[NT, :, 128:160] = np.concatenate([-sn, sn], 1).T
    dtab = np.zeros((128, 1296), np.float32)
    sc = 128.0 ** -0.5
    for h in range(4):
        lg = math.log(GAMMAS[h])
        for (L, dto, xio, zeo) in ((128, 0, 512, 1024), (32, 1032, 1160, 1288)):
            n = np.arange(L, dtype=np.float64)
            diff = n[None, :] - n[:, None]
            DT = np.where(diff >= 0, np.exp(np.maximum(diff, 0) * lg), 0.0) * sc
            dtab[0:L, dto + h * L:dto + (h + 1) * L] = DT
            dtab[:, xio + h * L:xio + (h + 1) * L] = np.exp((n + 1.0) * lg)[None, :]
            dtab[0:L, zeo + h] = np.exp((L - 1.0 - n) * lg) * sc
    mcol = np.zeros((128, 1024), np.float32)
    for i in range(8):
        mcol[:, i * 128:i * 128 + 4 * shift] = NEG
    vsv = np.ones((128, 3), np.float32)
    vsv[:, 0:shift] = 0.0
    return rot, dtab, mcol, vsv


def kernel(x_prompt, x_sample, cache_k, cache_v, state_ret, c_prompt, c_sample, w_ada, b_ada, norm1, norm2, w_in,
           lam_q1, lam_k1, lam_q2, lam_k2, subln_a, subln_r, w_ba, w_br, w_o, rel_bias, w_pq, peer_keys, peer_u, peer_v,
           final_norm):
    f = lambda a: np.ascontiguousarray(np.asarray(a, dtype=np.float32))
    x_prompt, x_sample = f(x_prompt), f(x_sample)
    oht5 = np.zeros((32, 1280), np.float32)
    for i in range(1151):
        oht5[_t5_bucket(R0 - i), i] = 1.0
    zwin = np.zeros((128, 256), np.float32)
    zwin[:, 127] = 1.0
    shared = dict(
        w_ada=f(w_ada)[0], b_ada=f(b_ada)[0], norm1=f(norm1)[0], norm2=f(norm2)[0], w_in=f(w_in)[0],
        lamv=np.stack([f(lam_q1)[0], f(lam_k1)[0], f(lam_q2)[0], f(lam_k2)[0]]),
        subln=np.stack([f(subln_a)[0], f(subln_r)[0]]), w_ba=f(w_ba)[0], w_br=f(w_br)[0], w_o=f(w_o)[0],
        rel_bias=f(rel_bias), w_pq=f(w_pq)[0], peer_keys=f(peer_keys)[0].reshape(16, 128, 128),
        peer_u=f(peer_u)[0], peer_v=f(peer_v)[0], final_norm=f(final_norm), oht5=oht5,
        jrev=np.ascontiguousarray(np.eye(128, dtype=np.float32)[::-1]), ident=np.eye(128, dtype=np.float32), zwin=zwin,
        iota16=np.tile(np.arange(16, dtype=np.float32)[None, :], (128, 1)),
    )
    tabs = [_static_tables(r) for r in range(4)]
    in_maps = []
    for c in range(8):
        b, r = c // 4, c % 4
        shift = 3 - r
        xa = np.zeros((NT * 128, D), np.float32)
        xa[shift * 512:] = x_prompt[b, :NT * 128 - shift * 512]
        rot, dtab, mcol, vsv = tabs[r]
        m = dict(shared)
        m.update(x_all=xa, x_s=x_sample[c], cache_k=f(cache_k)[0, c].reshape(1024, 512), cache_v=f(cache_v)[0, c].reshape(1024, 512),
                 state_in=f(state_ret)[0, c], c_both=np.stack([f(c_prompt)[b], f(c_sample)[c]]),
                 rot=rot, dtab=dtab, mcol=mcol, vsv=vsv)
        in_maps.append(m)
    nc = build_nc()
    if STOP:
        return nc, in_maps
    res = run_bass_kernel_spmd(nc, in_maps, core_ids=list(range(8)))
    R = res.results
    y_prompt = np.zeros((2, 16384, D), np.float32)
    k_prompt = np.zeros((1, 2, 16384, 4, 128), np.float32)
    v_prompt = np.zeros((1, 2, 16384, 4, 128), np.float32)
    ret_prompt = np.zeros((1, 2, 4, 128, 128), np.float32)
    y_sample = np.zeros((8, 32, D), np.float32)
    k_sample = np.zeros((1, 8, 32, 4, 128), np.float32)
    v_sample = np.zeros((1, 8, 32, 4, 128), np.float32)
    ret_sample = np.zeros((1, 8, 4, 128, 128), np.float32)
    for c in range(8):
        b, r = c // 4, c % 4
        shift = 3 - r
        for oi, s in enumerate(OWN):
            t0 = s * 128 - shift * 512
            y_prompt[b, t0:t0 + 128] = R[c]["y_own"][oi * 128:(oi + 1) * 128]
            k_prompt[0, b, t0:t0 + 128] = R[c]["k_own"][oi * 128:(oi + 1) * 128].reshape(128, 4, 128)
            v_prompt[0, b, t0:t0 + 128] = R[c]["v_own"][oi * 128:(oi + 1) * 128].reshape(128, 4, 128)
        if r == 3:
            ret_prompt[0, b] = R[c]["ret_p"]
        y_sample[c] = R[c]["y_s"]
        k_sample[0, c] = R[c]["k_s"].reshape(32, 4, 128)
        v_sample[0, c] = R[c]["v_s"].reshape(32, 4, 128)
        ret_sample[0, c] = R[c]["ret_s"]
    return (y_prompt, y_sample, k_prompt, v_prompt, ret_prompt, k_sample, v_sample, ret_sample)
```

```python
import math
from contextlib import ExitStack
from concourse.bass_utils import run_bass_kernel_spmd

import numpy as np
import concourse.bass as bass
import concourse.mybir as mybir

F32 = mybir.dt.float32
BF16 = mybir.dt.bfloat16
I32 = mybir.dt.int32
U32 = mybir.dt.uint32
U16 = mybir.dt.uint16
ALU = mybir.AluOpType
AF = mybir.ActivationFunctionType
AX = mybir.AxisListType

SEM_WINDOW = 12000
SAME_ENGINE_SYNC = True


class Obj:
    __slots__ = ("name", "w", "r", "excl")

    def __init__(self, name):
        self.name = name
        self.w = None
        self.r = {}
        self.excl = False


class Prog:
    ENGS = ("pe", "act", "dve", "pool", "sp")

    def __init__(self, nc, stack):
        self.nc = nc
        self.stack = stack
        self.cnt = {e: 0 for e in self.ENGS}
        self.ops = {e: [] for e in self.ENGS}
        self.waited = {e: {} for e in self.ENGS}
        self.esems = {}
        self.dsems = {}
        self.dcount = {}
        self.nsem = 0
        self.keymap = {}
        self.next_phys = 0

    def _esem(self, eng, k):
        key = (eng, k)
        if key not in self.esems:
            self.esems[key] = self.stack.enter_context(self.nc.semaphore(f"s_{eng}_{k}"))
            self.nsem += 1
        return self.esems[key]

    def _dkey(self, key):
        if key not in self.keymap:
            name = f"D{self.next_phys}"
            self.next_phys += 1
            if name not in self.dsems:
                self.dsems[name] = self.stack.enter_context(self.nc.semaphore(f"d_{name}"))
                self.dcount[name] = 0
                self.nsem += 1
            self.keymap[key] = name
        return self.keymap[key]

    def _deps(self, eng, reads, writes):
        deps = {}

        def add(src, val):
            if val > deps.get(src, 0):
                deps[src] = val

        for o in reads:
            if o.w is not None:
                add(*o.w)
            if o.excl:
                for s, v in o.r.items():
                    if s != eng:
                        add(s, v)
        for o in writes:
            if o.w is not None:
                add(*o.w)
            for s, v in o.r.items():
                add(s, v)
        waits = []
        for src, val in deps.items():
            if src == eng:
                if eng == "pe" or not SAME_ENGINE_SYNC:
                    continue
            if self.waited[eng].get(src, 0) >= val:
                continue
            self.waited[eng][src] = val
            if src in self.ENGS:
                k = (val - 1) // SEM_WINDOW
                waits.append((self._esem(src, k), (val - 1) % SEM_WINDOW + 1))
            else:
                waits.append((self.dsems[src], val))
        return waits

    def _mark(self, ev, reads, writes):
        src, val = ev
        for o in reads:
            if val > o.r.get(src, 0):
                o.r[src] = val
        for o in writes:
            o.w = ev
            o.r = {}

    def op(self, eng, fn, reads=(), writes=()):
        waits = self._deps(eng, reads, writes)
        n = self.cnt[eng] + 1
        self.cnt[eng] = n
        k = (n - 1) // SEM_WINDOW
        sem = self._esem(eng, k)
        self.ops[eng].append((waits, fn, sem, 1))
        self._mark((eng, n), reads, writes)

    def dma(self, q, fn, reads=(), writes=(), key=None):
        waits = self._deps(q, reads, writes)
        name = self._dkey(key)
        sem = self.dsems[name]
        self.dcount[name] += 16
        self.ops[q].append((waits, fn, sem, 16))
        self._mark((name, self.dcount[name]), reads, writes)

    def wait_all_dma(self, q="sp"):
        waits = []
        for key, sem in self.dsems.items():
            if self.dcount[key] > self.waited[q].get(key, 0):
                waits.append((sem, self.dcount[key]))
        self.ops[q].append((waits, None, None, 0))

    def emit(self):
        nc = self.nc
        handles = {"pe": "tensor", "act": "scalar", "dve": "vector", "pool": "gpsimd", "sp": "sync"}
        with nc.Block() as block:
            for eng in self.ENGS:
                lst = self.ops[eng]

                def body(e, lst=lst):
                    for waits, fn, sem, inc in lst:
                        for s, v in waits:
                            e.wait_ge(s, v)
                        if fn is not None:
                            ins = fn(e)
                            ins.then_inc(sem, inc)

                getattr(block, handles[eng])(body)


def _barrier(self):
    snap = [(s, v) for s, v in list(self.cnt.items()) + list(self.dcount.items()) if v > 0]
    for e in self.ENGS:
        waits = []
        for src, val in snap:
            if src == e and e in ("pe", "sp"):
                continue
            if self.waited[e].get(src, 0) >= val:
                continue
            self.waited[e][src] = val
            if src in self.ENGS:
                k = (val - 1) // SEM_WINDOW
                waits.append((self._esem(src, k), (val - 1) % SEM_WINDOW + 1))
            else:
                waits.append((self.dsems[src], val))
        self.ops[e].append((waits, None, None, 0))
    self.keymap = {}
    self.next_phys = 0


Prog.barrier = _barrier


D = 1024
NT = 128
OWN = [16 * i + 12 + j for i in range(8) for j in range(4)]
HA = 4
EPS = 1e-6
QA, KA, VA, QR, KR, VR, GR, GA, GB = 0, 512, 1024, 1536, 2048, 2560, 3072, 3584, 4608
LAM_INIT = 0.8 - 0.6 * math.exp(-0.3 * 0)
GAMMAS = [1.0 - 2.0 ** (-5.0 - h) for h in range(4)]
R0 = 511
NEG = -30000.0
WB = 49152
AR = 39936
import os
STOP = os.environ.get("MK_STOP", "")


class T:
    def __init__(self, ap, name):
        self.t = ap
        self.o = Obj(name)


def _prod(s):
    r = 1
    for v in s:
        r *= v
    return r


class Carver:
    def __init__(self, buf, size):
        self.buf = buf
        self.size = size
        self.off = 0

    def reset(self):
        self.off = 0

    def get(self, shape, dt, name):
        n = _prod(shape[1:])
        sz = n * (2 if dt in (F32, I32, U32) else 1)
        sz = (sz + 15) // 16 * 16
        assert self.off + sz <= self.size, (name, self.off, sz, self.size)
        v = self.buf[0:shape[0], self.off:self.off + sz]
        self.off += sz
        if dt != BF16:
            v = v.bitcast(dt)
        v = v[:, 0:n]
        if len(shape) == 3:
            v = v.rearrange("p (a b) -> p a b", a=shape[1])
        elif len(shape) == 4:
            v = v.rearrange("p (a b c) -> p a b c", a=shape[1], b=shape[2])
        return T(v, name)


def bcast(ap2, dims):
    base = ap2.ap
    new = [base[0]]
    for d in dims:
        if d[0] == 'b':
            new.append((0, d[1]))
        else:
            new.append(base[1])
    return bass.AP(ap2.tensor, ap2.offset, tuple(new))


def build_nc():
    nc = bass.Bass("TRN2", target_bir_lowering=False)
    din = lambda name, shape, dt=F32: nc.dram_tensor(name, list(shape), dt, kind="ExternalInput")
    dout = lambda name, shape, dt=F32: nc.dram_tensor(name, list(shape), dt, kind="ExternalOutput")
    dscr = lambda name, shape, dt: nc.dram_tensor(name, list(shape), dt, kind="Internal")

    x_all = din("x_all", [NT * 128, D]).ap()
    x_s = din("x_s", [32, D]).ap()
    cache_k = din("cache_k", [1024, 512]).ap()
    cache_v = din("cache_v", [1024, 512]).ap()
    state_in = din("state_in", [4, 128, 128]).ap()
    c_both = din("c_both", [2, D]).ap()
    w_ada = din("w_ada", [D, 6 * D]).ap()
    b_ada = din("b_ada", [6 * D])
    norm1 = din("norm1", [D]).ap()
    norm2 = din("norm2", [D]).ap()
    w_in = din("w_in", [D, 5632]).ap()
    lamv = din("lamv", [4, 64])
    subln = din("subln", [2, 128]).ap()
    w_ba = din("w_ba", [512, D]).ap()
    w_br = din("w_br", [512, D]).ap()
    w_o = din("w_o", [D, D]).ap()
    rel_bias = din("rel_bias", [32, 4])
    w_pq = din("w_pq", [D, 2048]).ap()
    peer_keys = din("peer_keys", [16, 128, 128]).ap()
    peer_u = din("peer_u", [16384, D])
    peer_v = din("peer_v", [16384, D])
    final_norm = din("final_norm", [D])
    rot = din("rot", [NT + 1, 128, 256]).ap()
    dtab_d = din("dtab", [128, 1296]).ap()
    mcol_d = din("mcol", [128, 1024]).ap()
    vsv_d = din("vsv", [128, 3]).ap()
    oht5 = din("oht5", [32, 1280]).ap()
    jrev_d = din("jrev", [128, 128]).ap()
    ident_d = din("ident", [128, 128]).ap()
    zwin_d = din("zwin", [128, 256]).ap()
    iota_d = din("iota16", [128, 16]).ap()

    y_own = dout("y_own", [4096, D]).ap()
    k_own = dout("k_own", [4096, 512]).ap()
    v_own = dout("v_own", [4096, 512]).ap()
    ret_p = dout("ret_p", [4, 128, 128]).ap()
    y_s = dout("y_s", [32, D]).ap()
    k_s = dout("k_s", [32, 512]).ap()
    v_s = dout("v_s", [32, 512]).ap()
    ret_s = dout("ret_s", [4, 128, 128]).ap()

    Kscr = dscr("Kscr", [4, 128, 16384], BF16).ap()
    Vscr = dscr("Vscr", [4, 128, 128, 128], BF16).ap()
    Qscr = dscr("Qscr", [4, 128, 4096], BF16).ap()
    YAscr = dscr("YAscr", [4, 128, 4096], BF16).ap()
    YRscr = dscr("YRscr", [4, 128, 4096], BF16).ap()
    KSs = dscr("KSs", [4, 128, 1152], BF16).ap()
    VSs = dscr("VSs", [4, 128, 9, 128], BF16).ap()
    UVbf = dscr("UVbf", [16384, 2, D], BF16)
    H2scr = dscr("H2scr", [33 * 128, D], BF16)
    modscr = dscr("modscr", [2, 6 * D], F32)
    Gscr = dscr("Gscr", [4, 1280], F32)
    OK_, OV_, OQ_, OYA, OYR, OKS, OVS, OMOD, OG = [Obj(n) for n in
                                                  ("Kscr", "Vscr", "Qscr", "YAscr", "YRscr", "KSs", "VSs", "modscr", "Gscr")]
    Oout = Obj("outputs")

    st = ExitStack()
    with st:
        P = Prog(nc, st)
        op, dma = P.op, P.dma
        sbt = lambda name, shape, dt: T(st.enter_context(nc.sbuf_tensor("sb_" + name, list(shape), dt))[:], name)
        wbuf_t = st.enter_context(nc.sbuf_tensor("wbuf", [128, WB], BF16))
        arena_t = st.enter_context(nc.sbuf_tensor("arena", [128, AR], BF16))
        Owb = Obj("wbuf_weights")
        s0 = T(st.enter_context(nc.psum_tensor("ps0", [128, 512], F32))[:], "ps0")
        s7 = T(st.enter_context(nc.psum_tensor("ps7", [128, 512], F32))[:], "ps7")
        pairs = []
        for nm in "ABC":
            pt = st.enter_context(nc.psum_tensor("pp" + nm, [128, 1024], F32))
            pairs.append((pt, T(pt[:, 0:512], nm + "0"), T(pt[:, 512:1024], nm + "1")))
        (pA, pA0, pA1), (pB, pB0, pB1), (pC, pC0, pC1) = pairs
        for _b in (s0, s7, pA0, pA1, pB0, pB1, pC0, pC1):
            _b.o.excl = True

        identb = sbt("identb", [128, 128], BF16)
        identf = sbt("identf", [128, 128], F32)
        jrev = sbt("jrev", [128, 128], F32)
        onesb = sbt("onesb", [128, 128], BF16)
        zwin = sbt("zwin", [128, 256], F32)
        iota16 = sbt("iota16", [128, 16], F32)
        epsc = sbt("epsc", [128, 1], F32)
        cols = sbt("cols", [128, 2, 6, 8], F32)
        ncols = sbt("ncols", [128, 2, 8], F32)
        AB = sbt("AB", [128, 2, 4, 8], F32)
        ABv = sbt("ABv", [128, 3, 2, 8], F32)
        vsv = sbt("vsv", [128, 3], F32)
        lamc = sbt("lamc", [128, 8], F32)
        sub = sbt("sub", [128, 2], F32)
        G1 = sbt("G1", [128, D], F32)
        G2 = sbt("G2", [128, D], F32)
        FN = sbt("FN", [128, D], F32)
        S = sbt("S", [128, 4, 128], F32)
        Sb = sbt("Sb", [128, 4, 128], BF16)
        Ss = sbt("Ss", [128, 4, 128], F32)
        Ssb = sbt("Ssb", [128, 4, 128], BF16)
        keysT = sbt("keysT", [128, 16, 128], BF16)
        dtab = sbt("dtab", [128, 1296], F32)
        qs_T = sbt("qs_T", [128, 4, 32], BF16)
        yas_T = sbt("yas_T", [128, 4, 32], BF16)
        yrs_T = sbt("yrs_T", [128, 4, 32], BF16)

        A = Carver(arena_t, AR)
        W = Carver(wbuf_t, WB)

        mult, add, sub_ = ALU.mult, ALU.add, ALU.subtract

        def ld(dst, src, key, reads=()):
            dma("sp", lambda e: e.dma_start(out=dst.t if isinstance(dst, T) else dst, in_=src),
                reads=list(reads), writes=[dst.o], key=key)

        def ld_slow(dst_ap, dst_obj, src, key, reads=()):
            dma("sp", lambda e: e.dma_start(out=dst_ap, in_=src, allow_slow_non_contiguous=True),
                reads=list(reads), writes=[dst_obj], key=key)

        ld(identf, ident_d, "c0")
        ld(jrev, jrev_d, "c1")
        ld(zwin, zwin_d, "c2")
        ld(iota16, iota_d, "c3")
        ld(dtab, dtab_d, "c4")
        ld(vsv, vsv_d, "c5")
        op("pool", lambda e: e.memset(onesb.t[:], 1.0), writes=[onesb.o])
        op("pool", lambda e: e.memset(epsc.t[:], EPS), writes=[epsc.o])
        op("dve", lambda e: e.tensor_copy(out=identb.t[:], in_=identf.t[:]), reads=[identf.o], writes=[identb.o])
        op("pool", lambda e: e.memset(S.t[:], 0.0), writes=[S.o])
        op("pool", lambda e: e.memset(Sb.t[:], 0.0), writes=[Sb.o])

        A.reset()
        cT = A.get([128, 8, 2], F32, "cT")
        for g in range(2):
            ld_slow(cT.t[:, :, g], cT.o, c_both[g].rearrange("(k p) -> p k", p=128), "cT")
        op("act", lambda e: e.activation(out=cT.t[:], in_=cT.t[:], func=AF.Silu), reads=[cT.o], writes=[cT.o])
        wst = [A.get([128, 8, 512], F32, f"wst{i}") for i in range(2)]
        modsb = W.get([2, 6 * D], F32, "modsb")
        badd = W.get([2, 6 * D], F32, "badd")
        ld(badd, bass.AP(b_ada, 0, ((0, 2), (1, 6 * D))), "badd")
        w_ada_v = w_ada.rearrange("(k p) n -> p k n", p=128)
        for cg in range(12):
            ws = wst[cg % 2]
            ld(ws, w_ada_v[:, :, cg * 512:(cg + 1) * 512], f"wst{cg % 2}")
            bank = pA0 if cg % 2 == 0 else pA1
            for k in range(8):
                op("pe", lambda e, k=k, ws=ws, bank=bank: e.matmul(bank.t[0:2, :], lhsT=cT.t[:, k, :], rhs=ws.t[:, k, :],
                                                                   start=(k == 0), stop=(k == 7)),
                   reads=[cT.o, ws.o], writes=[bank.o])
            op("dve", lambda e, cg=cg, bank=bank: e.tensor_tensor(out=modsb.t[:, cg * 512:(cg + 1) * 512], in0=bank.t[0:2, :],
                                                                  in1=badd.t[:, cg * 512:(cg + 1) * 512], op=add),
               reads=[bank.o, badd.o], writes=[modsb.o])
        dma("pool", lambda e: e.dma_start(out=modscr.ap(), in_=modsb.t[:]), reads=[modsb.o], writes=[OMOD], key="modscr")
        for g in range(2):
            for v in range(6):
                src = bass.AP(modscr, g * 6 * D + v * D, ((1, 128), (128, 8)))
                ld_slow(cols.t[:, g, v, :], cols.o, src, "cols", reads=[OMOD])
        ld_slow(ncols.t[:, 0, :], ncols.o, norm1.rearrange("(k p) -> p k", p=128), "ncols")
        ld_slow(ncols.t[:, 1, :], ncols.o, norm2.rearrange("(k p) -> p k", p=128), "ncols")
        for g in range(2):
            for j, (vsc, vsh) in enumerate(((1, 0), (4, 3))):
                op("dve", lambda e, g=g, j=j, vsc=vsc: e.scalar_tensor_tensor(
                    out=AB.t[:, g, 2 * j, :], in0=cols.t[:, g, vsc, :], scalar=1.0, in1=ncols.t[:, j, :], op0=add, op1=mult),
                   reads=[cols.o, ncols.o], writes=[AB.o])
                op("dve", lambda e, g=g, j=j, vsh=vsh: e.tensor_copy(out=AB.t[:, g, 2 * j + 1, :], in_=cols.t[:, g, vsh, :]),
                   reads=[cols.o], writes=[AB.o])
        for gi in range(3):
            for j in range(2):
                op("dve", lambda e, gi=gi, j=j: e.tensor_scalar(out=ABv.t[:, gi, j, :], in0=AB.t[:, 0, j, :],
                                                                scalar1=vsv.t[:, gi:gi + 1], scalar2=None, op0=mult),
                   reads=[AB.o, vsv.o], writes=[ABv.o])

        def load_rows(g):
            ld(G1, bass.AP(modscr, g * 6 * D + 2 * D, ((0, 128), (1, D))), "G1", reads=[OMOD])
            ld(G2, bass.AP(modscr, g * 6 * D + 5 * D, ((0, 128), (1, D))), "G2", reads=[OMOD])

        ld(FN, bass.AP(final_norm, 0, ((0, 128), (1, D))), "FN")
        lq = A.get([128, 4, 64], F32, "lq")
        ld(lq, bass.AP(lamv, 0, ((0, 128), (1, 256))), "lq")
        ljunk = A.get([128, 64], F32, "ljunk")
        for i in range(2):
            op("dve", lambda e, i=i: e.scalar_tensor_tensor(out=ljunk.t[:], in0=lq.t[:, 2 * i, :], scalar=1.0, in1=lq.t[:, 2 * i + 1, :],
                                                            op0=mult, op1=mult, accum_out=lamc.t[:, i:i + 1]),
               reads=[lq.o], writes=[ljunk.o, lamc.o])
        op("act", lambda e: e.activation(out=lamc.t[:, 2:4], in_=lamc.t[:, 0:2], func=AF.Exp), reads=[lamc.o], writes=[lamc.o])
        op("dve", lambda e: e.tensor_tensor(out=lamc.t[:, 4:5], in0=lamc.t[:, 3:4], in1=lamc.t[:, 2:3], op=sub_),
           reads=[lamc.o], writes=[lamc.o])
        op("dve", lambda e: e.tensor_scalar(out=lamc.t[:, 5:6], in0=lamc.t[:, 4:5], scalar1=-LAM_INIT, scalar2=None, op0=add),
           reads=[lamc.o], writes=[lamc.o])
        nlam = lamc.t[:, 5:6]
        ld_slow(sub.t[:, :], sub.o, subln.rearrange("a p -> p a"), "sub")
        op("dve", lambda e: e.tensor_scalar(out=sub.t[:, 0:1], in0=sub.t[:, 0:1], scalar1=1.0 - LAM_INIT, scalar2=None, op0=mult),
           reads=[sub.o], writes=[sub.o])
        rb = A.get([32, 4], F32, "rb")
        rb15 = A.get([32, 4], F32, "rb15")
        oh = A.get([32, 1280], F32, "oh")
        gsb = A.get([4, 1280], F32, "gsb")
        ld(rb, rel_bias.ap(), "rb")
        ld(rb15, bass.AP(rel_bias, 15 * 4, ((0, 32), (1, 4))), "rb15")
        ld(oh, oht5, "oh")
        op("dve", lambda e: e.tensor_tensor(out=rb.t[:], in0=rb.t[:], in1=rb15.t[:], op=sub_), reads=[rb.o, rb15.o], writes=[rb.o])
        for i, (a, b) in enumerate(((0, 512), (512, 1024), (1024, 1280))):
            op("pe", lambda e, a=a, b=b: e.matmul(pB0.t[0:4, 0:b - a], lhsT=rb.t[:, :], rhs=oh.t[:, a:b], start=True, stop=True),
               reads=[rb.o, oh.o], writes=[pB0.o])
            op("act", lambda e, a=a, b=b: e.copy(out=gsb.t[:, a:b], in_=pB0.t[0:4, 0:b - a]), reads=[pB0.o], writes=[gsb.o])
        dma("pool", lambda e: e.dma_start(out=Gscr.ap(), in_=gsb.t[:]), reads=[gsb.o], writes=[OG], key="gscr")
        kst = W.get([128, 16, 128], F32, "kst")
        ksb = W.get([128, 16, 128], BF16, "ksb")
        ld(kst, peer_keys.rearrange("c n d -> n c d"), "kst")
        op("pool", lambda e: e.tensor_copy(out=ksb.t[:], in_=kst.t[:]), reads=[kst.o], writes=[ksb.o])
        s0b = s0.t[:].bitcast(BF16)
        for c4 in range(4):
            for j in range(4):
                c = c4 * 4 + j
                op("pe", lambda e, c=c, j=j: e.transpose(out=s0b[:, j * 128:(j + 1) * 128], in_=ksb.t[:, c, :], identity=identb.t[:]),
                   reads=[ksb.o, identb.o], writes=[s0.o])
            op("dve", lambda e, c4=c4: e.tensor_copy(out=keysT.t[:, c4 * 4:(c4 + 1) * 4, :],
                                                     in_=s0b[:, 0:512].rearrange("p (a b) -> p a b", a=4)),
               reads=[s0.o], writes=[keysT.o])
        cvi = [W.get([128, 2, D], F32, f"cvi{i}") for i in range(3)]
        cvo = [W.get([128, 2, D], BF16, f"cvo{i}") for i in range(3)]
        ci_ = 0
        for ti_, tab in enumerate((peer_u, peer_v)):
            tv = tab.ap().rearrange("(p r) d -> p r d", p=128)
            dv = UVbf.ap().rearrange("(p r) t d -> p r t d", p=128)[:, :, ti_, :]
            for rr in range(0, 128, 2):
                a_, b_ = cvi[ci_ % 3], cvo[ci_ % 3]
                ld(a_, tv[:, rr:rr + 2, :], a_.o.name)
                eng = ("dve", "act", "pool")[ci_ % 3]
                if eng == "act":
                    op("act", lambda e, a_=a_, b_=b_: e.copy(out=b_.t[:], in_=a_.t[:]), reads=[a_.o], writes=[b_.o])
                else:
                    op(eng, lambda e, a_=a_, b_=b_: e.tensor_copy(out=b_.t[:], in_=a_.t[:]), reads=[a_.o], writes=[b_.o])
                dma("pool" if ci_ % 2 == 0 else "sp", lambda e, b_=b_, dv=dv, rr=rr: e.dma_start(out=dv[:, rr:rr + 2, :], in_=b_.t[:]),
                    reads=[b_.o], key=b_.o.name)
                ci_ += 1
        P.barrier()
        if STOP == "p0":
            P.wait_all_dma("sp")
            P.emit()
            return nc

        def load_weights(specs, stage):
            for i, (dst, src, K, n) in enumerate(specs):
                sg = stage[i % len(stage)]
                dma("sp", lambda e, sg=sg, src=src, K=K, n=n: e.dma_start(out=sg.t[:, 0:K, 0:n], in_=src),
                    writes=[sg.o], key=sg.o.name)
                eng = "pool" if i % 2 == 0 else "dve"
                op(eng, lambda e, sg=sg, dst=dst, K=K, n=n: e.tensor_copy(out=dst, in_=sg.t[:, 0:K, 0:n]),
                   reads=[sg.o], writes=[Owb])

        def wspecs(dst3, src2, K, ncol, src_cols=None, piece=256):
            out = []
            sv = src2.rearrange("(k p) n -> p k n", p=128)
            for a in range(0, ncol, piece):
                n = min(piece, ncol - a)
                out.append((dst3[:, :, a:a + n], sv[:, :, src_cols + a:src_cols + a + n], K, n))
            return out

        A.reset()
        W.reset()
        Wall = W.get([128, 8, 2560], BF16, "Wall").t
        Wown = W.get([128, 8, 2048], BF16, "Wown").t
        stage = [A.get([128, 8, 256], F32, f"stg{i}") for i in range(2)]
        specs = []
        specs += wspecs(Wall[:, :, 0:512], w_in, 8, 512, KA)
        specs += wspecs(Wall[:, :, 512:1024], w_in, 8, 512, KR)
        specs += wspecs(Wall[:, :, 1536:2048], w_in, 8, 512, VA)
        specs += wspecs(Wall[:, :, 2048:2560], w_in, 8, 512, VR)
        specs += wspecs(Wown[:, :, 0:512], w_in, 8, 512, QA)
        specs += wspecs(Wown[:, :, 512:1024], w_in, 8, 512, QR)
        specs += wspecs(Wown[:, :, 1536:2048], w_in, 8, 512, GR)
        for h in range(4):
            specs += wspecs(Wall[:, :, 1024 + h * 128:1024 + h * 128 + 64], w_in, 8, 64, KR + h * 128 + 64)
            specs += wspecs(Wall[:, :, 1024 + h * 128 + 64:1024 + h * 128 + 128], w_in, 8, 64, KR + h * 128)
            specs += wspecs(Wown[:, :, 1024 + h * 128:1024 + h * 128 + 64], w_in, 8, 64, QR + h * 128 + 64)
            specs += wspecs(Wown[:, :, 1024 + h * 128 + 64:1024 + h * 128 + 128], w_in, 8, 64, QR + h * 128)
        load_weights(specs, stage)

        xs = [A.get([128, D], F32, f"xs{i}") for i in range(2)]
        F1 = A.get([128, D], F32, "F1")
        xn = A.get([128, D], BF16, "xn")
        hTs = [A.get([128, 8, 128], BF16, f"hT{i}") for i in range(2)]
        ss = A.get([128, 4], F32, "ss")
        rots = [A.get([128, 256], F32, f"rot{i}") for i in range(2)]
        kaT = [A.get([128, 4, 128], BF16, f"kaT{i}") for i in range(2)]
        vab = [A.get([128, 512], BF16, f"vab{i}") for i in range(2)]
        kout = [A.get([128, 512], F32, f"kout{i}") for i in range(2)]
        vout = [A.get([128, 512], F32, f"vout{i}") for i in range(2)]
        t1 = A.get([128, 4, 128], F32, "t1")
        t2 = A.get([128, 4, 128], F32, "t2")
        krot = A.get([128, 4, 128], BF16, "krot")
        krz = A.get([128, 4, 128], BF16, "krz")
        vrb = A.get([128, 512], BF16, "vrb")
        qb = [A.get([128, 4, 128], BF16, f"qb{i}") for i in range(2)]
        qrf = A.get([128, 4, 128], F32, "qrf")
        qrb = A.get([128, 4, 128], BF16, "qrb")
        qxb = A.get([128, 4, 128], BF16, "qxb")
        PTr = A.get([128, 4, 128], BF16, "PTr")
        orr = A.get([128, 4, 128], F32, "orr")
        sq = A.get([128, 512], BF16, "sq")
        sd = A.get([128, 512], F32, "sd")
        gsl = A.get([128, 4, 128], F32, "gsl")
        yrb = [A.get([128, 4, 128], BF16, f"yrb{i}") for i in range(2)]
        ckf = A.get([128, 512], F32, "ckf")
        ckb = A.get([128, 512], BF16, "ckb")

        def norm_transpose(xt, n, Acol, Bcol, AcolO, hT):
            op("act", lambda e: e.activation(out=F1.t[0:n, :], in_=xt.t[0:n, :], func=AF.Square, accum_out=ss.t[0:n, 0:1]),
               reads=[xt.o], writes=[F1.o, ss.o])
            op("act", lambda e: e.activation(out=ss.t[0:n, 1:2], in_=ss.t[0:n, 0:1], func=AF.Sqrt, scale=1.0 / D, bias=epsc.t[0:n, :]),
               reads=[ss.o, epsc.o], writes=[ss.o])
            op("dve", lambda e: e.reciprocal(out=ss.t[0:n, 2:3], in_=ss.t[0:n, 1:2]), reads=[ss.o], writes=[ss.o])
            op("dve", lambda e: e.tensor_scalar(out=xn.t[0:n, :], in0=xt.t[0:n, :], scalar1=ss.t[0:n, 2:3], scalar2=None, op0=mult),
               reads=[xt.o, ss.o], writes=[xn.o])
            for c in range(8):
                op("pe", lambda e, c=c: e.transpose(out=s0b[:, c * 128:c * 128 + n], in_=xn.t[0:n, c * 128:(c + 1) * 128],
                                                    identity=identb.t[0:n, 0:n]),
                   reads=[xn.o, identb.o], writes=[s0.o])
            for c in range(8):
                op("dve" if c % 2 == 0 else "act",
                   (lambda e, c=c: e.tensor_scalar(out=hT.t[:, c, 0:n], in0=s0b[:, c * 128:c * 128 + n],
                                                   scalar1=Acol[:, c:c + 1], scalar2=Bcol[:, c:c + 1], op0=mult, op1=add))
                   if c % 2 == 0 else
                   (lambda e, c=c: e.activation(out=hT.t[:, c, 0:n], in_=s0b[:, c * 128:c * 128 + n], func=AF.Identity,
                                                scale=Acol[:, c:c + 1], bias=Bcol[:, c:c + 1])),
                   reads=[s0.o, AcolO], writes=[hT.o])

        def fm_proj(bank, Wt, col0, nchunks, hT, n, reads_extra=()):
            for j in range(nchunks):
                for k in range(8):
                    op("pe", lambda e, j=j, k=k: e.matmul(bank.t[:, j * 128:j * 128 + n],
                                                          lhsT=Wt[:, k, col0 + j * 128:col0 + (j + 1) * 128],
                                                          rhs=hT.t[:, k, 0:n], start=(k == 0), stop=(k == 7)),
                       reads=[hT.o, Owb], writes=[bank.o])

        def tm_proj(bank, Wt, col0, hT, n):
            for k in range(8):
                op("pe", lambda e, k=k: e.matmul(bank.t[0:n, :], lhsT=hT.t[:, k, 0:n], rhs=Wt[:, k, col0:col0 + 512],
                                                 start=(k == 0), stop=(k == 7)),
                   reads=[hT.o, Owb], writes=[bank.o])

        def v4(bank, n):
            return bank.t[:, :].rearrange("p (a b) -> p a b", a=4)[:, :, 0:n]

        def rotate(bx, bxs, rt, n, out_f32):
            Cb = bcast(rt.t[:, 0:n], [('b', 4), ('x',)])
            Sb_ = bcast(rt.t[:, 128:128 + n], [('b', 4), ('x',)])
            op("dve", lambda e: e.tensor_tensor(out=t1.t[:, :, 0:n], in0=v4(bx, n), in1=Cb, op=mult),
               reads=[bx.o, rt.o], writes=[t1.o])
            op("dve", lambda e: e.tensor_tensor(out=t2.t[:, :, 0:n], in0=v4(bxs, n), in1=Sb_, op=mult),
               reads=[bxs.o, rt.o], writes=[t2.o])
            op("pool", lambda e: e.tensor_tensor(out=out_f32.t[:, :, 0:n], in0=t1.t[:, :, 0:n], in1=t2.t[:, :, 0:n], op=add),
               reads=[t1.o, t2.o], writes=[out_f32.o])

        def p1_tile(s, n, own_idx, sample):
            slot = s % 2
            xt = xs[slot]
            hT = hTs[slot]
            rt = rots[slot]
            g = 1 if sample else 0
            if sample:
                dma("sp", lambda e: e.dma_start(out=xt.t[0:32, :], in_=x_s), writes=[xt.o], key=f"xs{slot}")
            else:
                ld(xt, x_all[s * 128:(s + 1) * 128, :], f"xs{slot}")
            ld(rt, rot[NT if sample else s], f"rot{slot}")
            if (not sample) and s < 12:
                Acol, Bcol, AO = ABv.t[:, s // 4, 0, :], ABv.t[:, s // 4, 1, :], ABv.o
            else:
                Acol, Bcol, AO = AB.t[:, g, 0, :], AB.t[:, g, 1, :], AB.o
            norm_transpose(xt, n, Acol, Bcol, AO, hT)
            St, Sbt = (Ss, Ssb) if sample else (S, Sb)
            dto = 1032 if sample else 0
            xio = 1160 if sample else 512
            zeo = 1288 if sample else 1024
            dtw = 32 if sample else 128
            fm_proj(pA0, Wall, 0, 4, hT, n)
            kt = kaT[slot]
            op("act", lambda e: e.copy(out=kt.t[:, :, 0:n], in_=v4(pA0, n)), reads=[pA0.o], writes=[kt.o])
            if sample:
                dma("pool", lambda e: e.dma_start(out=KSs[:, :, 1024:1056].rearrange("h p t -> p h t"), in_=kt.t[:, :, 0:32]),
                    reads=[kt.o], key=f"kaT{slot}")
            else:
                dma("pool", lambda e: e.dma_start(out=Kscr[:, :, s * 128:(s + 1) * 128].rearrange("h p t -> p h t"), in_=kt.t[:]),
                    reads=[kt.o], key=f"kaT{slot}")
            tm_proj(pB1, Wall, 1536, hT, n)
            vb_ = vab[slot]
            op("act", lambda e: e.copy(out=vb_.t[0:n, :], in_=pB1.t[0:n, :]), reads=[pB1.o], writes=[vb_.o])
            if sample:
                dma("pool", lambda e: e.dma_start(out=VSs[:, 0:32, 8, :].rearrange("h p d -> p h d"),
                                                  in_=vb_.t[0:32, :].rearrange("p (h d) -> p h d", h=4)),
                    reads=[vb_.o], key=f"vab{slot}")
            else:
                dma("pool", lambda e: e.dma_start(out=Vscr[:, :, s, :].rearrange("h p d -> p h d"),
                                                  in_=vb_.t[:, :].rearrange("p (h d) -> p h d", h=4)),
                    reads=[vb_.o], key=f"vab{slot}")
            if own_idx is not None:
                vo = vout[own_idx % 2]
                op("dve", lambda e: e.tensor_copy(out=vo.t[0:n, :], in_=pB1.t[0:n, :]), reads=[pB1.o], writes=[vo.o])
                dst = v_s if sample else v_own[own_idx * 128:(own_idx + 1) * 128, :]
                dma("pool", lambda e, dst=dst: e.dma_start(out=dst, in_=vo.t[0:n, :]), reads=[vo.o], key=f"vout{own_idx % 2}")
                tm_proj(pC1, Wall, 0, hT, n)
                ko = kout[own_idx % 2]
                op("act", lambda e: e.copy(out=ko.t[0:n, :], in_=pC1.t[0:n, :]), reads=[pC1.o], writes=[ko.o])
                dst = k_s if sample else k_own[own_idx * 128:(own_idx + 1) * 128, :]
                dma("pool", lambda e, dst=dst: e.dma_start(out=dst, in_=ko.t[0:n, :]), reads=[ko.o], key=f"kout{own_idx % 2}")
            fm_proj(pA1, Wall, 512, 4, hT, n)
            fm_proj(pB0, Wall, 1024, 4, hT, n)
            rotate(pA1, pB0, rt, n, qrf)
            op("act", lambda e: e.copy(out=krot.t[:, :, 0:n], in_=qrf.t[:, :, 0:n]), reads=[qrf.o], writes=[krot.o])
            for h in range(4):
                op("pe", lambda e, h=h: e.transpose(out=s0b[0:n, h * 128:(h + 1) * 128], in_=krot.t[:, h, 0:n], identity=identb.t[:]),
                   reads=[krot.o, identb.o], writes=[s0.o])
            for h in range(4):
                op("dve", lambda e, h=h: e.tensor_scalar(out=krz.t[0:n, h, :], in0=s0b[0:n, h * 128:(h + 1) * 128],
                                                         scalar1=dtab.t[0:n, zeo + h:zeo + h + 1], scalar2=None, op0=mult),
                   reads=[s0.o, dtab.o], writes=[krz.o])
            tm_proj(pC0, Wall, 2048, hT, n)
            op("act", lambda e: e.copy(out=vrb.t[0:n, :], in_=pC0.t[0:n, :]), reads=[pC0.o], writes=[vrb.o])
            if own_idx is not None and int(os.environ.get("MK_CUT", "99")) >= 1:
                fm_proj(pA0, Wown, 0, 4, hT, n)
                q_ = qb[own_idx % 2]
                if sample:
                    op("act", lambda e: e.activation(out=qs_T.t[:, :, :], in_=v4(pA0, 32), func=AF.Copy, scale=0.125),
                       reads=[pA0.o], writes=[qs_T.o])
                else:
                    op("act", lambda e: e.activation(out=q_.t[:], in_=v4(pA0, 128), func=AF.Copy, scale=0.125),
                       reads=[pA0.o], writes=[q_.o])
                    dma("pool", lambda e: e.dma_start(out=Qscr[:, :, own_idx * 128:(own_idx + 1) * 128].rearrange("h p t -> p h t"),
                                                      in_=q_.t[:]), reads=[q_.o], key=f"qb{own_idx % 2}")
                if int(os.environ.get("MK_CUT", "99")) >= 2:
                    fm_proj(pA1, Wown, 512, 4, hT, n)
                    fm_proj(pB0, Wown, 1024, 4, hT, n)
                    rotate(pA1, pB0, rt, n, qrf)
                    op("act", lambda e: e.copy(out=qrb.t[:, :, 0:n], in_=qrf.t[:, :, 0:n]), reads=[qrf.o], writes=[qrb.o])
                    XIv = dtab.t[:, xio:xio + 4 * dtw].rearrange("p (a b) -> p a b", a=4)
                    op("pool", lambda e: e.tensor_tensor(out=qxb.t[:, :, 0:n], in0=qrf.t[:, :, 0:n], in1=XIv[:, :, 0:n], op=mult),
                       reads=[qrf.o, dtab.o], writes=[qxb.o])
                    for h in range(4):
                        op("pe", lambda e, h=h: e.matmul(pC1.t[0:n, h * 128:h * 128 + n], lhsT=krot.t[:, h, 0:n], rhs=qrb.t[:, h, 0:n],
                                                         start=True, stop=True), reads=[krot.o, qrb.o], writes=[pC1.o])
                    DTv = dtab.t[:, dto:dto + 4 * dtw].rearrange("p (a b) -> p a b", a=4)
                    op("dve", lambda e: e.tensor_tensor(out=PTr.t[0:n, :, 0:n], in0=v4(pC1, n)[0:n], in1=DTv[0:n, :, 0:n], op=mult),
                       reads=[pC1.o, dtab.o], writes=[PTr.o])
                    for h in range(4):
                        op("pe", lambda e, h=h: e.matmul(s7.t[:, h * 128:h * 128 + n], lhsT=vrb.t[0:n, h * 128:(h + 1) * 128],
                                                         rhs=PTr.t[0:n, h, 0:n], start=True, stop=False),
                           reads=[vrb.o, PTr.o], writes=[s7.o])
                        op("pe", lambda e, h=h: e.matmul(s7.t[:, h * 128:h * 128 + n], lhsT=Sbt.t[:, h, :],
                                                         rhs=qxb.t[:, h, 0:n], start=False, stop=True),
                           reads=[Sbt.o, qxb.o], writes=[s7.o])
                    op("act", lambda e: e.copy(out=orr.t[:, :, 0:n], in_=v4(s7, n)), reads=[s7.o], writes=[orr.o])
                    op("act", lambda e: e.activation(out=sq.t[:, 0:512].rearrange("p (a b) -> p a b", a=4)[:, :, 0:n],
                                                     in_=orr.t[:, :, 0:n], func=AF.Square), reads=[orr.o], writes=[sq.o])
                    for h in range(4):
                        op("pe", lambda e, h=h: e.matmul(pC1.t[:, h * 128:h * 128 + n], lhsT=onesb.t[:, :],
                                                         rhs=sq.t[:, h * 128:h * 128 + n], start=True, stop=True),
                           reads=[onesb.o, sq.o], writes=[pC1.o])
                    sdv = sd.t[:, :].rearrange("p (a b) -> p a b", a=4)[:, :, 0:n]
                    op("act", lambda e: e.activation(out=sdv, in_=v4(pC1, n), func=AF.Sqrt, scale=1.0 / 128, bias=epsc.t[:, :]),
                       reads=[pC1.o, epsc.o], writes=[sd.o])
                    op("dve", lambda e: e.reciprocal(out=sdv, in_=sdv), reads=[sd.o], writes=[sd.o])
                    op("dve", lambda e: e.scalar_tensor_tensor(out=orr.t[:, :, 0:n], in0=orr.t[:, :, 0:n], scalar=sub.t[:, 1:2],
                                                               in1=sdv, op0=mult, op1=mult),
                       reads=[orr.o, sub.o, sd.o], writes=[orr.o])
                    fm_proj(pA0, Wown, 1536, 4, hT, n)
                    op("act", lambda e: e.activation(out=gsl.t[:, :, 0:n], in_=v4(pA0, n), func=AF.Silu), reads=[pA0.o], writes=[gsl.o])
                    if sample:
                        op("pool", lambda e: e.tensor_tensor(out=yrs_T.t[:, :, :], in0=orr.t[:, :, 0:32], in1=gsl.t[:, :, 0:32], op=mult),
                           reads=[orr.o, gsl.o], writes=[yrs_T.o])
                    else:
                        y_ = yrb[own_idx % 2]
                        op("pool", lambda e: e.tensor_tensor(out=y_.t[:], in0=orr.t[:], in1=gsl.t[:], op=mult),
                           reads=[orr.o, gsl.o], writes=[y_.o])
                        dma("pool", lambda e: e.dma_start(out=YRscr[:, :, own_idx * 128:(own_idx + 1) * 128].rearrange("h p t -> p h t"),
                                                          in_=y_.t[:]), reads=[y_.o], key=f"yrb{own_idx % 2}")
            L = 32 if sample else 128
            for h in range(4):
                op("pe", lambda e, h=h: e.matmul(s7.t[:, h * 128:(h + 1) * 128], lhsT=krz.t[0:n, h, :],
                                                 rhs=vrb.t[0:n, h * 128:(h + 1) * 128], start=True, stop=True),
                   reads=[krz.o, vrb.o], writes=[s7.o])
            for h in range(4):
                op("dve", lambda e, h=h: e.scalar_tensor_tensor(out=St.t[:, h, :], in0=St.t[:, h, :], scalar=float(GAMMAS[h] ** L),
                                                                in1=s7.t[:, h * 128:(h + 1) * 128], op0=mult, op1=add),
                   reads=[St.o, s7.o], writes=[St.o])
            op("pool", lambda e: e.tensor_copy(out=Sbt.t[:], in_=St.t[:]), reads=[St.o], writes=[Sbt.o])

        _tiles = [int(v) for v in os.environ["MK_TILES"].split(",")] if os.environ.get("MK_TILES") else list(range(NT))
        for s in _tiles:
            p1_tile(s, 128, OWN.index(s) if s in OWN else None, False)
        dma("pool", lambda e: e.dma_start(out=ret_p.rearrange("h k v -> k h v"), in_=S.t[:]), reads=[S.o], key="retp")

        if os.environ.get("MK_NOSAMPLE"):
            P.barrier()
            P.wait_all_dma("sp")
            P.emit()
            return nc
        ld(Ss, state_in.rearrange("h k v -> k h v"), "Ss")
        op("pool", lambda e: e.tensor_copy(out=Ssb.t[:], in_=Ss.t[:]), reads=[Ss.o], writes=[Ssb.o])
        for blk in range(8):
            ld(ckf, cache_k[blk * 128:(blk + 1) * 128, :], "ckf")
            op("pool", lambda e: e.tensor_copy(out=ckb.t[:], in_=ckf.t[:]), reads=[ckf.o], writes=[ckb.o])
            for h in range(4):
                op("pe", lambda e, h=h: e.transpose(out=s0b[:, h * 128:(h + 1) * 128], in_=ckb.t[:, h * 128:(h + 1) * 128],
                                                    identity=identb.t[:]), reads=[ckb.o, identb.o], writes=[s0.o])
            kt = kaT[blk % 2]
            op("act", lambda e, kt=kt: e.copy(out=kt.t[:], in_=s0b[:, 0:512].rearrange("p (a b) -> p a b", a=4)),
               reads=[s0.o], writes=[kt.o])
            dma("pool", lambda e, kt=kt, blk=blk: e.dma_start(out=KSs[:, :, blk * 128:(blk + 1) * 128].rearrange("h p t -> p h t"),
                                                             in_=kt.t[:]), reads=[kt.o], key=f"kaT{blk % 2}")
            ld(ckf, cache_v[blk * 128:(blk + 1) * 128, :], "ckf")
            vb_ = vab[blk % 2]
            op("pool", lambda e, vb_=vb_: e.tensor_copy(out=vb_.t[:], in_=ckf.t[:]), reads=[ckf.o], writes=[vb_.o])
            dma("pool", lambda e, vb_=vb_, blk=blk: e.dma_start(out=VSs[:, :, blk, :].rearrange("h p d -> p h d"),
                                                               in_=vb_.t[:, :].rearrange("p (h d) -> p h d", h=4)),
                reads=[vb_.o], key=f"vab{blk % 2}")
        p1_tile(NT, 32, 32, True)
        dma("pool", lambda e: e.dma_start(out=ret_s.rearrange("h k v -> k h v"), in_=Ss.t[:]), reads=[Ss.o], key="rets")
        P.barrier()
        if STOP == "p1":
            P.wait_all_dma("sp")
            P.emit()
            return nc

        W.reset()
        Kt = [W.get([128, 1024], BF16, f"Kt{i}") for i in range(2)]
        Vc = [W.get([128, 8, 128], BF16, f"Vc{i}") for i in range(2)]
        Qt = [W.get([128, 512], BF16, f"Qt{i}") for i in range(2)]
        PT = [W.get([128, 512], BF16, f"PT{i}") for i in range(6)]
        sacc = [W.get([128, 512], F32, f"sacc{i}") for i in range(2)]
        onesf = W.get([128, 128], F32, "onesf")
        shi = W.get([128, 512], BF16, "shi")
        slo = W.get([128, 512], BF16, "slo")
        tmpf = [W.get([128, 512], F32, f"tmpf{i}") for i in range(2)]
        BT = [W.get([128, 512], F32, f"BT{i}") for i in range(5)]
        Hk = W.get([128, 512], F32, "Hk")
        mcol = W.get([128, 1024], F32, "mcol")
        fR = W.get([128, 512], F32, "fR")
        fo0 = W.get([128, 512], F32, "fo0")
        fo1 = W.get([128, 512], F32, "fo1")
        foa = W.get([128, 512], F32, "foa")
        fsd = W.get([128, 512], F32, "fsd")
        fsq = W.get([128, 512], BF16, "fsq")
        yab = [W.get([128, 512], BF16, f"yab{i}") for i in range(2)]
        zc = W.get([128, 1], F32, "zc")
        ld(mcol, mcol_d, "mcol")
        op("pool", lambda e: e.memset(zc.t[:], 0.0), writes=[zc.o])
        op("pool", lambda e: e.memset(onesf.t[:], 1.0), writes=[onesf.o])
        STb = [s0, s7, pC0, pC1]
        cnt = {"st": 0, "pt": 0, "ch": 0, "q": 0, "tf": 0, "ya": 0}

        def attend(h, Qap, Qo, nq, blocks, out_cb):
            accO = [pA0, pB0]
            accL = [pA1, pB1]
            nb = len(blocks)
            pend = {}

            def stage1(bi):
                b = blocks[bi]
                nk = b["nk"]
                Kap, Ko = b["K"]
                stbs, pts = [], []
                for m in range(2):
                    stb = STb[cnt["st"] % 4]; cnt["st"] += 1
                    stbs.append(stb)
                    op("pe", lambda e, m=m, stb=stb: e.matmul(stb.t[0:nk, 0:nq], lhsT=Kap[m * 64:(m + 1) * 64, :],
                                                              rhs=Qap[m * 64:(m + 1) * 64, :], start=True, stop=True),
                       reads=[Ko, Qo], writes=[stb.o])
                for m in range(2):
                    stb = stbs[m]
                    pt = PT[cnt["pt"] % 6]; cnt["pt"] += 1
                    pts.append(pt)
                    if b["bias"] is not None:
                        bap, bo = b["bias"]
                        tf = tmpf[cnt["tf"] % 2]; cnt["tf"] += 1
                        op("dve", lambda e, tf=tf, stb=stb: e.tensor_tensor(out=tf.t[0:nk, 0:nq], in0=stb.t[0:nk, 0:nq], in1=bap, op=add),
                           reads=[stb.o, bo], writes=[tf.o])
                        src, so = tf.t[0:nk, 0:nq], tf.o
                    else:
                        src, so = stb.t[0:nk, 0:nq], stb.o
                    op("act", lambda e, pt=pt, src=src: e.activation(out=pt.t[0:nk, 0:nq], in_=src, func=AF.Exp, bias=b["mc"][0:nk, :]),
                       reads=[so, mcol.o, zc.o], writes=[pt.o])
                    for (r0, r1, c0, c1) in b["zero"]:
                        op("pool", lambda e, pt=pt, r0=r0, r1=r1, c0=c0, c1=c1: e.memset(pt.t[r0:r1, c0:c1], 0.0), writes=[pt.o])
                pend[bi] = pts

            def stage2(bi):
                b = blocks[bi]
                nk = b["nk"]
                pts = pend.pop(bi)
                Vap, Vo = b["V"]
                for m in range(2):
                    pt = pts[m]
                    O_ = accO[m]
                    op("pe", lambda e, pt=pt, O_=O_: e.matmul(O_.t[:, 0:nq], lhsT=Vap, rhs=pt.t[0:nk, 0:nq], start=(bi == 0), stop=(bi == nb - 1)),
                       reads=[Vo, pt.o], writes=[O_.o])
                for m in range(2):
                    pt = pts[m]
                    sa = sacc[m]
                    if bi == 0:
                        op("dve", lambda e, pt=pt, sa=sa: e.tensor_copy(out=sa.t[0:nk, 0:nq], in_=pt.t[0:nk, 0:nq]), reads=[pt.o], writes=[sa.o])
                    else:
                        op("dve", lambda e, pt=pt, sa=sa: e.tensor_tensor(out=sa.t[0:nk, 0:nq], in0=sa.t[0:nk, 0:nq], in1=pt.t[0:nk, 0:nq], op=add),
                           reads=[pt.o, sa.o], writes=[sa.o])

            if blocks[0].get("pre") is not None:
                blocks[0]["pre"]()
            for bi in range(nb + 1):
                if bi < nb:
                    stage1(bi)
                if bi >= 1:
                    stage2(bi - 1)
                if 1 <= bi < nb and blocks[bi].get("pre") is not None:
                    blocks[bi]["pre"]()
            for m, fo in ((0, fo0), (1, fo1)):
                O_, L_ = accO[m], accL[m]
                sa = sacc[m]
                op("dve", lambda e, sa=sa: e.tensor_copy(out=shi.t[:, 0:nq], in_=sa.t[:, 0:nq]), reads=[sa.o], writes=[shi.o])
                op("dve", lambda e, sa=sa: e.tensor_tensor(out=slo.t[:, 0:nq], in0=sa.t[:, 0:nq], in1=shi.t[:, 0:nq], op=sub_),
                   reads=[sa.o, shi.o], writes=[slo.o])
                op("pe", lambda e, L_=L_: e.matmul(L_.t[:, 0:nq], lhsT=onesb.t[:, :], rhs=shi.t[:, 0:nq], start=True, stop=False),
                   reads=[onesb.o, shi.o], writes=[L_.o])
                op("pe", lambda e, L_=L_: e.matmul(L_.t[:, 0:nq], lhsT=onesb.t[:, :], rhs=slo.t[:, 0:nq], start=False, stop=True),
                   reads=[onesb.o, slo.o], writes=[L_.o])
                op("dve", lambda e, L_=L_: e.reciprocal(out=fR.t[:, 0:nq], in_=L_.t[:, 0:nq]), reads=[L_.o], writes=[fR.o])
                op("dve", lambda e, O_=O_, fo=fo: e.tensor_tensor(out=fo.t[:, 0:nq], in0=O_.t[:, 0:nq], in1=fR.t[:, 0:nq], op=mult),
                   reads=[O_.o, fR.o], writes=[fo.o])
            op("dve", lambda e: e.scalar_tensor_tensor(out=foa.t[:, 0:nq], in0=fo1.t[:, 0:nq], scalar=nlam, in1=fo0.t[:, 0:nq],
                                                       op0=mult, op1=add), reads=[fo0.o, fo1.o, lamc.o], writes=[foa.o])
            op("act", lambda e: e.activation(out=fsq.t[:, 0:nq], in_=foa.t[:, 0:nq], func=AF.Square), reads=[foa.o], writes=[fsq.o])
            op("pe", lambda e: e.matmul(pC1.t[:, 0:nq], lhsT=onesb.t[:, :], rhs=fsq.t[:, 0:nq], start=True, stop=True),
               reads=[onesb.o, fsq.o], writes=[pC1.o])
            op("act", lambda e: e.activation(out=fsd.t[:, 0:nq], in_=pC1.t[:, 0:nq], func=AF.Sqrt, scale=1.0 / 128, bias=epsc.t[:, :]),
               reads=[pC1.o, epsc.o], writes=[fsd.o])
            op("dve", lambda e: e.reciprocal(out=fsd.t[:, 0:nq], in_=fsd.t[:, 0:nq]), reads=[fsd.o], writes=[fsd.o])
            out_cb()

        for h in range(4):
            for j in range(-1, 4):
                src = bass.AP(Gscr, h * 1280 + (R0 - 127 - 128 * j), ((1, 128), (1, 512)))
                ld(Hk, src, "Hk", reads=[OG])
                op("pe", lambda e: e.matmul(pC1.t[:, :], lhsT=jrev.t[:, :], rhs=Hk.t[:, :], start=True, stop=True),
                   reads=[jrev.o, Hk.o], writes=[pC1.o])
                bt = BT[j + 1]
                op("act", lambda e, bt=bt: e.copy(out=bt.t[:], in_=pC1.t[:, :]), reads=[pC1.o], writes=[bt.o])
            for i in range(8):
                sg = 4 * i + 3
                nblk = 4 * sg + 4
                q_ = Qt[cnt["q"] % 2]; cnt["q"] += 1
                ld(q_, Qscr[h, :, i * 512:(i + 1) * 512], q_.o.name, reads=[OQ_])
                blocks = []
                base = cnt["ch"]
                nch = nblk // 8
                cnt["ch"] += nch

                def issue(ci, h=h, base=base):
                    kc_ = Kt[(base + ci) % 2]; vc_ = Vc[(base + ci) % 2]
                    ld(kc_, Kscr[h, :, ci * 1024:(ci + 1) * 1024], kc_.o.name, reads=[OK_])
                    ld(vc_, Vscr[h, :, ci * 8:(ci + 1) * 8, :], vc_.o.name, reads=[OV_])

                for kb in range(nblk):
                    ci = kb // 8
                    kc = Kt[(base + ci) % 2]; vc = Vc[(base + ci) % 2]
                    kl = kb % 8
                    j = kb - 4 * sg
                    b = dict(K=(kc.t[:, kl * 128:(kl + 1) * 128], kc.o), V=(vc.t[:, kl, :], vc.o), nk=128,
                             bias=None, mc=mcol.t[:, i * 128 + kb:i * 128 + kb + 1], zero=[], pre=None)
                    if kb == 0:
                        b["pre"] = (lambda issue=issue, nch=nch: [issue(c_) for c_ in range(min(2, nch))])
                    elif kl == 0 and ci + 1 < nch:
                        b["pre"] = (lambda issue=issue, ci=ci: issue(ci + 1))
                    if j >= -1:
                        b["bias"] = (BT[j + 1].t[:, :], BT[j + 1].o)
                    if j >= 0:
                        if j > 0:
                            b["zero"].append((0, 128, 0, 128 * j))
                        b["zero"].append((64, 128, 128 * j, 128 * j + 64))
                    blocks.append(b)

                def out_cb(i=i, h=h):
                    ya = yab[cnt["ya"] % 2]; cnt["ya"] += 1
                    op("dve", lambda e: e.scalar_tensor_tensor(out=ya.t[:, :], in0=foa.t[:, :], scalar=sub.t[:, 0:1], in1=fsd.t[:, :],
                                                               op0=mult, op1=mult), reads=[foa.o, sub.o, fsd.o], writes=[ya.o])
                    dma("pool", lambda e: e.dma_start(out=YAscr[h, :, i * 512:(i + 1) * 512], in_=ya.t[:, :]),
                        reads=[ya.o], key=ya.o.name)

                attend(h, q_.t[:, :], q_.o, 512, blocks, out_cb)
            kc = Kt[cnt["ch"] % 2]; vc = Vc[cnt["ch"] % 2]; cnt["ch"] += 1
            ld(kc, KSs[h, :, 0:1024], kc.o.name, reads=[OKS])
            ld(vc, VSs[h, :, 0:8, :], vc.o.name, reads=[OVS])
            kc2 = Kt[cnt["ch"] % 2]; vc2 = Vc[cnt["ch"] % 2]; cnt["ch"] += 1
            dma("sp", lambda e, kc2=kc2, h=h: e.dma_start(out=kc2.t[:, 0:32], in_=KSs[h, :, 1024:1056]), reads=[OKS], writes=[kc2.o],
                key=kc2.o.name)
            dma("sp", lambda e, vc2=vc2, h=h: e.dma_start(out=vc2.t[0:32, 0, :], in_=VSs[h, 0:32, 8, :]), reads=[OVS], writes=[vc2.o],
                key=vc2.o.name)
            blocks = []
            for kb in range(8):
                b = dict(K=(kc.t[:, kb * 128:(kb + 1) * 128], kc.o), V=(vc.t[:, kb, :], vc.o), nk=128, bias=None, mc=zc.t[:, 0:1], zero=[])
                if kb == 7:
                    b["bias"] = (BT[0].t[:, 0:32], BT[0].o)
                blocks.append(b)
            blocks.append(dict(K=(kc2.t[:, 0:32], kc2.o), V=(vc2.t[0:32, 0, :], vc2.o), nk=32,
                               bias=(BT[1].t[0:32, 0:32], BT[1].o), mc=zc.t[:, 0:1], zero=[]))

            def out_cb_s(h=h):
                op("dve", lambda e: e.scalar_tensor_tensor(out=yas_T.t[:, h, :], in0=foa.t[:, 0:32], scalar=sub.t[:, 0:1],
                                                           in1=fsd.t[:, 0:32], op0=mult, op1=mult),
                   reads=[foa.o, sub.o, fsd.o], writes=[yas_T.o])

            attend(h, qs_T.t[:, h, :], qs_T.o, 32, blocks, out_cb_s)
        P.barrier()
        if STOP == "p2":
            P.wait_all_dma("sp")
            P.emit()
            return nc

        A.reset()
        W.reset()
        Wg = W.get([128, 8, 2048], BF16, "Wg").t
        Wo = W.get([128, 8, 1024], BF16, "Wo").t
        Wpq = W.get([128, 8, 2048], BF16, "Wpq").t
        Wba = W.get([128, 4, 1024], BF16, "Wba").t
        Wbr = W.get([128, 4, 1024], BF16, "Wbr").t
        craw = A.get([128, 2, 8, 128], F32, "cand")
        stage3 = []
        for i in range(2):
            sg_ = T(craw.t[:, i], "cand")
            sg_.o = craw.o
            stage3.append(sg_)
        NG = 5
        uvb = [A.get([128, 2 * D], BF16, f"uv{i}") for i in range(NG)]
        acol = [A.get([128, 1], F32, f"acol{i}") for i in range(NG)]
        gcol = [A.get([128, 1], F32, f"gcol{i}") for i in range(NG)]
        specs = []
        specs += wspecs(Wg, w_in, 8, 2048, GA, piece=128)
        specs += wspecs(Wo, w_o, 8, 1024, 0, piece=128)
        specs += wspecs(Wpq, w_pq, 8, 2048, 0, piece=128)
        specs += wspecs(Wba, w_ba, 4, 1024, 0, piece=128)
        specs += wspecs(Wbr, w_br, 4, 1024, 0, piece=128)
        load_weights(specs, stage3)
        xs3 = A.get([128, D], F32, "xs3")
        xnew = A.get([128, D], F32, "xnew")
        F3 = A.get([128, D], F32, "F1b")
        xn3 = A.get([128, D], BF16, "xnb")
        hT3 = A.get([128, 8, 128], BF16, "hT3")
        h2T = A.get([128, 8, 128], BF16, "h2T")
        ss3 = A.get([128, 4], F32, "ss3")
        sga = A.get([128, 8, 128], BF16, "sga")
        sgb = A.get([128, 8, 128], BF16, "sgb")
        yain = A.get([128, 4, 128], BF16, "yain")
        yrin = A.get([128, 4, 128], BF16, "yrin")
        u1 = A.get([128, 4, 128], F32, "u1")
        u2 = A.get([128, 4, 128], F32, "u2")
        yT = A.get([128, 8, 128], BF16, "yT")
        h2tm = A.get([128, D], BF16, "h2tm")
        qT = A.get([128, 16, 128], BF16, "qT")
        wk = A.get([128, 256], F32, "wk")
        mx = A.get([128, 16, 16], F32, "mx")
        ix = A.get([128, 16, 16], U32, "ix")
        ixf = A.get([128, 16, 16], F32, "ixf")
        cand = T(craw.t.rearrange("p a b c -> p (a b c)").rearrange("p (a b c) -> p a b c", a=8, b=16), "cand")
        cand.o = craw.o
        ts_ = A.get([128, 8, 16], F32, "ts")
        pos = A.get([128, 8, 16], U32, "pos")
        piu = A.get([128, 8, 16], U32, "piu")
        pju = A.get([128, 8, 16], U32, "pju")
        pj = A.get([128, 8, 16], F32, "pj")
        pi_ = A.get([128, 8, 16], F32, "pi")
        seli = A.get([128, 8, 16], F32, "seli")
        selj = A.get([128, 8, 16], F32, "selj")
        eidx = A.get([128, 128], F32, "eidx")
        ee = A.get([128, 8, 16], F32, "ee")
        esum = A.get([128, 8], F32, "esum")
        gg = A.get([128, 128], F32, "gg")
        eidxT = A.get([128, 128], I32, "eidxT")
        gT = A.get([128, 128], F32, "gT")
        cm = [A.get([128, 128], BF16, f"cm{i}") for i in range(NG)]
        s0b = s0.t[:].bitcast(BF16)
        load_rows(0)

        def norm_transpose3(xt, n, Acol, Bcol, hT):
            op("act", lambda e: e.activation(out=F3.t[0:n, :], in_=xt.t[0:n, :], func=AF.Square, accum_out=ss3.t[0:n, 0:1]),
               reads=[xt.o], writes=[F3.o, ss3.o])
            op("act", lambda e: e.activation(out=ss3.t[0:n, 1:2], in_=ss3.t[0:n, 0:1], func=AF.Sqrt, scale=1.0 / D, bias=epsc.t[0:n, :]),
               reads=[ss3.o, epsc.o], writes=[ss3.o])
            op("dve", lambda e: e.reciprocal(out=ss3.t[0:n, 2:3], in_=ss3.t[0:n, 1:2]), reads=[ss3.o], writes=[ss3.o])
            op("dve", lambda e: e.tensor_scalar(out=xn3.t[0:n, :], in0=xt.t[0:n, :], scalar1=ss3.t[0:n, 2:3], scalar2=None, op0=mult),
               reads=[xt.o, ss3.o], writes=[xn3.o])
            for c in range(8):
                op("pe", lambda e, c=c: e.transpose(out=s0b[:, c * 128:c * 128 + n], in_=xn3.t[0:n, c * 128:(c + 1) * 128],
                                                    identity=identb.t[0:n, 0:n]), reads=[xn3.o, identb.o], writes=[s0.o])
            for c in range(8):
                op("dve", lambda e, c=c: e.tensor_scalar(out=hT.t[:, c, 0:n], in0=s0b[:, c * 128:c * 128 + n],
                                                         scalar1=Acol[:, c:c + 1], scalar2=Bcol[:, c:c + 1], op0=mult, op1=add),
                   reads=[s0.o, AB.o], writes=[hT.o])

        def top16(src_ap, src_o, n, width, out_v, out_i, vo, io):
            op("dve", lambda e: e.max(out=out_v[:, 0:8], in_=src_ap), reads=[src_o], writes=[vo])
            op("dve", lambda e: e.match_replace(out=wk.t[0:n, 0:width], in_to_replace=out_v[:, 0:8], in_values=src_ap, imm_value=-1e30),
               reads=[src_o, vo], writes=[wk.o])
            op("dve", lambda e: e.max(out=out_v[:, 8:16], in_=wk.t[0:n, 0:width]), reads=[wk.o], writes=[vo])
            op("dve", lambda e: e.max_index(out=out_i[:, 0:8], in_max=out_v[:, 0:8], in_values=src_ap), reads=[src_o, vo], writes=[io])
            op("dve", lambda e: e.max_index(out=out_i[:, 8:16], in_max=out_v[:, 8:16], in_values=wk.t[0:n, 0:width]),
               reads=[wk.o, vo], writes=[io])

        def p3_tile(oi, n, sample):
            g = 1 if sample else 0
            A1c, B1c, A2c, B2c = (AB.t[:, g, k, :] for k in range(4))
            if sample:
                dma("sp", lambda e: e.dma_start(out=xs3.t[0:32, :], in_=x_s), writes=[xs3.o], key="xs3")
                ya_ap, yr_ap, ya_o, yr_o = yas_T.t, yrs_T.t, yas_T.o, yrs_T.o
            else:
                s = OWN[oi]
                ld(xs3, x_all[s * 128:(s + 1) * 128, :], "xs3")
                ld(yain, YAscr[:, :, oi * 128:(oi + 1) * 128].rearrange("h p t -> p h t"), "yain", reads=[OYA])
                ld(yrin, YRscr[:, :, oi * 128:(oi + 1) * 128].rearrange("h p t -> p h t"), "yrin", reads=[OYR])
                ya_ap, yr_ap, ya_o, yr_o = yain.t, yrin.t, yain.o, yrin.o
            norm_transpose3(xs3, n, A1c, B1c, hT3)
            for gi, sgt in ((0, sga), (1, sgb)):
                for half, bank in ((0, pA0), (1, pA1)):
                    fm_proj(bank, Wg, gi * 1024 + half * 512, 4, hT3, n)
                    op("act", lambda e, sgt=sgt, half=half, bank=bank: e.activation(out=sgt.t[:, half * 4:(half + 1) * 4, 0:n], in_=v4(bank, n),
                                                                                    func=AF.Sigmoid), reads=[bank.o], writes=[sgt.o])
            for half, (bA, bR) in ((0, (pB0, pC0)), (1, (pB1, pC1))):
                for j in range(4):
                    jj = half * 4 + j
                    for h in range(4):
                        op("pe", lambda e, j=j, jj=jj, h=h, bA=bA: e.matmul(bA.t[:, j * 128:j * 128 + n], lhsT=Wba[:, h, jj * 128:(jj + 1) * 128],
                                                                             rhs=ya_ap[:, h, 0:n], start=(h == 0), stop=(h == 3)),
                           reads=[Owb, ya_o], writes=[bA.o])
                    for h in range(4):
                        op("pe", lambda e, j=j, jj=jj, h=h, bR=bR: e.matmul(bR.t[:, j * 128:j * 128 + n], lhsT=Wbr[:, h, jj * 128:(jj + 1) * 128],
                                                                             rhs=yr_ap[:, h, 0:n], start=(h == 0), stop=(h == 3)),
                           reads=[Owb, yr_o], writes=[bR.o])
                op("dve", lambda e, half=half, bA=bA: e.tensor_tensor(out=u1.t[:, :, 0:n], in0=v4(bA, n), in1=sga.t[:, half * 4:(half + 1) * 4, 0:n],
                                                                      op=mult), reads=[bA.o, sga.o], writes=[u1.o])
                op("dve", lambda e, half=half, bR=bR: e.tensor_tensor(out=u2.t[:, :, 0:n], in0=v4(bR, n), in1=sgb.t[:, half * 4:(half + 1) * 4, 0:n],
                                                                      op=mult), reads=[bR.o, sgb.o], writes=[u2.o])
                op("pool", lambda e, half=half: e.tensor_tensor(out=yT.t[:, half * 4:(half + 1) * 4, 0:n], in0=u1.t[:, :, 0:n], in1=u2.t[:, :, 0:n],
                                                                op=add), reads=[u1.o, u2.o], writes=[yT.o])
            for half, bank in ((0, pA0), (1, pA1)):
                for k in range(8):
                    op("pe", lambda e, k=k, half=half, bank=bank: e.matmul(bank.t[0:n, :], lhsT=yT.t[:, k, 0:n], rhs=Wo[:, k, half * 512:(half + 1) * 512],
                                                                           start=(k == 0), stop=(k == 7)), reads=[yT.o, Owb], writes=[bank.o])
            op("dve", lambda e: e.tensor_tensor(out=F3.t[0:n, :], in0=pA[0:n, :], in1=G1.t[0:n, :], op=mult),
               reads=[pA0.o, pA1.o, G1.o], writes=[F3.o])
            op("pool", lambda e: e.tensor_tensor(out=xnew.t[0:n, :], in0=F3.t[0:n, :], in1=xs3.t[0:n, :], op=add),
               reads=[F3.o, xs3.o], writes=[xnew.o])
            norm_transpose3(xnew, n, A2c, B2c, h2T)
            for c in range(8):
                op("pe", lambda e, c=c: e.transpose(out=s0b[0:n, c * 128:(c + 1) * 128], in_=h2T.t[:, c, 0:n], identity=identb.t[:, :]),
                   reads=[h2T.o, identb.o], writes=[s0.o])
            op("act", lambda e: e.copy(out=h2tm.t[0:n, :], in_=s0b[0:n, :]), reads=[s0.o], writes=[h2tm.o])
            for c4, bank in enumerate((pA0, pA1, pB0, pB1)):
                fm_proj(bank, Wpq, c4 * 512, 4, h2T, n)
                op("act", lambda e, c4=c4, bank=bank: e.copy(out=qT.t[:, c4 * 4:(c4 + 1) * 4, 0:n], in_=v4(bank, n)), reads=[bank.o], writes=[qT.o])
            banks4 = (pA0, pA1, pB0, pB1)
            for c in range(16):
                bank = banks4[c // 4]
                op("pe", lambda e, c=c, bank=bank: e.matmul(bank.t[0:n, (c % 4) * 128:(c % 4 + 1) * 128], lhsT=qT.t[:, c, 0:n], rhs=keysT.t[:, c, :],
                                                            start=True, stop=True), reads=[qT.o, keysT.o], writes=[bank.o])
            for c in range(16):
                bank = banks4[c // 4]
                top16(bank.t[0:n, (c % 4) * 128:(c % 4 + 1) * 128], bank.o, n, 128, mx.t[0:n, c, :], ix.t[0:n, c, :], mx.o, ix.o)
            op("dve", lambda e: e.tensor_copy(out=ixf.t[0:n], in_=ix.t[0:n]), reads=[ix.o], writes=[ixf.o])

            def mxv(off, pat):
                base = mx.t[0:n, 0, 0:1]
                return bass.AP(base.tensor, base.offset + off, (base.ap[0],) + pat)

            def ixv(off, pat):
                base = ixf.t[0:n, 0, 0:1]
                return bass.AP(base.tensor, base.offset + off, (base.ap[0],) + pat)

            op("dve", lambda e: e.tensor_tensor(out=cand.t[0:n], in0=mxv(0, ((32, 8), (1, 16), (0, 16))), in1=mxv(16, ((32, 8), (0, 16), (1, 16))),
                                                op=add), reads=[mx.o], writes=[cand.o])
            for h in range(8):
                top16(cand.t[0:n, h].rearrange("p a b -> p (a b)"), cand.o, n, 256, ts_.t[0:n, h, :], pos.t[0:n, h, :], ts_.o, pos.o)
            t0 = bass.AP(ts_.t[0:n, 0, 0:1].tensor, ts_.t[0:n, 0, 0:1].offset, (ts_.t[0:n, 0, 0:1].ap[0], (16, 8), (0, 16)))
            op("dve", lambda e: e.tensor_tensor(out=ee.t[0:n], in0=ts_.t[0:n], in1=t0, op=sub_), reads=[ts_.o], writes=[ee.o])
            op("act", lambda e: e.activation(out=ee.t[0:n], in_=ee.t[0:n], func=AF.Exp), reads=[ee.o], writes=[ee.o])
            op("dve", lambda e: e.reduce_sum(out=esum.t[0:n, :], in_=ee.t[0:n], axis=AX.X), reads=[ee.o], writes=[esum.o])
            op("dve", lambda e: e.reciprocal(out=esum.t[0:n, :], in_=esum.t[0:n, :]), reads=[esum.o], writes=[esum.o])
            e0 = bass.AP(esum.t[0:n, 0:1].tensor, esum.t[0:n, 0:1].offset, (esum.t[0:n, 0:1].ap[0], (1, 8), (0, 16)))
            op("dve", lambda e: e.tensor_tensor(out=gg.t[0:n, :].rearrange("p (a b) -> p a b", a=8), in0=ee.t[0:n], in1=e0, op=mult),
               reads=[ee.o, esum.o], writes=[gg.o])
            op("dve", lambda e: e.tensor_scalar(out=piu.t[0:n], in0=pos.t[0:n], scalar1=4, scalar2=None, op0=ALU.logical_shift_right),
               reads=[pos.o], writes=[piu.o])
            op("dve", lambda e: e.tensor_scalar(out=pju.t[0:n], in0=pos.t[0:n], scalar1=15, scalar2=None, op0=ALU.bitwise_and),
               reads=[pos.o], writes=[pju.o])
            op("dve", lambda e: e.tensor_copy(out=pi_.t[0:n], in_=piu.t[0:n]), reads=[piu.o], writes=[pi_.o])
            op("dve", lambda e: e.tensor_copy(out=pj.t[0:n], in_=pju.t[0:n]), reads=[pju.o], writes=[pj.o])
            io_ = iota16.t[0:n, 0:1]
            iotav = bass.AP(io_.tensor, io_.offset, (io_.ap[0], (0, 8), (0, 16), (1, 16)))
            for (pp, off, sel) in ((pi_, 0, seli), (pj, 16, selj)):
                b_ = pp.t[0:n, 0, 0:1]
                pv = bass.AP(b_.tensor, b_.offset, (b_.ap[0], (16, 8), (1, 16), (0, 16)))
                op("dve", lambda e, pv=pv: e.tensor_tensor(out=cand.t[0:n], in0=pv, in1=iotav, op=ALU.is_equal),
                   reads=[pp.o, iota16.o], writes=[cand.o])
                op("dve", lambda e, off=off: e.tensor_tensor(out=cand.t[0:n], in0=cand.t[0:n], in1=ixv(off, ((32, 8), (0, 16), (1, 16))), op=mult),
                   reads=[cand.o, ixf.o], writes=[cand.o])
                op("dve", lambda e, sel=sel: e.reduce_sum(out=sel.t[0:n], in_=cand.t[0:n], axis=AX.X), reads=[cand.o], writes=[sel.o])
            op("dve", lambda e: e.scalar_tensor_tensor(out=eidx.t[0:n, :].rearrange("p (a b) -> p a b", a=8), in0=seli.t[0:n], scalar=128.0,
                                                       in1=selj.t[0:n], op0=mult, op1=add), reads=[seli.o, selj.o], writes=[eidx.o])
            op("pe", lambda e: e.transpose(out=s7.t[:, 0:n], in_=eidx.t[0:n, :], identity=identf.t[0:n, 0:n]), reads=[eidx.o, identf.o], writes=[s7.o])
            op("dve", lambda e: e.tensor_copy(out=eidxT.t[:, 0:n], in_=s7.t[:, 0:n]), reads=[s7.o], writes=[eidxT.o])
            op("pe", lambda e: e.transpose(out=s7.t[:, 128:128 + n], in_=gg.t[0:n, :], identity=identf.t[0:n, 0:n]), reads=[gg.o, identf.o], writes=[s7.o])
            op("act", lambda e: e.copy(out=gT.t[:, 0:n], in_=s7.t[:, 128:128 + n]), reads=[s7.o], writes=[gT.o])
            UVsrc = UVbf.ap().rearrange("e t d -> e (t d)")

            def st_gather(t):
                uv = uvb[t % NG]
                dma("pool", lambda e: e.indirect_dma_start(out=uv.t[:, :], out_offset=None, in_=UVsrc,
                                                           in_offset=bass.IndirectOffsetOnAxis(ap=eidxT.t[:, t:t + 1], axis=0)),
                    reads=[eidxT.o], writes=[uv.o], key=f"{uv.o.name}_{oi % 4}")

            def pair(t):
                return (pC, pC0, pC1) if t % 2 == 0 else (pA, pA0, pA1)

            def st_bcast(t):
                idc = identb.t[0:n, t:t + 1]
                selT = bass.AP(idc.tensor, idc.offset, (idc.ap[0], (0, 128)))
                pX, pX0, pX1 = pair(t)
                for half in range(2):
                    op("pe", lambda e, half=half: e.matmul(pX[:, half * 512:(half + 1) * 512], lhsT=selT,
                                                           rhs=h2tm.t[0:n, half * 512:(half + 1) * 512], start=True, stop=True),
                       reads=[identb.o, h2tm.o], writes=[pX0.o if half == 0 else pX1.o])

            def st_dot(t):
                uv, ac, gc = uvb[t % NG], acol[t % NG], gcol[t % NG]
                pX, pX0, pX1 = pair(t)
                op("dve", lambda e: e.scalar_tensor_tensor(out=F3.t[:, :], in0=uv.t[:, 0:D], scalar=1.0, in1=pX[:, :],
                                                           op0=mult, op1=mult, accum_out=ac.t[:, 0:1]),
                   reads=[uv.o, pX0.o, pX1.o], writes=[F3.o, ac.o])
                op("act", lambda e: e.activation(out=gc.t[:, 0:1], in_=ac.t[:, 0:1], func=AF.Gelu), reads=[ac.o], writes=[gc.o])

            def st_cm(t):
                gc, c_ = gcol[t % NG], cm[t % NG]
                op("dve", lambda e: e.tensor_scalar(out=c_.t[:, 0:n], in0=zwin.t[:, 127 - t:127 - t + n], scalar1=gc.t[:, 0:1],
                                                    scalar2=gT.t[:, t:t + 1], op0=mult, op1=mult),
                   reads=[zwin.o, gc.o, gT.o], writes=[c_.o])

            def st_acc(t):
                uv, c_ = uvb[t % NG], cm[t % NG]
                for half, bank in ((0, pB0), (1, pB1)):
                    op("pe", lambda e, half=half, bank=bank: e.matmul(bank.t[0:n, :], lhsT=c_.t[:, 0:n],
                                                                      rhs=uv.t[:, D + half * 512:D + (half + 1) * 512],
                                                                      start=(t == 0), stop=(t == n - 1)),
                       reads=[c_.o, uv.o], writes=[bank.o])

            for t in range(n + 2):
                if t < n:
                    st_gather(t)
                    st_bcast(t)
                    st_dot(t)
                if 1 <= t <= n:
                    st_cm(t - 1)
                if t >= 2:
                    st_acc(t - 2)
            op("dve", lambda e: e.tensor_tensor(out=F3.t[0:n, :], in0=pB[0:n, :], in1=G2.t[0:n, :], op=mult), reads=[pB0.o, pB1.o, G2.o], writes=[F3.o])
            op("pool", lambda e: e.tensor_tensor(out=xnew.t[0:n, :], in0=F3.t[0:n, :], in1=xnew.t[0:n, :], op=add), reads=[F3.o, xnew.o], writes=[xnew.o])
            op("act", lambda e: e.activation(out=F3.t[0:n, :], in_=xnew.t[0:n, :], func=AF.Square, accum_out=ss3.t[0:n, 0:1]), reads=[xnew.o], writes=[F3.o, ss3.o])
            op("act", lambda e: e.activation(out=ss3.t[0:n, 1:2], in_=ss3.t[0:n, 0:1], func=AF.Sqrt, scale=1.0 / D, bias=epsc.t[0:n, :]),
               reads=[ss3.o, epsc.o], writes=[ss3.o])
            op("dve", lambda e: e.reciprocal(out=ss3.t[0:n, 2:3], in_=ss3.t[0:n, 1:2]), reads=[ss3.o], writes=[ss3.o])
            op("dve", lambda e: e.scalar_tensor_tensor(out=F3.t[0:n, :], in0=xnew.t[0:n, :], scalar=ss3.t[0:n, 2:3], in1=FN.t[0:n, :], op0=mult, op1=mult),
               reads=[xnew.o, ss3.o, FN.o], writes=[F3.o])
            dst = y_s if sample else y_own[oi * 128:(oi + 1) * 128, :]
            dma("sp", lambda e: e.dma_start(out=dst, in_=F3.t[0:n, :]), reads=[F3.o], key="yout")

        for oi in range(32):
            p3_tile(oi, 128, False)
        load_rows(1)
        p3_tile(32, 32, True)
        P.wait_all_dma("sp")
        P.emit()
    return nc


def _t5_bucket(rel):
    nb = 16
    ret = 16 if rel > 0 else 0
    n = abs(rel)
    if n < 8:
        return ret + n
    nf = np.float32(max(n, 8))
    large = 8 + int(np.float32(np.log(nf / np.float32(8)) / np.float32(math.log(16.0)) * np.float32(8)))
    return ret + min(large, 15)


def _static_tables(r):
    shift = 3 - r
    inv = (1.0 / (10000.0 ** np.linspace(0.0, 1.0, 64, dtype=np.float32))).astype(np.float32)
    rot = np.zeros((NT + 1, 128, 256), np.float32)
    pos = (np.arange(NT * 128, dtype=np.int64) - shift * 512).clip(0).astype(np.float32)
    ang = (pos[:, None] * inv[None, :]).astype(np.float32).astype(np.float64)
    cos, sin = np.cos(ang).astype(np.float32), np.sin(ang).astype(np.float32)
    C = np.concatenate([cos, cos], 1).T.reshape(128, NT, 128)
    Sg = np.concatenate([-sin, sin], 1).T.reshape(128, NT, 128)
    rot[:NT, :, 0:128] = C.transpose(1, 0, 2)
    rot[:NT, :, 128:256] = Sg.transpose(1, 0, 2)
    ps = (1024 + np.arange(32)).astype(np.float32)
    angs = (ps[:, None] * inv[None, :]).astype(np.float32).astype(np.float64)
    cs, sn = np.cos(angs).astype(np.float32), np.sin(angs).astype(np.float32)
    rot[NT, :, 0:32] = np.concatenate([cs, cs], 1).T
    rot[NT, :, 128:160] = np.concatenate([-sn, sn], 1).T
    dtab = np.zeros((128, 1296), np.float32)
    sc = 128.0 ** -0.5
    for h in range(4):
        lg = math.log(GAMMAS[h])
        for (L, dto, xio, zeo) in ((128, 0, 512, 1024), (32, 1032, 1160, 1288)):
            n = np.arange(L, dtype=np.float64)
            diff = n[None, :] - n[:, None]
            DT = np.where(diff >= 0, np.exp(np.maximum(diff, 0) * lg), 0.0) * sc
            dtab[0:L, dto + h * L:dto + (h + 1) * L] = DT
            dtab[:, xio + h * L:xio + (h + 1) * L] = np.exp((n + 1.0) * lg)[None, :]
            dtab[0:L, zeo + h] = np.exp((L - 1.0 - n) * lg) * sc
    mcol = np.zeros((128, 1024), np.float32)
    for i in range(8):
        mcol[:, i * 128:i * 128 + 4 * shift] = NEG
    vsv = np.ones((128, 3), np.float32)
    vsv[:, 0:shift] = 0.0
    return rot, dtab, mcol, vsv


def kernel(x_prompt, x_sample, cache_k, cache_v, state_ret, c_prompt, c_sample, w_ada, b_ada, norm1, norm2, w_in,
           lam_q1, lam_k1, lam_q2, lam_k2, subln_a, subln_r, w_ba, w_br, w_o, rel_bias, w_pq, peer_keys, peer_u, peer_v,
           final_norm):
    f = lambda a: np.ascontiguousarray(np.asarray(a, dtype=np.float32))
    x_prompt, x_sample = f(x_prompt), f(x_sample)
    oht5 = np.zeros((32, 1280), np.float32)
    for i in range(1151):
        oht5[_t5_bucket(R0 - i), i] = 1.0
    zwin = np.zeros((128, 256), np.float32)
    zwin[:, 127] = 1.0
    shared = dict(
        w_ada=f(w_ada)[0], b_ada=f(b_ada)[0], norm1=f(norm1)[0], norm2=f(norm2)[0], w_in=f(w_in)[0],
        lamv=np.stack([f(lam_q1)[0], f(lam_k1)[0], f(lam_q2)[0], f(lam_k2)[0]]),
        subln=np.stack([f(subln_a)[0], f(subln_r)[0]]), w_ba=f(w_ba)[0], w_br=f(w_br)[0], w_o=f(w_o)[0],
        rel_bias=f(rel_bias), w_pq=f(w_pq)[0], peer_keys=f(peer_keys)[0].reshape(16, 128, 128),
        peer_u=f(peer_u)[0], peer_v=f(peer_v)[0], final_norm=f(final_norm), oht5=oht5,
        jrev=np.ascontiguousarray(np.eye(128, dtype=np.float32)[::-1]), ident=np.eye(128, dtype=np.float32), zwin=zwin,
        iota16=np.tile(np.arange(16, dtype=np.float32)[None, :], (128, 1)),
    )
    tabs = [_static_tables(r) for r in range(4)]
    in_maps = []
    for c in range(8):
        b, r = c // 4, c % 4
        shift = 3 - r
        xa = np.zeros((NT * 128, D), np.float32)
        xa[shift * 512:] = x_prompt[b, :NT * 128 - shift * 512]
        rot, dtab, mcol, vsv = tabs[r]
        m = dict(shared)
        m.update(x_all=xa, x_s=x_sample[c], cache_k=f(cache_k)[0, c].reshape(1024, 512), cache_v=f(cache_v)[0, c].reshape(1024, 512),
                 state_in=f(state_ret)[0, c], c_both=np.stack([f(c_prompt)[b], f(c_sample)[c]]),
                 rot=rot, dtab=dtab, mcol=mcol, vsv=vsv)
        in_maps.append(m)
    nc = build_nc()
    if STOP:
        return nc, in_maps
    res = run_bass_kernel_spmd(nc, in_maps, core_ids=list(range(8)))
    R = res.results
    y_prompt = np.zeros((2, 16384, D), np.float32)
    k_prompt = np.zeros((1, 2, 16384, 4, 128), np.float32)
    v_prompt = np.zeros((1, 2, 16384, 4, 128), np.float32)
    ret_prompt = np.zeros((1, 2, 4, 128, 128), np.float32)
    y_sample = np.zeros((8, 32, D), np.float32)
    k_sample = np.zeros((1, 8, 32, 4, 128), np.float32)
    v_sample = np.zeros((1, 8, 32, 4, 128), np.float32)
    ret_sample = np.zeros((1, 8, 4, 128, 128), np.float32)
    for c in range(8):
        b, r = c // 4, c % 4
        shift = 3 - r
        for oi, s in enumerate(OWN):
            t0 = s * 128 - shift * 512
            y_prompt[b, t0:t0 + 128] = R[c]["y_own"][oi * 128:(oi + 1) * 128]
            k_prompt[0, b, t0:t0 + 128] = R[c]["k_own"][oi * 128:(oi + 1) * 128].reshape(128, 4, 128)
            v_prompt[0, b, t0:t0 + 128] = R[c]["v_own"][oi * 128:(oi + 1) * 128].reshape(128, 4, 128)
        if r == 3:
            ret_prompt[0, b] = R[c]["ret_p"]
        y_sample[c] = R[c]["y_s"]
        k_sample[0, c] = R[c]["k_s"].reshape(32, 4, 128)
        v_sample[0, c] = R[c]["v_s"].reshape(32, 4, 128)
        ret_sample[0, c] = R[c]["ret_s"]
    return (y_prompt, y_sample, k_prompt, v_prompt, ret_prompt, k_sample, v_sample, ret_sample)
```
